# Optimizing a Trainium2 kernel written in Bass

```python
import math
import jax, jax.numpy as jnp
from jax import lax
import numpy as np

D_MODEL = 1024
BATCH = 16
SEQ = 2048
DEPTH = 2

SSD_HEADS = 16
SSD_HEAD_DIM = 64
SSD_INNER = SSD_HEADS * SSD_HEAD_DIM
SSD_GROUPS = 2
SSD_STATE = 128
SSD_CONV = 4
SSD_CHUNK = 128
SSD_CONV_CH = SSD_INNER + 2 * SSD_GROUPS * SSD_STATE
SB_HEADS = 16
SB_HEAD_DIM = 64
SB_INNER = SB_HEADS * SB_HEAD_DIM
SB_BLOCK = 128
HY_SPLITS = [SSD_INNER, SSD_INNER + SSD_CONV_CH, SSD_INNER + SSD_CONV_CH + SSD_HEADS,
             SSD_INNER + SSD_CONV_CH + SSD_HEADS + SB_INNER,
             SSD_INNER + SSD_CONV_CH + SSD_HEADS + 2 * SB_INNER]
HY_IN_COLS = SSD_INNER + SSD_CONV_CH + SSD_HEADS + 3 * SB_INNER
HY_MIX = SSD_INNER + SB_INNER
ML_HEADS = 8
ML_QK_DIM = 64
ML_V_DIM = 128
ML_QK = ML_HEADS * ML_QK_DIM
ML_V = ML_HEADS * ML_V_DIM
ML_CHUNK = 128
ML_GATE_CAP = 15.0
ML_SPLITS = [ML_QK, 2 * ML_QK, 2 * ML_QK + ML_V, 2 * ML_QK + 2 * ML_V,
             2 * ML_QK + 2 * ML_V + ML_HEADS]
ML_IN_COLS = 2 * ML_QK + 2 * ML_V + 2 * ML_HEADS
D_FF = 2816
FFN_CONV = 3
RMS_EPS = 1e-6
N_EVEN = (DEPTH + 1) // 2
N_ODD = DEPTH // 2

kernel_name = "hybrid_ssd_stickbreak_mlstm_convffn"


def rmsnorm(x, w):
    xf = x.astype(jnp.float32)
    y = xf * lax.rsqrt(jnp.mean(xf * xf, axis=-1, keepdims=True) + RMS_EPS)
    return (y * w.astype(jnp.float32)).astype(x.dtype)


def causal_dwconv(x, w, b):
    k_width, ch = w.shape
    y = lax.conv_general_dilated(
        x, w[:, None, :].astype(x.dtype), window_strides=(1,),
        padding=[(k_width - 1, 0)], dimension_numbers=("NWC", "WIO", "NWC"),
        feature_group_count=ch)
    return y + b.astype(x.dtype)


def soft_cap(x, cap):
    return cap * jnp.tanh(x / cap)


def ssd_scan(xh, dt, a, bm, cm):
    bsz, seq, n_heads, p = xh.shape
    g, n = bm.shape[-2:]
    r = n_heads // g
    nc, l = seq // SSD_CHUNK, SSD_CHUNK
    x = (xh * dt[..., None]).reshape(bsz, nc, l, g, r, p)
    da = (dt * a).reshape(bsz, nc, l, g, r)
    bm = bm.reshape(bsz, nc, l, g, n)
    cm = cm.reshape(bsz, nc, l, g, n)
    a_cs = jnp.cumsum(da, axis=2)
    tri = jnp.tril(jnp.ones((l, l), dtype=bool))
    seg = a_cs[:, :, :, None] - a_cs[:, :, None, :]
    decay = jnp.exp(jnp.where(tri[:, :, None, None], seg, -jnp.inf))
    cb = jnp.einsum("bctgn,bcsgn->bctsg", cm, bm)
    y_diag = jnp.einsum("bctsg,bctsgr,bcsgrp->bctgrp", cb, decay, x)
    decay_end = jnp.exp(a_cs[:, :, -1:] - a_cs)
    chunk_states = jnp.einsum("bcsgn,bcsgr,bcsgrp->bcgrpn", bm, decay_end, x)
    chunk_decay = jnp.exp(a_cs[:, :, -1])

    def step(state, inp):
        dec, new = inp
        return dec[..., None, None] * state + new, state

    init = jnp.zeros((bsz, g, r, p, n), x.dtype)
    _, prev = lax.scan(step, init, (jnp.moveaxis(chunk_decay, 1, 0),
                                    jnp.moveaxis(chunk_states, 1, 0)))
    prev = jnp.moveaxis(prev, 0, 1)
    y_off = jnp.einsum("bctgn,bcgrpn,bctgr->bctgrp", cm, prev, jnp.exp(a_cs))
    return (y_diag + y_off).reshape(bsz, seq, n_heads, p)


def stick_breaking(q, k, v):
    bsz, n_heads, seq, dh = q.shape
    scale = dh ** -0.5
    outs = []
    for blk in range(seq // SB_BLOCK):
        t0 = blk * SB_BLOCK
        t1 = t0 + SB_BLOCK
        z = jnp.einsum("bhtd,bhsd->bhts", q[:, :, t0:t1], k[:, :, :t1]).astype(jnp.float32) * scale
        t_pos = t0 + jnp.arange(SB_BLOCK)
        s_pos = jnp.arange(t1)
        causal = s_pos[None, :] < t_pos[:, None]
        log_rem = jnp.where(causal, jax.nn.log_sigmoid(-z), 0.0)
        rem_after = lax.cumsum(log_rem, axis=3, reverse=True) - log_rem
        w = jnp.where(causal, jnp.exp(jax.nn.log_sigmoid(z) + rem_after), 0.0)
        outs.append(jnp.einsum("bhts,bhsd->bhtd", w.astype(v.dtype), v[:, :, :t1]))
    return jnp.concatenate(outs, axis=2)


def mlstm_chunkwise(q, k, v, logi, logf):
    bsz, seq, n_heads, kd = q.shape
    vd = v.shape[-1]
    nc, l = seq // ML_CHUNK, ML_CHUNK
    q = q.reshape(bsz, nc, l, n_heads, kd)
    k = k.reshape(bsz, nc, l, n_heads, kd)
    v = v.reshape(bsz, nc, l, n_heads, vd)
    logi = logi.reshape(bsz, nc, l, n_heads)
    logf = logf.reshape(bsz, nc, l, n_heads)
    b_cs = jnp.cumsum(logf, axis=2)
    g = b_cs[:, :, -1]
    w_end = g[:, :, None] - b_cs + logi
    m_loc = jnp.max(w_end, axis=2)
    e_end = jnp.exp(w_end - m_loc[:, :, None])
    loc_c = jnp.einsum("bcsh,bcshk,bcshv->bchkv", e_end, k, v)
    loc_n = jnp.einsum("bcsh,bcshk->bchk", e_end, k)

    def step(carry, inp):
        c_st, n_st, m_st = carry
        g_c, m_loc_c, lc, ln = inp
        m_new = jnp.maximum(g_c + m_st, m_loc_c)
        a_old = jnp.exp(g_c + m_st - m_new)
        a_loc = jnp.exp(m_loc_c - m_new)
        c_new = a_old[..., None, None] * c_st + a_loc[..., None, None] * lc
        n_new = a_old[..., None] * n_st + a_loc[..., None] * ln
        return (c_new, n_new, m_new), (c_st, n_st, m_st)

    init = (jnp.zeros((bsz, n_heads, kd, vd), q.dtype),
            jnp.zeros((bsz, n_heads, kd), q.dtype),
            jnp.zeros((bsz, n_heads), q.dtype))
    mv = lambda t: jnp.moveaxis(t, 1, 0)
    _, (c_prev, n_prev, m_prev) = lax.scan(step, init, (mv(g), mv(m_loc), mv(loc_c), mv(loc_n)))
    c_prev, n_prev, m_prev = mv(c_prev), mv(n_prev), mv(m_prev)
    bt = jnp.moveaxis(b_cs, 3, 2)
    it = jnp.moveaxis(logi, 3, 2)
    tri = jnp.tril(jnp.ones((l, l), dtype=bool))
    dmat = jnp.where(tri, bt[..., :, None] - bt[..., None, :] + it[..., None, :], -jnp.inf)
    inter = bt + m_prev[..., None]
    m_t = jnp.maximum(inter, jnp.max(dmat, axis=-1))
    sw = jnp.exp(dmat - m_t[..., None]) * jnp.einsum("bcthk,bcshk->bchts", q, k)
    inter_w = jnp.exp(inter - m_t)
    num = (jnp.einsum("bchts,bcshv->bcthv", sw, v)
           + jnp.einsum("bcthk,bchkv->bcthv", q, c_prev) * jnp.moveaxis(inter_w, 2, 3)[..., None])
    den = jnp.sum(sw, axis=-1) + jnp.einsum("bcthk,bchk->bcht", q, n_prev) * inter_w
    denom = jnp.maximum(jnp.abs(den), jnp.exp(-m_t))
    h = num / jnp.moveaxis(denom, 2, 3)[..., None]
    return h.reshape(bsz, seq, n_heads, vd)


def ssd_stickbreak_mixer(h, in_w, conv_w, conv_b, dt_bias, a_log, d_skip, ssd_norm_w,
                         q_norm_w, k_norm_w, out_w):
    bsz, seq, _ = h.shape
    f32 = jnp.float32
    z, xbc, dt_raw, q, k, v = jnp.split(h @ in_w, HY_SPLITS, axis=-1)
    xbc = jax.nn.silu(causal_dwconv(xbc, conv_w, conv_b))
    xs, bm, cm = jnp.split(xbc, [SSD_INNER, SSD_INNER + SSD_GROUPS * SSD_STATE], axis=-1)
    dt = jax.nn.softplus(dt_raw.astype(f32) + dt_bias.astype(f32))
    a = -jnp.exp(a_log.astype(f32))
    xh = xs.reshape(bsz, seq, SSD_HEADS, SSD_HEAD_DIM).astype(f32)
    y = ssd_scan(xh, dt, a,
                 bm.reshape(bsz, seq, SSD_GROUPS, SSD_STATE).astype(f32),
                 cm.reshape(bsz, seq, SSD_GROUPS, SSD_STATE).astype(f32))
    y = (y + xh * d_skip.astype(f32)[:, None]).reshape(bsz, seq, SSD_INNER).astype(h.dtype)
    y_ssd = rmsnorm(y * jax.nn.silu(z), ssd_norm_w)
    heads = lambda t: t.reshape(bsz, seq, SB_HEADS, SB_HEAD_DIM)
    qh = rmsnorm(heads(q), q_norm_w).transpose(0, 2, 1, 3)
    kh = rmsnorm(heads(k), k_norm_w).transpose(0, 2, 1, 3)
    vh = heads(v).transpose(0, 2, 1, 3)
    y_sb = stick_breaking(qh, kh, vh).transpose(0, 2, 1, 3).reshape(bsz, seq, SB_INNER)
    return jnp.concatenate([y_ssd, y_sb], axis=-1) @ out_w


def mlstm_mixer(h, in_w, i_b, f_b, norm_w, out_w):
    bsz, seq, _ = h.shape
    f32 = jnp.float32
    q, k, v, o, i_pre, f_pre = jnp.split(h @ in_w, ML_SPLITS, axis=-1)
    q = q.reshape(bsz, seq, ML_HEADS, ML_QK_DIM).astype(f32) * (ML_QK_DIM ** -0.5)
    k = k.reshape(bsz, seq, ML_HEADS, ML_QK_DIM).astype(f32)
    v = v.reshape(bsz, seq, ML_HEADS, ML_V_DIM).astype(f32)
    logi = soft_cap(i_pre.astype(f32) + i_b.astype(f32), ML_GATE_CAP)
    logf = jax.nn.log_sigmoid(soft_cap(f_pre.astype(f32) + f_b.astype(f32), ML_GATE_CAP))
    hm = mlstm_chunkwise(q, k, v, logi, logf)
    hm = rmsnorm(hm, norm_w.reshape(ML_HEADS, ML_V_DIM)).reshape(bsz, seq, ML_V).astype(h.dtype)
    return (hm * jax.nn.sigmoid(o)) @ out_w


def conv_ffn(h, up_w, conv_w, conv_b, down_w):
    u = causal_dwconv(h @ up_w, conv_w, conv_b)
    gate, val = jnp.split(u, 2, axis=-1)
    return (jax.nn.silu(gate) * val) @ down_w


def setup_inputs(seed: int = 0) -> dict:
    key = jax.random.key(seed)
    ks = jax.random.split(key, 24)
    f32 = jnp.float32
    nrm = lambda kk, shape, s: jax.random.normal(kk, shape, f32) * s
    dt0 = jnp.exp(jax.random.uniform(ks[5], (N_EVEN, SSD_HEADS), f32, math.log(1e-3), math.log(1e-1)))
    return {
        "x": nrm(ks[0], (BATCH, SEQ, D_MODEL), 1.0),
        "norm_w": 1.0 + nrm(ks[1], (DEPTH, 2, D_MODEL), 0.02),
        "hy_in_w": nrm(ks[2], (N_EVEN, D_MODEL, HY_IN_COLS), D_MODEL ** -0.5),
        "ssd_conv_w": nrm(ks[3], (N_EVEN, SSD_CONV, SSD_CONV_CH), SSD_CONV ** -0.5),
        "ssd_conv_b": nrm(ks[4], (N_EVEN, SSD_CONV_CH), 0.02),
        "ssd_dt_bias": dt0 + jnp.log(-jnp.expm1(-dt0)),
        "ssd_a_log": jnp.log(jax.random.uniform(ks[6], (N_EVEN, SSD_HEADS), f32, 1.0, 16.0)),
        "ssd_d": 1.0 + nrm(ks[7], (N_EVEN, SSD_HEADS), 0.02),
        "ssd_norm_w": 1.0 + nrm(ks[8], (N_EVEN, SSD_INNER), 0.02),
        "sb_q_norm_w": 1.0 + nrm(ks[9], (N_EVEN, SB_HEAD_DIM), 0.02),
        "sb_k_norm_w": 1.0 + nrm(ks[10], (N_EVEN, SB_HEAD_DIM), 0.02),
        "hy_out_w": nrm(ks[11], (N_EVEN, HY_MIX, D_MODEL), HY_MIX ** -0.5),
        "ml_in_w": nrm(ks[12], (N_ODD, D_MODEL, ML_IN_COLS), D_MODEL ** -0.5),
        "ml_i_b": nrm(ks[13], (N_ODD, ML_HEADS), 0.01),
        "ml_f_b": jnp.linspace(3.0, 6.0, ML_HEADS, dtype=f32) + nrm(ks[14], (N_ODD, ML_HEADS), 0.02),
        "ml_norm_w": 1.0 + nrm(ks[15], (N_ODD, ML_V), 0.02),
        "ml_out_w": nrm(ks[16], (N_ODD, ML_V, D_MODEL), ML_V ** -0.5),
        "ffn_up_w": nrm(ks[17], (DEPTH, D_MODEL, 2 * D_FF), D_MODEL ** -0.5),
        "ffn_conv_w": nrm(ks[18], (DEPTH, FFN_CONV, 2 * D_FF), FFN_CONV ** -0.5),
        "ffn_conv_b": nrm(ks[19], (DEPTH, 2 * D_FF), 0.02),
        "ffn_down_w": nrm(ks[20], (DEPTH, D_FF, D_MODEL), D_FF ** -0.5),
    }


def reference(x, norm_w, hy_in_w, ssd_conv_w, ssd_conv_b, ssd_dt_bias, ssd_a_log, ssd_d,
              ssd_norm_w, sb_q_norm_w, sb_k_norm_w, hy_out_w, ml_in_w, ml_i_b, ml_f_b,
              ml_norm_w, ml_out_w, ffn_up_w, ffn_conv_w, ffn_conv_b, ffn_down_w):
    h = x
    for layer in range(DEPTH):
        j = layer // 2
        hn = rmsnorm(h, norm_w[layer, 0])
        if layer % 2 == 0:
            h = h + ssd_stickbreak_mixer(hn, hy_in_w[j], ssd_conv_w[j], ssd_conv_b[j],
                                         ssd_dt_bias[j], ssd_a_log[j], ssd_d[j], ssd_norm_w[j],
                                         sb_q_norm_w[j], sb_k_norm_w[j], hy_out_w[j])
        else:
            h = h + mlstm_mixer(hn, ml_in_w[j], ml_i_b[j], ml_f_b[j], ml_norm_w[j], ml_out_w[j])
        h = h + conv_ffn(rmsnorm(h, norm_w[layer, 1]), ffn_up_w[layer], ffn_conv_w[layer],
                         ffn_conv_b[layer], ffn_down_w[layer])
    return h
```

```python
import numpy as np
from contextlib import ExitStack
import concourse.bass as bass
import concourse.mybir as mybir
from concourse.bass_utils import run_bass_kernel_spmd

F32 = mybir.dt.float32
BF16 = mybir.dt.bfloat16
AF = mybir.ActivationFunctionType
ALU = mybir.AluOpType

NCORES = 8
NTOK = 4096
L = 2048
D = 1024
EPS = 1e-6
HYC = 5648
MLC = 3088
DFF = 2816
NTT = 8
DBGPRINT = False
OPLIMIT = None

ENGS = ("pe", "act", "dve", "pool", "sp")
NDSEM = 8


class Ins:
    __slots__ = ("eng", "fn", "deps", "is_dma", "needed", "ticket", "dslot", "dval", "waits", "q_n", "seq")

    def __init__(self, eng, fn, is_dma):
        self.eng = eng
        self.fn = fn
        self.is_dma = is_dma
        self.deps = []
        self.needed = False
        self.ticket = 0
        self.waits = []


class _Rec:
    def __getattr__(self, name):
        def f(*a, **kw):
            self.call = (name, a, kw)
        return f


class Sched:
    def __init__(self, nc, es):
        self.nc = nc
        self.sems = {}
        for e in ENGS:
            self.sems[("c", e)] = es.enter_context(nc.semaphore("c_" + e))
            for s in range(NDSEM):
                self.sems[("d", e, s)] = es.enter_context(nc.semaphore("d_%s_%d" % (e, s)))
        self.cnt = {e: 0 for e in ENGS}
        self.ndma = {e: 0 for e in ENGS}
        self.dma_hist = {e: [] for e in ENGS}
        self.hw = {e: {} for e in ENGS}
        self.nseq = 0
        self.limit = OPLIMIT
        self._reset()

    def _reset(self):
        self.streams = {e: [] for e in ENGS}
        self.all = []
        self.state = {}

    def _rec(self, ins, reads, writes, accumulate=False):
        if self.limit is not None and self.nseq >= self.limit:
            ins.deps = []
            ins.seq = self.nseq
            self.nseq += 1
            return ins
        deps = []
        for k in reads:
            st = self.state.get(k)
            if st:
                deps.extend(st[0])
        for k in writes:
            st = self.state.get(k)
            if st:
                deps.extend(st[1])
                deps.extend(st[0])
        out = []
        seen = set()
        for d in deps:
            if d is ins or id(d) in seen:
                continue
            seen.add(id(d))
            if (not d.is_dma) and (not ins.is_dma) and d.eng == ins.eng == "pe":
                continue
            out.append(d)
        last = {}
        red = []
        for d in out:
            if d.is_dma:
                red.append(d)
            elif d.eng not in last or d.seq > last[d.eng].seq:
                last[d.eng] = d
        red.extend(last.values())
        ins.deps = red
        ins.seq = self.nseq
        self.nseq += 1
        for k in reads:
            st = self.state.setdefault(k, [[], []])
            st[1].append(ins)
        for k in writes:
            st = self.state.setdefault(k, [[], []])
            if accumulate:
                st[0].append(ins)
            else:
                st[0] = [ins]
            st[1] = []
        self.all.append(ins)
        self.streams[ins.eng].append(ins)
        return ins

    def op(self, eng, fn, reads=(), writes=(), accumulate=False):
        rec = _Rec()
        fn(rec)
        name, a, kw = rec.call
        real = (lambda e, name=name, a=a, kw=kw: getattr(e, name)(*a, **kw))
        return self._rec(Ins(eng, real, False), list(reads), list(writes), accumulate)

    def dma(self, q, out, in_, reads=(), writes=()):
        ins = Ins(q, (lambda e, out=out, in_=in_: e.dma_start(out=out, in_=in_)), True)
        n = self.ndma[q]
        self.ndma[q] = n + 1
        ins.q_n = n
        ins.dslot = n % NDSEM
        ins.dval = 16 * (n // NDSEM + 1)
        hist = self.dma_hist[q]
        if self.limit is not None and self.nseq >= self.limit:
            self.ndma[q] = n
            self.nseq += 1
            return ins
        self._rec(ins, list(reads), list(writes))
        if n >= NDSEM:
            ins.deps.append(hist[n - NDSEM])
        hist.append(ins)
        return ins

    def flush(self):
        nc = self.nc
        for ins in self.all:
            for d in ins.deps:
                d.needed = True
        tails = []
        for e in ENGS:
            for ins in reversed(self.streams[e]):
                if not ins.is_dma:
                    ins.needed = True
                    tails.append(ins)
                    break
            hist = self.dma_hist[e]
            for ins in hist[max(0, len(hist) - NDSEM):]:
                tails.append(ins)
        for ins in self.all:
            if not ins.is_dma and ins.needed:
                self.cnt[ins.eng] += 1
                ins.ticket = self.cnt[ins.eng]
        hw = self.hw

        def key_val(d):
            if d.is_dma:
                return ("d", d.eng, d.dslot), d.dval
            return ("c", d.eng), d.ticket

        for ins in self.all:
            for d in ins.deps:
                key, val = key_val(d)
                if hw[ins.eng].get(key, 0) >= val:
                    continue
                hw[ins.eng][key] = val
                ins.waits.append((key, val))
        final = {e: [] for e in ENGS}
        for e in ENGS:
            for d in tails:
                key, val = key_val(d)
                if key == ("c", e) or hw[e].get(key, 0) >= val:
                    continue
                hw[e][key] = val
                final[e].append((key, val))
        sems = self.sems
        streams = self.streams

        def mk(ename):
            def body(eng):
                for ins in streams[ename]:
                    for key, val in ins.waits:
                        eng.wait_ge(sems[key], val)
                    r = ins.fn(eng)
                    if ins.is_dma:
                        r.then_inc(sems[("d", ename, ins.dslot)], 16)
                    elif ins.needed:
                        r.then_inc(sems[("c", ename)], 1)
                for key, val in final[ename]:
                    eng.wait_ge(sems[key], val)
            return body

        with nc.Block() as block:
            block.tensor(mk("pe"))
            block.scalar(mk("act"))
            block.vector(mk("dve"))
            block.gpsimd(mk("pool"))
            block.sync(mk("sp"))
        self._reset()


class Ring:
    def __init__(self, alloc, name, n, shape, dtype):
        self.tiles = [alloc("%s%d" % (name, i), shape, dtype) for i in range(n)]
        self.name = name
        self.i = 0

    def nxt(self):
        i = self.i % len(self.tiles)
        self.i += 1
        return self.tiles[i], (self.name, i)


def _col(vec):
    v = np.asarray(vec, np.float32).reshape(-1, 128)
    return np.ascontiguousarray(v.T)


PCOL = {}
PROW = {}


def _layout_tables():
    c = 0
    for name, n in (("scw", 48), ("scb", 12), ("sdd", 8), ("snw", 8), ("qw", 1), ("kw", 1),
                    ("fcw0", 132), ("fcb0", 44), ("fcw1", 132), ("fcb1", 44), ("mnw", 8)):
        PCOL[name] = (c, n)
        c += n
    PCOL["_n"] = c
    r = 0
    for name, n in (("nw00", 1024), ("nw01", 1024), ("nw10", 1024), ("nw11", 1024),
                    ("dtb", 16), ("alog", 16), ("mib", 8), ("mfb", 8)):
        PROW[name] = (r, n)
        r += n
    PROW["_n"] = r


_layout_tables()


def pack_params(inp):
    pc = np.zeros((128, PCOL["_n"]), np.float32)

    def put(name, arr):
        c0, n = PCOL[name]
        assert arr.shape == (128, n), (name, arr.shape)
        pc[:, c0:c0 + n] = arr

    cw = inp["ssd_conv_w"][0]
    put("scw", np.stack([_col(cw[k]) for k in range(4)], axis=2).reshape(128, 48))
    put("scb", _col(inp["ssd_conv_b"][0]))
    put("sdd", _col(np.repeat(inp["ssd_d"][0], 64)))
    put("snw", _col(inp["ssd_norm_w"][0]))
    put("qw", _col(np.tile(inp["sb_q_norm_w"][0], 2)))
    put("kw", _col(np.tile(inp["sb_k_norm_w"][0], 2)))
    for l in range(2):
        fw = inp["ffn_conv_w"][l]
        put("fcw%d" % l, np.stack([_col(fw[k]) for k in range(3)], axis=2).reshape(128, 132))
        put("fcb%d" % l, _col(inp["ffn_conv_b"][l]))
    put("mnw", _col(inp["ml_norm_w"][0]))
    pr = np.zeros((PROW["_n"],), np.float32)

    def putr(name, v):
        r0, n = PROW[name]
        pr[r0:r0 + n] = np.asarray(v, np.float32).reshape(n)

    putr("nw00", inp["norm_w"][0, 0])
    putr("nw01", inp["norm_w"][0, 1])
    putr("nw10", inp["norm_w"][1, 0])
    putr("nw11", inp["norm_w"][1, 1])
    putr("dtb", inp["ssd_dt_bias"][0])
    putr("alog", inp["ssd_a_log"][0])
    putr("mib", inp["ml_i_b"][0])
    putr("mfb", inp["ml_f_b"][0])
    prow = np.ascontiguousarray(np.broadcast_to(pr[None, :], (128, pr.size)))
    return pc, prow


def make_consts():
    i = np.arange(128)
    eye = (i[:, None] == i[None, :]).astype(np.float32)
    le = (i[:, None] <= i[None, :]).astype(np.float32)
    gt = (i[:, None] > i[None, :]).astype(np.float32)
    ge = (i[:, None] >= i[None, :]).astype(np.float32)
    ones = np.ones((128, 128), np.float32)
    bd = ((i[:, None] // 64) == (i[None, :] // 64)).astype(np.float32)
    negbig = -30000.0 * eye
    negu = -ge
    return np.ascontiguousarray(np.concatenate([eye, le, gt, ge, ones, bd, negbig, negu], axis=1))


CI = {"eye": 0, "le": 1, "gt": 2, "ge": 3, "ones": 4, "bd": 5, "negbig": 6, "negu": 7}


class K:
    pass


def build(nphase=99, dbg=False):
    nc = bass.Bass("TRN2", target_bir_lowering=False)
    k = K()
    k.nc = nc
    k.sfx = ""
    ein = lambda n, s, d=F32: nc.dram_tensor(n, s, d, kind="ExternalInput").ap()
    skind = "ExternalOutput" if dbg else "Internal"
    scr = lambda n, s, d: nc.dram_tensor(n, s, d, kind=skind).ap()
    k.x = ein("x", [NTOK, D])
    k.pcol_d = ein("pcol", [128, PCOL["_n"]])
    k.prow_d = ein("prow", [128, PROW["_n"]])
    k.consts_d = ein("consts", [128, 8 * 128])
    k.hy_in_w = ein("hy_in_w", [D, HYC])
    k.hy_out_w = ein("hy_out_w", [2048, D])
    k.ml_in_w = ein("ml_in_w", [D, MLC])
    k.ml_out_w = ein("ml_out_w", [D, D])
    k.ffn_up_w = ein("ffn_up_w", [2, D, 2 * DFF])
    k.ffn_down_w = ein("ffn_down_w", [2, DFF, D])
    k.out = nc.dram_tensor("out", [NTOK, D], F32, kind="ExternalOutput").ap()
    k.szT = scr("szT", [1024, NTOK], BF16)
    k.xbcT = scr("xbcT", [1536, NTOK], BF16)
    k.dts = scr("dts", [NTOK, 16], F32)
    k.qT = scr("qT", [1024, NTOK], BF16)
    k.kT = scr("kT", [1024, NTOK], BF16)
    k.vtok = scr("vtok", [NTOK, 1024], BF16)
    k.ycatT = scr("ycatT", [2048, NTOK], BF16)
    k.h1 = scr("h1", [NTOK, D], F32)
    k.h2 = scr("h2", [NTOK, D], F32)
    k.h3 = scr("h3", [NTOK, D], F32)
    k.gT = scr("gT", [DFF, NTOK], BF16)
    k.mqT = scr("mqT", [512, NTOK], BF16)
    k.mkT = scr("mkT", [512, NTOK], BF16)
    k.soT = scr("soT", [1024, NTOK], BF16)
    k.mkt = scr("mkt", [NTOK, 512], BF16)
    k.mvt = scr("mvt", [NTOK, 1024], BF16)
    k.gts = scr("gts", [NTOK, 16], F32)
    k.hmT = scr("hmT", [1024, NTOK], BF16)

    with ExitStack() as es:
        S = Sched(nc, es)
        k.S = S
        sb = lambda n, s, d: es.enter_context(nc.sbuf_tensor(n, s, d))
        k.cf = sb("cf", [128, 8 * 128], F32)
        k.cb = sb("cb", [128, 8 * 128], BF16)
        k.pcol = sb("pcolt", [128, PCOL["_n"]], F32)
        k.prow = sb("prowt", [128, PROW["_n"]], F32)
        S.dma("sp", k.cf[:], k.consts_d, writes=["cf"])
        S.dma("pool", k.cb[:], k.consts_d, writes=["cb"])
        S.dma("sp", k.pcol[:], k.pcol_d, writes=["pcol"])
        S.dma("sp", k.prow[:], k.prow_d, writes=["prow"])
        k.CF = lambda name: k.cf[:, CI[name] * 128:(CI[name] + 1) * 128]
        k.CB = lambda name: k.cb[:, CI[name] * 128:(CI[name] + 1) * 128]
        k.PC = lambda name, i=0, n=1: k.pcol[:, PCOL[name][0] + i:PCOL[name][0] + i + n]
        k.PR = lambda name: k.prow[:, PROW[name][0]:PROW[name][0] + PROW[name][1]]

        phases = [phase_a0, phase_b0, phase_c0,
                  lambda k, pes: phase_proj_res(k, pes, k.hy_out_w, 16, k.ycatT, k.x, k.h1),
                  lambda k, pes: phase_ffn_up(k, pes, 0, k.h1, k.gT),
                  lambda k, pes: phase_proj_res(k, pes, k.ffn_down_w[0], 22, k.gT, k.h1, k.h2),
                  phase_e1, phase_f1,
                  lambda k, pes: phase_proj_res(k, pes, k.ml_out_w, 8, k.hmT, k.h2, k.h3),
                  lambda k, pes: phase_ffn_up(k, pes, 1, k.h3, k.gT),
                  lambda k, pes: phase_proj_res(k, pes, k.ffn_down_w[1], 22, k.gT, k.h3, k.out)]
        for pi, ph in enumerate(phases[:nphase]):
            k.sfx = "_p%d" % pi
            with ExitStack() as pes:
                ph(k, pes)
                S.flush()
    return nc


def norm_transpose(k, S, xt, xkey, nw, hn_ring, hnT, hnT_key, ptr_ring, junk, small):
    ssq, lnv, rstd = small
    for j in range(4):
        S.op("act", lambda e, j=j: e.activation(out=junk[:], in_=xt[:, j, :], func=AF.Square,
                                                accum_out=ssq[:, j:j + 1]),
             reads=[xkey], writes=["junk", "ssq"])
    S.op("act", lambda e: e.activation(out=lnv[:], in_=ssq[:], func=AF.Ln, bias=EPS, scale=1.0 / D),
         reads=["ssq"], writes=["lnv"])
    S.op("act", lambda e: e.activation(out=rstd[:], in_=lnv[:], func=AF.Exp, scale=-0.5),
         reads=["lnv"], writes=["rstd"])
    hn, hkey = hn_ring.nxt()
    for j in range(4):
        S.op("dve", lambda e, j=j: e.scalar_tensor_tensor(out=hn[:, j, :], in0=xt[:, j, :], scalar=rstd[:, j:j + 1],
                                                          in1=nw, op0=ALU.mult, op1=ALU.mult),
             reads=[xkey, "rstd", "prow"], writes=[(hkey, j)])
    for j in range(4):
        ptr, pkey = ptr_ring.nxt()
        for kc in range(8):
            S.op("pe", lambda e, j=j, kc=kc, ptr=ptr: e.transpose(out=ptr[:, kc * 128:(kc + 1) * 128],
                                                                  in_=hn[:, j, kc * 128:(kc + 1) * 128],
                                                                  identity=k.CB("eye")),
                 reads=[(hkey, j), "cb"], writes=[pkey], accumulate=(kc > 0))
        eng = "act" if j % 2 == 0 else "dve"
        if eng == "act":
            S.op("act", lambda e, j=j, ptr=ptr: e.copy(hnT[:, :, j * 128:(j + 1) * 128],
                                                       ptr[:].rearrange("p (c t) -> p c t", t=128)),
                 reads=[pkey], writes=[hnT_key])
        else:
            S.op("dve", lambda e, j=j, ptr=ptr: e.tensor_copy(hnT[:, :, j * 128:(j + 1) * 128],
                                                              ptr[:].rearrange("p (c t) -> p c t", t=128)),
                 reads=[pkey], writes=[hnT_key])


def phase_a0(k, es):
    nc, S = k.nc, k.S
    sb = lambda n, s, d: es.enter_context(nc.sbuf_tensor(n + k.sfx, s, d))
    ps = lambda n, s, d: es.enter_context(nc.psum_tensor(n + k.sfx, s, d))
    win = sb("win", [128, 8, HYC], BF16)
    for kc in range(8):
        S.dma("pool", win[:, kc, :], k.hy_in_w[kc * 128:(kc + 1) * 128, :], writes=[("win", kc)])
    winkeys = [("win", kc) for kc in range(8)]
    xt_ring = Ring(sb, "xt", 2, [128, 4, D], F32)
    hn_ring = Ring(sb, "hn", 1, [128, 4, D], BF16)
    hnT_ring = Ring(sb, "hnT", 2, [128, 8, 512], BF16)
    ptr_ring = Ring(ps, "ptr", 1, [128, 1024], BF16)
    pb_ring = Ring(ps, "pb", 5, [128, 512], F32)
    pss_ring = Ring(ps, "pss", 2, [128, 512], F32)
    junk = sb("junk", [128, D], BF16)
    small = (sb("ssq", [128, 4], F32), sb("lnv", [128, 4], F32), sb("rstd", [128, 4], F32))
    ob_ring = Ring(sb, "ob", 4, [128, 512], BF16)
    raw_ring = Ring(sb, "raw", 3, [128, 515], F32)
    acc_ring = Ring(sb, "acc", 4, [128, 512], F32)
    sq_ring = Ring(sb, "sqb", 3, [128, 512], BF16)
    ta_ring = Ring(sb, "ta", 2, [128, 512], F32)
    tb_ring = Ring(sb, "tb", 3, [128, 512], F32)
    halo = sb("halo", [128, 12, 3], F32)
    qws = sb("qws", [128, 1], F32)
    dtt_ring = Ring(sb, "dtt", 2, [128, 4, 16], F32)
    dte = sb("dte", [128, 64], F32)
    S.op("act", lambda e: e.mul(qws[:], k.PC("qw"), 0.125), reads=["pcol"], writes=["qws"])

    def load_x(tt):
        xt, xkey = xt_ring.nxt()
        S.dma("sp", xt[:], k.x[tt * 512:(tt + 1) * 512, :].rearrange("(j p) d -> p j d", p=128), writes=[xkey])
        return xt, xkey

    def prologue(xx):
        xt, xkey = xx
        hnT, hkey = hnT_ring.nxt()
        norm_transpose(k, S, xt, xkey, k.PR("nw00"), hn_ring, hnT, hkey, ptr_ring, junk, small)
        return hnT, hkey

    xs = {0: load_x(0)}
    if NTT > 1:
        xs[1] = load_x(1)
    pro = {0: prologue(xs[0])}
    for tt in range(NTT):
        if tt + 2 < NTT:
            xs[tt + 2] = load_x(tt + 2)
        hnT, hkey = pro[tt]
        tok0 = tt * 512
        seq_start = (tt % 4 == 0)

        def proj_fm(col0):
            pb, pkey = pb_ring.nxt()
            for kc in range(8):
                S.op("pe", lambda e, kc=kc, pb=pb: e.matmul(pb[:], lhsT=win[:, kc, col0:col0 + 128], rhs=hnT[:, kc, :],
                                                            start=(kc == 0), stop=(kc == 7)),
                     reads=[hkey, ("win", kc)], writes=[pkey], accumulate=(kc > 0))
            return pb, pkey

        for c in range(8):
            pb, pkey = proj_fm(c * 128)
            ob, okey = ob_ring.nxt()
            S.op("act", lambda e, pb=pb, ob=ob: e.activation(out=ob[:], in_=pb[:], func=AF.Silu),
                 reads=[pkey], writes=[okey])
            S.dma("sp", k.szT[c * 128:(c + 1) * 128, tok0:tok0 + 512], ob[:], reads=[okey])
        if DBGPRINT: print('a0 after z', S.nseq)
        def x1(c):
            pb, pkey = proj_fm(1024 + c * 128)
            raw, rkey = raw_ring.nxt()
            acc, akey = acc_ring.nxt()
            if seq_start:
                S.op("pool", lambda e: e.memset(raw[:, 0:3], 0.0), writes=[(rkey, "h")])
            else:
                S.op("pool", lambda e: e.tensor_copy(raw[:, 0:3], halo[:, c, :]), reads=[("halo", c)],
                     writes=[(rkey, "h")])
            S.op("act", lambda e: e.copy(raw[:, 3:515], pb[:]), reads=[pkey], writes=[rkey])
            S.op("act", lambda e: e.activation(out=acc[:], in_=pb[:], func=AF.Identity, bias=k.PC("scb", c),
                                               scale=k.PC("scw", c * 4 + 3)), reads=[pkey, "pcol"], writes=[akey])
            return (raw, rkey, acc, akey, c)

        def xtap(t, tp):
            raw, rkey, acc, akey, c = t
            S.op("dve", lambda e: e.scalar_tensor_tensor(out=acc[:], in0=raw[:, tp:tp + 512],
                                                         scalar=k.PC("scw", c * 4 + tp), in1=acc[:], op0=ALU.mult,
                                                         op1=ALU.add), reads=[rkey, (rkey, "h"), akey, "pcol"], writes=[akey])

        def xhalo(t):
            raw, rkey, acc, akey, c = t
            S.op("pool", lambda e: e.tensor_copy(halo[:, c, :], raw[:, 512:515]), reads=[rkey], writes=[("halo", c)])

        def x3(t):
            raw, rkey, acc, akey, c = t
            ob, okey = ob_ring.nxt()
            S.op("act", lambda e: e.activation(out=ob[:], in_=acc[:], func=AF.Silu), reads=[akey], writes=[okey])
            S.dma("sp", k.xbcT[c * 128:(c + 1) * 128, tok0:tok0 + 512], ob[:], reads=[okey])

        its = {}
        for n in range(12 + 3):
            if n < 12:
                its[n] = x1(n)
            a_ok, b_ok = 0 <= n - 1 < 12, 0 <= n - 2 < 12
            if a_ok:
                xtap(its[n - 1], 0)
            if b_ok:
                xtap(its[n - 2], 2)
                xhalo(its[n - 2])
            if a_ok:
                xtap(its[n - 1], 1)
            if 0 <= n - 3 < 12:
                x3(its[n - 3])
        if tt + 1 < NTT:
            pro[tt + 1] = prologue(xs[tt + 1])

        def q1(n):
            which, c = n // 8, n % 8
            pb, pkey = proj_fm((2576 if which == 0 else 3600) + c * 128)
            sq, sqkey = sq_ring.nxt()
            S.op("act", lambda e: e.activation(out=sq[:], in_=pb[:], func=AF.Square), reads=[pkey], writes=[sqkey])
            return {"pb": pb, "pkey": pkey, "sq": sq, "sqkey": sqkey, "which": which, "c": c}

        def q2(t):
            pss, psskey = pss_ring.nxt()
            sq, sqkey = t["sq"], t["sqkey"]
            S.op("pe", lambda e: e.matmul(pss[:], lhsT=k.CB("bd"), rhs=sq[:], start=True, stop=True),
                 reads=[sqkey, "cb"], writes=[psskey])
            ta, takey = ta_ring.nxt()
            tb, tbkey = tb_ring.nxt()
            S.op("act", lambda e: e.activation(out=ta[:], in_=pss[:], func=AF.Ln, bias=EPS, scale=1.0 / 64),
                 reads=[psskey], writes=[takey])
            S.op("act", lambda e: e.activation(out=tb[:], in_=ta[:], func=AF.Exp, scale=-0.5), reads=[takey], writes=[tbkey])
            t["tb"], t["tbkey"] = tb, tbkey

        def q3(t):
            ob, okey = ob_ring.nxt()
            pb, pkey, tb, tbkey, which, c = t["pb"], t["pkey"], t["tb"], t["tbkey"], t["which"], t["c"]
            wcol = qws[:, 0:1] if which == 0 else k.PC("kw")
            S.op("dve", lambda e: e.scalar_tensor_tensor(out=ob[:], in0=pb[:], scalar=wcol, in1=tb[:], op0=ALU.mult,
                                                         op1=ALU.mult), reads=[pkey, tbkey, "qws", "pcol"], writes=[okey])
            dst = k.qT if which == 0 else k.kT
            S.dma("sp", dst[c * 128:(c + 1) * 128, tok0:tok0 + 512], ob[:], reads=[okey])

        its = {}
        for n in range(16 + 2):
            if n < 16:
                its[n] = q1(n)
            if 0 <= n - 1 < 16:
                q2(its[n - 1])
            if 0 <= n - 2 < 16:
                q3(its[n - 2])
        if DBGPRINT: print('a0 after qk', S.nseq)
        pb, pkey = pb_ring.nxt()
        for j in range(4):
            for kc in range(8):
                S.op("pe", lambda e, j=j, kc=kc, pb=pb: e.matmul(pb[:, j * 16:(j + 1) * 16],
                                                                lhsT=hnT[:, kc, j * 128:(j + 1) * 128],
                                                                rhs=win[:, kc, 2560:2576], start=(kc == 0), stop=(kc == 7)),
                     reads=[hkey, ("win", kc)], writes=[pkey], accumulate=(j + kc > 0))
        dtt, dkey = dtt_ring.nxt()
        S.op("dve", lambda e, pb=pb: e.tensor_tensor(dte[:].rearrange("p (j h) -> p j h", h=16),
                                                     pb[:, 0:64].rearrange("p (j h) -> p j h", h=16),
                                                     k.PR("dtb").unsqueeze(1).to_broadcast([128, 4, 16]), op=ALU.add),
             reads=[pkey, "prow"], writes=["dte"])
        S.op("act", lambda e: e.activation(out=dte[:], in_=dte[:], func=AF.Exp), reads=["dte"], writes=["dte"])
        S.op("act", lambda e, dtt=dtt: e.activation(out=dtt[:].rearrange("p j h -> p (j h)"), in_=dte[:], func=AF.Ln,
                                                    bias=1.0),
             reads=["dte"], writes=[dkey])
        S.dma("sp", k.dts[tok0:tok0 + 512, :].rearrange("(j p) h -> p j h", p=128), dtt[:], reads=[dkey])
        if DBGPRINT: print('a0 after dt', S.nseq)
        for j in range(4):
            for half in range(2):
                pb, pkey = pb_ring.nxt()
                for kc in range(8):
                    S.op("pe", lambda e, j=j, kc=kc, pb=pb, half=half: e.matmul(
                        pb[:], lhsT=hnT[:, kc, j * 128:(j + 1) * 128],
                        rhs=win[:, kc, 4624 + half * 512:4624 + (half + 1) * 512], start=(kc == 0), stop=(kc == 7)),
                        reads=[hkey, ("win", kc)], writes=[pkey], accumulate=(kc > 0))
                ob, okey = ob_ring.nxt()
                if half == 0:
                    S.op("act", lambda e, pb=pb, ob=ob: e.copy(ob[:], pb[:]), reads=[pkey], writes=[okey])
                else:
                    S.op("dve", lambda e, pb=pb, ob=ob: e.tensor_copy(ob[:], pb[:]), reads=[pkey], writes=[okey])
                S.dma("sp", k.vtok[tok0 + j * 128:tok0 + (j + 1) * 128, half * 512:(half + 1) * 512], ob[:],
                      reads=[okey])


def phase_b0(k, es):
    nc, S = k.nc, k.S
    sb = lambda n, s, d: es.enter_context(nc.sbuf_tensor(n + k.sfx, s, d))
    ps = lambda n, s, d: es.enter_context(nc.psum_tensor(n + k.sfx, s, d))
    CF, CB, PC, PR = k.CF, k.CB, k.PC, k.PR
    xb_ring = Ring(sb, "xb", 2, [128, 12, 512], BF16)
    sz_ring = Ring(sb, "szt", 2, [128, 8, 512], BF16)
    dtl_ring = Ring(sb, "dtl", 2, [128, 4, 16], F32)
    yo_ring = Ring(sb, "yo", 2, [128, 8, 512], BF16)
    da_ring = Ring(sb, "da", 3, [128, 16], F32)
    sm_ring = Ring(sb, "sm", 3, [128, 48], F32)
    xtil_ring = Ring(sb, "xtil", 3, [128, 1024], BF16)
    xtd_ring = Ring(sb, "xtd", 3, [128, 1024], BF16)
    bmt_ring = Ring(sb, "bmt", 3, [128, 256], BF16)
    cbm_ring = Ring(sb, "cbm", 3, [128, 2, 128], F32)
    dec_ring = Ring(sb, "dec", 2, [128, 512], F32)
    eb_ring = Ring(sb, "eb", 2, [128, 512], F32)
    mt_ring = Ring(sb, "mt", 9, [128, 512], BF16)
    ce_ring = Ring(sb, "ce", 9, [128, 512], BF16)
    gv_ring = Ring(sb, "gv", 3, [128, 8, 128], F32)
    sq_ring = Ring(sb, "ssq2", 3, [128, 8, 128], BF16)
    rs_ring = Ring(sb, "rs", 2, [128, 128], F32)
    ln_ring = Ring(sb, "lnb", 2, [128, 128], F32)
    ab = sb("ab", [128, 16], F32)
    g4 = sb("g4", [128, 512], F32)
    prev_f = sb("prev_f", [128, 1024], F32)
    prev_b = sb("prev_b", [128, 1024], BF16)
    pmA_ring = Ring(ps, "pmA", 1, [128, 512], F32)
    ptx_ring = Ring(ps, "ptx", 1, [128, 1024], BF16)
    ptb_ring = Ring(ps, "ptb", 1, [128, 256], BF16)
    pw_ring = Ring(ps, "pw", 3, [128, 512], F32)
    py_ring = Ring(ps, "py", 2, [128, 512], F32)

    S.op("act", lambda e: e.activation(out=ab[:], in_=PR("alog"), func=AF.Exp), reads=["prow"], writes=["ab"])
    S.op("act", lambda e: e.mul(ab[:], ab[:], -1.0), reads=["ab"], writes=["ab"])
    for q in range(4):
        S.op("pool", lambda e, q=q: e.tensor_copy(g4[:, q * 128:(q + 1) * 128], CF("gt")), reads=["cf"], writes=["g4"],
             accumulate=(q > 0))

    def load(tt):
        tok0 = tt * 512
        xb, xkey = xb_ring.nxt()
        szt, skey = sz_ring.nxt()
        dtl, dkey = dtl_ring.nxt()
        S.dma("sp", xb[:], k.xbcT[:, tok0:tok0 + 512].rearrange("(c p) t -> p c t", p=128), writes=[xkey])
        S.dma("sp", szt[:], k.szT[:, tok0:tok0 + 512].rearrange("(c p) t -> p c t", p=128), writes=[skey])
        S.dma("sp", dtl[:], k.dts[tok0:tok0 + 512, :].rearrange("(j p) h -> p j h", p=128), writes=[dkey])
        return xb, xkey, szt, skey, dtl, dkey

    tiles = {}

    def get_tile(tt):
        if tt not in tiles:
            tiles[tt] = load(tt) + yo_ring.nxt()
        return tiles[tt]

    def P(c):
        xb, xkey, szt, skey, dtl, dkey, yo, yokey = get_tile(c["tt"])
        j = c["j"]
        js = slice(j * 128, (j + 1) * 128)
        da, dakey = da_ring.nxt()
        S.op("dve", lambda e: e.tensor_tensor(da[:], dtl[:, j, :], ab[:], op=ALU.mult), reads=[dkey, "ab"], writes=[dakey])
        pmA, pmkey = pmA_ring.nxt()
        for i, cname in enumerate(("le", "gt", "ones")):
            S.op("pe", lambda e: e.matmul(pmA[:, i * 16:(i + 1) * 16], lhsT=CF(cname), rhs=da[:], start=True, stop=True),
                 reads=[dakey, "cf"], writes=[(pmkey, "c")], accumulate=(i > 0))
        sm, smkey = sm_ring.nxt()
        S.op("act", lambda e: e.mul(sm[:, 0:16], pmA[:, 0:16], -1.0), reads=[(pmkey, "c")], writes=[smkey])
        S.op("act", lambda e: e.activation(out=sm[:, 16:48], in_=pmA[:, 16:48], func=AF.Exp), reads=[(pmkey, "c")],
             writes=[smkey], accumulate=True)
        ptx, ptxkey = ptx_ring.nxt()
        for fc in range(8):
            S.op("pe", lambda e: e.transpose(out=ptx[:, fc * 128:(fc + 1) * 128], in_=xb[:, fc, js], identity=CB("eye")),
                 reads=[xkey, "cb"], writes=[ptxkey], accumulate=(fc > 0))
        xtil, xtkey = xtil_ring.nxt()
        xtd, xdkey = xtd_ring.nxt()
        S.op("dve", lambda e: e.tensor_tensor(
            xtil[:].rearrange("p (h d) -> p h d", d=64), ptx[:].rearrange("p (h d) -> p h d", d=64),
            dtl[:, j, :].unsqueeze(2).to_broadcast([128, 16, 64]), op=ALU.mult), reads=[ptxkey, dkey], writes=[xtkey])
        S.op("pool", lambda e: e.tensor_tensor(
            xtd[:].rearrange("p (h d) -> p h d", d=64), xtil[:].rearrange("p (h d) -> p h d", d=64),
            sm[:, 16:32].unsqueeze(2).to_broadcast([128, 16, 64]), op=ALU.mult), reads=[xtkey, smkey], writes=[xdkey])
        ptb, ptbkey = ptb_ring.nxt()
        for g in range(2):
            S.op("pe", lambda e: e.transpose(out=ptb[:, g * 128:(g + 1) * 128], in_=xb[:, 8 + g, js], identity=CB("eye")),
                 reads=[xkey, "cb"], writes=[ptbkey], accumulate=(g > 0))
        bmt, bmkey = bmt_ring.nxt()
        S.op("act", lambda e: e.copy(bmt[:], ptb[:]), reads=[ptbkey], writes=[bmkey])
        for g in range(2):
            S.op("pe", lambda e: e.matmul(pmA[:, 64 + g * 128:64 + (g + 1) * 128], lhsT=xb[:, 8 + g, js],
                                          rhs=xb[:, 10 + g, js], start=True, stop=True),
                 reads=[xkey], writes=[(pmkey, "cb")], accumulate=(g > 0))
        cbm, cbkey = cbm_ring.nxt()
        S.op("dve", lambda e: e.tensor_tensor(cbm[:], pmA[:, 64:320].rearrange("p (g t) -> p g t", t=128),
                                              CF("le").unsqueeze(1).to_broadcast([128, 2, 128]), op=ALU.mult),
             reads=[(pmkey, "cb"), "cf"], writes=[cbkey])
        c.update(da=da, dakey=dakey, pmA=pmA, pmkey=pmkey, sm=sm, smkey=smkey, xtil=xtil, xtkey=xtkey, xtd=xtd,
                 xdkey=xdkey, bmt=bmt, bmkey=bmkey, cbm=cbm, cbkey=cbkey)
        c["mt"], c["ce"] = [], []
        for hq in range(4):
            g = hq // 2
            pam, pamkey = pw_ring.nxt()
            pau, paukey = pw_ring.nxt()
            S.op("pe", lambda e: e.matmul(pam[:], lhsT=CF("negbig"), rhs=g4[:], start=True, stop=False),
                 reads=["cf", "g4"], writes=[pamkey])
            for hh in range(4):
                h = hq * 4 + hh
                cs = slice(hh * 128, (hh + 1) * 128)
                lb = da[:, h:h + 1].to_broadcast([128, 128])
                S.op("pe", lambda e: e.matmul(pam[:, cs], lhsT=lb, rhs=CF("le"), start=False, stop=True),
                     reads=[dakey, "cf"], writes=[pamkey], accumulate=True)
                S.op("pe", lambda e: e.matmul(pau[:, cs], lhsT=lb, rhs=CF("le"), start=True, stop=True),
                     reads=[dakey, "cf"], writes=[paukey], accumulate=(hh > 0))
            eb, ebkey = eb_ring.nxt()
            S.op("act", lambda e: e.activation(out=eb[:], in_=pau[:], func=AF.Exp), reads=[paukey], writes=[ebkey])
            dec, deckey = dec_ring.nxt()
            for hh in range(4):
                h = hq * 4 + hh
                cs = slice(hh * 128, (hh + 1) * 128)
                S.op("act", lambda e: e.activation(out=dec[:, cs], in_=pam[:, cs], func=AF.Exp, bias=sm[:, h:h + 1]),
                     reads=[pamkey, smkey], writes=[deckey], accumulate=(hh > 0))
            mt, mtkey = mt_ring.nxt()
            S.op("dve", lambda e: e.tensor_tensor(mt[:].rearrange("p (h t) -> p h t", t=128),
                                                  dec[:].rearrange("p (h t) -> p h t", t=128),
                                                  cbm[:, g, :].unsqueeze(1).to_broadcast([128, 4, 128]), op=ALU.mult),
                 reads=[deckey, cbkey], writes=[mtkey])
            ce, cekey = ce_ring.nxt()
            S.op("dve", lambda e: e.tensor_tensor(ce[:].rearrange("p (h t) -> p h t", t=128),
                                                  eb[:].rearrange("p (h t) -> p h t", t=128),
                                                  xb[:, 10 + g, js].unsqueeze(1).to_broadcast([128, 4, 128]), op=ALU.mult),
                 reads=[xkey, ebkey], writes=[cekey])
            c["mt"].append((mt, mtkey))
            c["ce"].append((ce, cekey))

    def H(c):
        xtil, xtkey = c["xtil"], c["xtkey"]
        if c["tt"] % 4 == 0 and c["j"] == 0:
            S.op("pool", lambda e: e.memset(prev_f[:], 0.0), writes=["prev_f"])
            S.op("pool", lambda e: e.memset(prev_b[:], 0.0), writes=["prev_b"])
        pys = [py_ring.nxt(), py_ring.nxt()]
        c["pys"] = pys
        for h in range(16):
            mt, mtkey = c["mt"][h // 4]
            ce, cekey = c["ce"][h // 4]
            cs = slice((h % 4) * 128, (h % 4 + 1) * 128)
            py, pykey = pys[h // 8]
            fcl = (h // 2) % 4
            po = (h % 2) * 64
            outap = py[po:po + 64, fcl * 128:(fcl + 1) * 128]
            S.op("pe", lambda e: e.matmul(outap, lhsT=xtil[:, h * 64:(h + 1) * 64], rhs=mt[:, cs], start=True, stop=False),
                 reads=[xtkey, mtkey], writes=[pykey], accumulate=(h % 8 > 0))
            S.op("pe", lambda e: e.matmul(outap, lhsT=prev_b[:, h * 64:(h + 1) * 64], rhs=ce[:, cs], start=False, stop=True),
                 reads=["prev_b", cekey], writes=[pykey], accumulate=True)
        sm, smkey, bmt, bmkey, xtd, xdkey = c["sm"], c["smkey"], c["bmt"], c["bmkey"], c["xtd"], c["xdkey"]
        psts = [pw_ring.nxt(), pw_ring.nxt()]
        for g in range(2):
            pst, pstkey = psts[g]
            S.op("pe", lambda e: e.matmul(pst[:], lhsT=bmt[:, g * 128:(g + 1) * 128], rhs=xtd[:, g * 512:(g + 1) * 512],
                                          start=True, stop=True), reads=[bmkey, xdkey], writes=[pstkey])
        S.op("dve", lambda e: e.tensor_tensor(
            prev_f[:].rearrange("p (h d) -> p h d", d=64), prev_f[:].rearrange("p (h d) -> p h d", d=64),
            sm[:, 32:48].unsqueeze(2).to_broadcast([128, 16, 64]), op=ALU.mult),
            reads=["prev_f", smkey], writes=["prev_f"])
        for g in range(2):
            pst, pstkey = psts[g]
            S.op("dve", lambda e: e.tensor_tensor(prev_f[:, g * 512:(g + 1) * 512], prev_f[:, g * 512:(g + 1) * 512],
                                                  pst[:], op=ALU.add), reads=["prev_f", pstkey], writes=["prev_f"])
        S.op("act", lambda e: e.copy(prev_b[:], prev_f[:]), reads=["prev_f"], writes=["prev_b"])

    def E(c):
        xb, xkey, szt, skey, dtl, dkey, yo, yokey = get_tile(c["tt"])
        j = c["j"]
        js = slice(j * 128, (j + 1) * 128)
        pys, pmA, pmkey = c["pys"], c["pmA"], c["pmkey"]
        gv, gvkey = gv_ring.nxt()
        for fc in range(8):
            py, pykey = pys[fc // 4]
            S.op("dve", lambda e: e.scalar_tensor_tensor(
                out=gv[:, fc, :], in0=xb[:, fc, js], scalar=PC("sdd", fc), in1=py[:, (fc % 4) * 128:(fc % 4 + 1) * 128],
                op0=ALU.mult, op1=ALU.add), reads=[xkey, pykey, "pcol"], writes=[gvkey], accumulate=(fc > 0))
        S.op("pool", lambda e: e.tensor_tensor(gv[:], gv[:], szt[:, :, js], op=ALU.mult), reads=[gvkey, skey],
             writes=[gvkey])
        sq, sqkey = sq_ring.nxt()
        S.op("act", lambda e: e.activation(out=sq[:], in_=gv[:], func=AF.Square), reads=[gvkey], writes=[sqkey])
        c.update(gv=gv, gvkey=gvkey, sq=sq, sqkey=sqkey)

    def E2(c):
        xb, xkey, szt, skey, dtl, dkey, yo, yokey = get_tile(c["tt"])
        j = c["j"]
        js = slice(j * 128, (j + 1) * 128)
        pmA, pmkey, gv, gvkey, sq, sqkey = c["pmA"], c["pmkey"], c["gv"], c["gvkey"], c["sq"], c["sqkey"]
        for fc in range(8):
            S.op("pe", lambda e: e.matmul(pmA[:, 384:512], lhsT=CB("ones"), rhs=sq[:, fc, :], start=(fc == 0),
                                          stop=(fc == 7)), reads=[sqkey, "cb"], writes=[(pmkey, "ss")], accumulate=(fc > 0))
        lnb, lnkey = ln_ring.nxt()
        rs, rskey = rs_ring.nxt()
        S.op("act", lambda e: e.activation(out=lnb[:], in_=pmA[:, 384:512], func=AF.Ln, bias=EPS, scale=1.0 / 1024),
             reads=[(pmkey, "ss")], writes=[lnkey])
        S.op("act", lambda e: e.activation(out=rs[:], in_=lnb[:], func=AF.Exp, scale=-0.5), reads=[lnkey], writes=[rskey])
        S.op("dve", lambda e: e.tensor_tensor(gv[:], gv[:], rs[:].unsqueeze(1).to_broadcast([128, 8, 128]), op=ALU.mult),
             reads=[gvkey, rskey], writes=[gvkey])
        S.op("pool", lambda e: e.tensor_tensor(yo[:, :, js], gv[:], PC("snw", 0, 8).unsqueeze(2).to_broadcast([128, 8, 128]),
                                               op=ALU.mult), reads=[gvkey, "pcol"], writes=[yokey], accumulate=(j > 0))
        if j == 3:
            tok0 = c["tt"] * 512
            S.dma("sp", k.ycatT[0:1024, tok0:tok0 + 512].rearrange("(c p) t -> p c t", p=128), yo[:], reads=[yokey])

    chunks = [{"tt": tt, "j": j} for tt in range(NTT) for j in range(4)]
    get_tile(0)
    P(chunks[0])
    for ci, c in enumerate(chunks):
        if c["j"] == 0 and c["tt"] + 1 < NTT:
            get_tile(c["tt"] + 1)
        H(c)
        if ci > 0:
            E2(chunks[ci - 1])
        if ci + 1 < len(chunks):
            P(chunks[ci + 1])
        E(c)
    E2(chunks[-1])


def phase_c0(k, es):
    nc, S = k.nc, k.S
    sb = lambda n, s, d: es.enter_context(nc.sbuf_tensor(n + k.sfx, s, d))
    ps = lambda n, s, d: es.enter_context(nc.psum_tensor(n + k.sfx, s, d))
    CF, CB = k.CF, k.CB
    q_ring = Ring(sb, "qp", 2, [128, L], BF16)
    k_ring = Ring(sb, "kp", 2, [128, L], BF16)
    vb_ring = Ring(sb, "vb", 1, [128, 16, 1024], BF16)
    e_ring = Ring(sb, "ee", 3, [128, 512], F32)
    sp_ring = Ring(sb, "spp", 5, [128, 512], BF16)
    bt_ring = Ring(sb, "btt", 3, [128, 512], BF16)
    r_ring = Ring(sb, "rr", 3, [128, 4], F32)
    accs = [sb("accA", [128, 4, 64], F32), sb("accB", [128, 4, 64], F32)]
    osb_ring = Ring(sb, "osb", 2, [128, 4, 128], BF16)
    oT_ring = Ring(sb, "oT", 2, [128, 512], BF16)
    pz_ring = Ring(ps, "pz", 2, [128, 512], F32)
    py_ring = Ring(ps, "pyy", 3, [128, 512], F32)
    pov_ring = Ring(ps, "pov", 2, [128, 4, 65], F32)
    ptr_ring = Ring(ps, "ptc", 1, [128, 512], BF16)

    for b in range(2):
        vb, vkey = vb_ring.nxt()
        S.dma("sp", vb[:], k.vtok[b * L:(b + 1) * L, :].rearrange("(i p) d -> p i d", p=128), writes=[vkey])
        for hp in range(8):
            qp, qkey = q_ring.nxt()
            kp, kkey = k_ring.nxt()
            S.dma("sp", qp[:], k.qT[hp * 128:(hp + 1) * 128, b * L:(b + 1) * L], writes=[qkey])
            S.dma("sp", kp[:], k.kT[hp * 128:(hp + 1) * 128, b * L:(b + 1) * L], writes=[kkey])
            for g in range(4):
                items = []
                for i in range(4 * g + 4):
                    for hh in range(2):
                        items.append({"i": i, "hh": hh})
                osb, oskey = osb_ring.nxt()
                for hh in range(2):
                    S.op("pool", lambda e, hh=hh: e.memset(accs[hh][:], 0.0), writes=[("acc", hh)])

                def geom(it):
                    i = it["i"]
                    qlo = max(0, i - 4 * g)
                    n = (4 - qlo) * 128
                    t0 = (4 * g + qlo) * 128
                    po = it["hh"] * 64
                    return i, qlo, n, t0, po

                def zmm(it, pt, pkey):
                    i, qlo, n, t0, po = geom(it)
                    diag = i >= 4 * g
                    S.op("pe", lambda e: e.matmul(pt[:, 0:n], lhsT=kp[po:po + 64, i * 128:(i + 1) * 128],
                                                  rhs=qp[po:po + 64, t0:t0 + n], start=True, stop=False),
                         reads=[qkey, kkey], writes=[pkey])
                    if diag:
                        S.op("pe", lambda e: e.matmul(pt[:, 0:128], lhsT=CB("negbig"), rhs=CB("ge"), start=False,
                                                      stop=False), reads=["cb"], writes=[pkey], accumulate=True)

                def s1(it):
                    it["pz"], it["pzkey"] = pz_ring.nxt()
                    zmm(it, it["pz"], it["pzkey"])
                    return

                def s2(it):
                    i, qlo, n, t0, po = geom(it)
                    ee, ekey = e_ring.nxt()
                    sp, spkey = sp_ring.nxt()
                    it["sp"], it["spkey"] = sp, spkey
                    pz, pzkey = it["pz"], it["pzkey"]
                    S.op("act", lambda e: e.activation(out=ee[:, 0:n], in_=pz[:, 0:n], func=AF.Exp),
                         reads=[pzkey], writes=[ekey])
                    S.op("act", lambda e: e.activation(out=sp[:, 0:n], in_=ee[:, 0:n], func=AF.Ln, bias=1.0),
                         reads=[ekey], writes=[spkey])

                def s3(it):
                    i, qlo, n, t0, po = geom(it)
                    it["py"], it["pykey"] = py_ring.nxt()
                    zmm(it, it["py"], it["pykey"])
                    py, sp = it["py"], it["sp"]
                    S.op("pe", lambda e: e.matmul(py[:, 0:n], lhsT=CB("negu"), rhs=sp[:, 0:n], start=False, stop=True),
                         reads=[it["spkey"], "cb"], writes=[it["pykey"]], accumulate=True)

                def s4(it):
                    i, qlo, n, t0, po = geom(it)
                    bt, btkey = bt_ring.nxt()
                    it["bt"], it["btkey"] = bt, btkey
                    py = it["py"]
                    S.op("act", lambda e: e.activation(out=bt[:, 0:n], in_=py[:, 0:n], func=AF.Exp),
                         reads=[it["pykey"]], writes=[btkey])

                def s5(it):
                    i, qlo, n, t0, po = geom(it)
                    pov, povkey = pov_ring.nxt()
                    it["pov"], it["povkey"] = pov, povkey
                    bt, sp = it["bt"], it["sp"]
                    h = hp * 2 + it["hh"]
                    first = True
                    for qq in range(qlo, 4):
                        cs = slice((qq - qlo) * 128, (qq - qlo + 1) * 128)
                        S.op("pe", lambda e, qq=qq, cs=cs: e.matmul(pov[:, qq, 0:64], lhsT=bt[:, cs],
                                                                    rhs=vb[:, i, h * 64:(h + 1) * 64], start=True, stop=True),
                             reads=[it["btkey"], vkey], writes=[povkey], accumulate=(not first))
                        first = False
                        S.op("pe", lambda e, qq=qq, cs=cs: e.matmul(pov[:, qq, 64:65], lhsT=sp[:, cs], rhs=CB("ones")[:, 0:1],
                                                                    start=True, stop=True),
                             reads=[it["spkey"], "cb"], writes=[povkey], accumulate=True)

                def s6(it):
                    i, qlo, n, t0, po = geom(it)
                    rr, rkey = r_ring.nxt()
                    pov, povkey = it["pov"], it["povkey"]
                    hh = it["hh"]
                    acc = accs[hh]
                    nq = 4 - qlo
                    S.op("act", lambda e: e.activation(out=rr[:, qlo:4].unsqueeze(2), in_=pov[:, qlo:4, 64:65], func=AF.Exp,
                                                       scale=-1.0), reads=[povkey], writes=[rkey])
                    S.op("dve", lambda e: e.tensor_tensor(acc[:, qlo:4, :], acc[:, qlo:4, :],
                                                          rr[:, qlo:4].unsqueeze(2).to_broadcast([128, nq, 64]), op=ALU.mult),
                         reads=[rkey, ("acc", hh)], writes=[("acc", hh)])
                    S.op("dve", lambda e: e.tensor_tensor(acc[:, qlo:4, :], acc[:, qlo:4, :], pov[:, qlo:4, 0:64], op=ALU.add),
                         reads=[povkey, ("acc", hh)], writes=[("acc", hh)])

                stages = [s1, s2, s3, s4, s5, s6]
                lag = [0, 0, 1, 1, 2, 2]
                for n in range(len(items) + 2):
                    for st, lg in zip(stages, lag):
                        m = n - lg
                        if 0 <= m < len(items):
                            st(items[m])
                for hh in range(2):
                    S.op("pool", lambda e, hh=hh: e.tensor_copy(osb[:, :, hh * 64:(hh + 1) * 64], accs[hh][:]),
                         reads=[("acc", hh)], writes=[oskey], accumulate=(hh > 0))
                ptc, ptckey = ptr_ring.nxt()
                for qq in range(4):
                    S.op("pe", lambda e, qq=qq: e.transpose(out=ptc[:, qq * 128:(qq + 1) * 128], in_=osb[:, qq, :],
                                                            identity=CB("eye")),
                         reads=[oskey, "cb"], writes=[ptckey], accumulate=(qq > 0))
                oT, oTkey = oT_ring.nxt()
                S.op("dve", lambda e: e.tensor_copy(oT[:], ptc[:]), reads=[ptckey], writes=[oTkey])
                tok0 = b * L + g * 512
                S.dma("sp", k.ycatT[1024 + hp * 128:1024 + (hp + 1) * 128, tok0:tok0 + 512], oT[:], reads=[oTkey])


def phase_proj_res(k, es, w_dram, kc_n, srcT, h_in, h_out):
    nc, S = k.nc, k.S
    sb = lambda n, s, d: es.enter_context(nc.sbuf_tensor(n + k.sfx, s, d))
    ps = lambda n, s, d: es.enter_context(nc.psum_tensor(n + k.sfx, s, d))
    w = sb("wres", [128, kc_n, D], BF16)
    S.dma("pool", w[:], w_dram.rearrange("(c p) d -> p c d", p=128), writes=["wres"])
    src_ring = Ring(sb, "srct", 2, [128, kc_n, 512], BF16)
    h_ring = Ring(sb, "hres", 2, [128, 4, D], F32)
    po_ring = Ring(ps, "pres", 4, [128, 512], F32)

    def load(tt):
        src, skey = src_ring.nxt()
        ht, hkey = h_ring.nxt()
        S.dma("sp", src[:], srcT[:, tt * 512:(tt + 1) * 512].rearrange("(c p) t -> p c t", p=128), writes=[skey])
        S.dma("sp", ht[:], h_in[tt * 512:(tt + 1) * 512, :].rearrange("(j p) d -> p j d", p=128), writes=[hkey])
        return src, skey, ht, hkey

    nxt = load(0)
    for tt in range(NTT):
        src, skey, ht, hkey = nxt
        if tt + 1 < NTT:
            nxt = load(tt + 1)
        for j in range(4):
            for half in range(2):
                po, pkey = po_ring.nxt()
                for c in range(kc_n):
                    S.op("pe", lambda e: e.matmul(po[:], lhsT=src[:, c, j * 128:(j + 1) * 128],
                                                  rhs=w[:, c, half * 512:(half + 1) * 512], start=(c == 0),
                                                  stop=(c == kc_n - 1)),
                         reads=[skey, "wres"], writes=[pkey], accumulate=(c > 0))
                S.op("dve", lambda e: e.tensor_tensor(ht[:, j, half * 512:(half + 1) * 512],
                                                      ht[:, j, half * 512:(half + 1) * 512], po[:], op=ALU.add),
                     reads=[pkey, hkey], writes=[hkey])
        S.dma("sp", h_out[tt * 512:(tt + 1) * 512, :].rearrange("(j p) d -> p j d", p=128), ht[:], reads=[hkey])


def phase_ffn_up(k, es, layer, h_in, gT):
    nc, S = k.nc, k.S
    sb = lambda n, s, d: es.enter_context(nc.sbuf_tensor(n + k.sfx, s, d))
    ps = lambda n, s, d: es.enter_context(nc.psum_tensor(n + k.sfx, s, d))
    PC, PR = k.PC, k.PR
    wup = sb("wup", [128, 8, 2 * DFF], BF16)
    for kc in range(8):
        S.dma("pool", wup[:, kc, :], k.ffn_up_w[layer, kc * 128:(kc + 1) * 128, :], writes=[("wup", kc)])
    xt_ring = Ring(sb, "xtf", 2, [128, 4, D], F32)
    hn_ring = Ring(sb, "hnf", 1, [128, 4, D], BF16)
    hnT_ring = Ring(sb, "hnTf", 2, [128, 8, 512], BF16)
    ptr_ring = Ring(ps, "ptrf", 2, [128, 1024], BF16)
    pb_ring = Ring(ps, "pbf", 6, [128, 512], F32)
    junk = sb("junkf", [128, D], BF16)
    small = (sb("ssqf", [128, 4], F32), sb("lnvf", [128, 4], F32), sb("rstdf", [128, 4], F32))
    raw_ring = Ring(sb, "rawf", 6, [128, 514], F32)
    acc_ring = Ring(sb, "accf", 6, [128, 512], F32)
    sg_ring = Ring(sb, "sgf", 2, [128, 512], F32)
    ob_ring = Ring(sb, "obf", 3, [128, 512], BF16)
    halo = sb("halof", [128, 44, 2], F32)
    cwn, cbn, nwn = "fcw%d" % layer, "fcb%d" % layer, "nw%d1" % layer

    def load_x(tt):
        xt, xkey = xt_ring.nxt()
        S.dma("sp", xt[:], h_in[tt * 512:(tt + 1) * 512, :].rearrange("(j p) d -> p j d", p=128), writes=[xkey])
        return xt, xkey

    def prologue(xx):
        xt, xkey = xx
        hnT, hkey = hnT_ring.nxt()
        norm_transpose(k, S, xt, xkey, PR(nwn), hn_ring, hnT, hkey, ptr_ring, junk, small)
        return hnT, hkey

    xs = {0: load_x(0)}
    if NTT > 1:
        xs[1] = load_x(1)
    pro = {0: prologue(xs[0])}
    for tt in range(NTT):
        if tt + 2 < NTT:
            xs[tt + 2] = load_x(tt + 2)
        hnT, hkey = pro[tt]
        tok0 = tt * 512
        seq_start = (tt % 4 == 0)

        def st1(cc):
            pb, pkey = pb_ring.nxt()
            for kc in range(8):
                S.op("pe", lambda e: e.matmul(pb[:], lhsT=wup[:, kc, cc * 128:(cc + 1) * 128], rhs=hnT[:, kc, :],
                                              start=(kc == 0), stop=(kc == 7)),
                     reads=[hkey, ("wup", kc)], writes=[pkey], accumulate=(kc > 0))
            raw, rkey = raw_ring.nxt()
            acc, akey = acc_ring.nxt()
            if seq_start:
                S.op("pool", lambda e: e.memset(raw[:, 0:2], 0.0), writes=[(rkey, "h")])
            else:
                S.op("pool", lambda e: e.tensor_copy(raw[:, 0:2], halo[:, cc, :]), reads=[("halof", cc)],
                     writes=[(rkey, "h")])
            S.op("act", lambda e: e.copy(raw[:, 2:514], pb[:]), reads=[pkey], writes=[rkey])
            S.op("act", lambda e: e.activation(out=acc[:], in_=pb[:], func=AF.Identity, bias=PC(cbn, cc),
                                               scale=PC(cwn, cc * 3 + 2)), reads=[pkey, "pcol"], writes=[akey])
            return (raw, rkey, acc, akey, cc)

        def st2(ta, tb_):
            for tp in range(2):
                for (raw, rkey, acc, akey, cc) in (ta, tb_):
                    S.op("dve", lambda e: e.scalar_tensor_tensor(out=acc[:], in0=raw[:, tp:tp + 512],
                                                                 scalar=PC(cwn, cc * 3 + tp), in1=acc[:], op0=ALU.mult,
                                                                 op1=ALU.add), reads=[rkey, (rkey, "h"), akey, "pcol"],
                         writes=[akey])
            for (raw, rkey, acc, akey, cc) in (ta, tb_):
                S.op("pool", lambda e: e.tensor_copy(halo[:, cc, :], raw[:, 512:514]), reads=[rkey], writes=[("halof", cc)])

        def st3(tg, tv, c):
            ag, agkey = tg[2], tg[3]
            av, avkey = tv[2], tv[3]
            sg, sgkey = sg_ring.nxt()
            S.op("act", lambda e: e.activation(out=sg[:], in_=ag[:], func=AF.Silu), reads=[agkey], writes=[sgkey])
            ob, okey = ob_ring.nxt()
            S.op("pool", lambda e: e.tensor_tensor(ob[:], sg[:], av[:], op=ALU.mult), reads=[sgkey, avkey], writes=[okey])
            S.dma("sp", gT[c * 128:(c + 1) * 128, tok0:tok0 + 512], ob[:], reads=[okey])

        its = {}
        for n in range(22 + 2):
            if n == 11 and tt + 1 < NTT:
                pro[tt + 1] = prologue(xs[tt + 1])
            if n < 22:
                its[n] = (st1(n), st1(22 + n))
            if 0 <= n - 1 < 22:
                st2(its[n - 1][0], its[n - 1][1])
            if 0 <= n - 2 < 22:
                st3(its[n - 2][0], its[n - 2][1], n - 2)


def phase_e1(k, es):
    nc, S = k.nc, k.S
    sb = lambda n, s, d: es.enter_context(nc.sbuf_tensor(n + k.sfx, s, d))
    ps = lambda n, s, d: es.enter_context(nc.psum_tensor(n + k.sfx, s, d))
    PC, PR = k.PC, k.PR
    win = sb("winm", [128, 8, MLC], BF16)
    for kc in range(8):
        S.dma("pool", win[:, kc, :], k.ml_in_w[kc * 128:(kc + 1) * 128, :], writes=[("winm", kc)])
    xt_ring = Ring(sb, "xtm", 2, [128, 4, D], F32)
    hn_ring = Ring(sb, "hnm", 1, [128, 4, D], BF16)
    hnT_ring = Ring(sb, "hnTm", 2, [128, 8, 512], BF16)
    ptr_ring = Ring(ps, "ptrm", 2, [128, 1024], BF16)
    pb_ring = Ring(ps, "pbm", 5, [128, 512], F32)
    junk = sb("junkm", [128, D], BF16)
    small = (sb("ssqm", [128, 4], F32), sb("lnvm", [128, 4], F32), sb("rstdm", [128, 4], F32))
    ob_ring = Ring(sb, "obm", 4, [128, 512], BF16)
    gg = sb("ggm", [128, 4, 16], F32)
    th = sb("thm", [128, 4, 16], F32)
    ef = sb("efm", [128, 4, 8], F32)
    gt_ring = Ring(sb, "gtm", 2, [128, 4, 16], F32)

    def load_x(tt):
        xt, xkey = xt_ring.nxt()
        S.dma("sp", xt[:], k.h2[tt * 512:(tt + 1) * 512, :].rearrange("(j p) d -> p j d", p=128), writes=[xkey])
        return xt, xkey

    def prologue(xx):
        xt, xkey = xx
        hnT, hkey = hnT_ring.nxt()
        norm_transpose(k, S, xt, xkey, PR("nw10"), hn_ring, hnT, hkey, ptr_ring, junk, small)
        return hnT, hkey

    xs = {0: load_x(0)}
    if NTT > 1:
        xs[1] = load_x(1)
    pro = {0: prologue(xs[0])}
    for tt in range(NTT):
        if tt + 2 < NTT:
            xs[tt + 2] = load_x(tt + 2)
        hnT, hkey = pro[tt]
        tok0 = tt * 512

        def proj_fm(col0):
            pb, pkey = pb_ring.nxt()
            for kc in range(8):
                S.op("pe", lambda e: e.matmul(pb[:], lhsT=win[:, kc, col0:col0 + 128], rhs=hnT[:, kc, :],
                                              start=(kc == 0), stop=(kc == 7)),
                     reads=[hkey, ("winm", kc)], writes=[pkey], accumulate=(kc > 0))
            return pb, pkey

        def proj_tm(j, col0, n):
            pb, pkey = pb_ring.nxt()
            for kc in range(8):
                S.op("pe", lambda e: e.matmul(pb[:, 0:n], lhsT=hnT[:, kc, j * 128:(j + 1) * 128],
                                              rhs=win[:, kc, col0:col0 + n], start=(kc == 0), stop=(kc == 7)),
                     reads=[hkey, ("winm", kc)], writes=[pkey], accumulate=(kc > 0))
            return pb, pkey

        for c in range(4):
            pb, pkey = proj_fm(c * 128)
            ob, okey = ob_ring.nxt()
            S.op("act", lambda e: e.mul(ob[:], pb[:], 0.125), reads=[pkey], writes=[okey])
            S.dma("sp", k.mqT[c * 128:(c + 1) * 128, tok0:tok0 + 512], ob[:], reads=[okey])
        for c in range(4):
            pb, pkey = proj_fm(512 + c * 128)
            ob, okey = ob_ring.nxt()
            S.op("dve", lambda e: e.tensor_copy(ob[:], pb[:]), reads=[pkey], writes=[okey])
            S.dma("sp", k.mkT[c * 128:(c + 1) * 128, tok0:tok0 + 512], ob[:], reads=[okey])
        for c in range(8):
            pb, pkey = proj_fm(2048 + c * 128)
            ob, okey = ob_ring.nxt()
            S.op("act", lambda e: e.activation(out=ob[:], in_=pb[:], func=AF.Sigmoid), reads=[pkey], writes=[okey])
            S.dma("sp", k.soT[c * 128:(c + 1) * 128, tok0:tok0 + 512], ob[:], reads=[okey])
        if tt + 1 < NTT:
            pro[tt + 1] = prologue(xs[tt + 1])
        for j in range(4):
            for part in range(3):
                col0 = 512 if part == 0 else 1024 + (part - 1) * 512
                pb, pkey = proj_tm(j, col0, 512)
                ob, okey = ob_ring.nxt()
                if part == 1:
                    S.op("act", lambda e: e.copy(ob[:], pb[:]), reads=[pkey], writes=[okey])
                else:
                    S.op("dve", lambda e: e.tensor_copy(ob[:], pb[:]), reads=[pkey], writes=[okey])
                if part == 0:
                    S.dma("sp", k.mkt[tok0 + j * 128:tok0 + (j + 1) * 128, :], ob[:], reads=[okey])
                else:
                    S.dma("sp", k.mvt[tok0 + j * 128:tok0 + (j + 1) * 128, (part - 1) * 512:part * 512], ob[:],
                          reads=[okey])
        pb, pkey = pb_ring.nxt()
        for j in range(4):
            for kc in range(8):
                S.op("pe", lambda e: e.matmul(pb[:, j * 16:(j + 1) * 16], lhsT=hnT[:, kc, j * 128:(j + 1) * 128],
                                              rhs=win[:, kc, 3072:3088], start=(kc == 0), stop=(kc == 7)),
                     reads=[hkey, ("winm", kc)], writes=[pkey], accumulate=(j + kc > 0))
        gt, gtkey = gt_ring.nxt()
        r0 = PROW["mib"][0]
        S.op("dve", lambda e: e.tensor_tensor(gg[:], pb[:, 0:64].rearrange("p (j h) -> p j h", h=16),
                                              k.prow[:, r0:r0 + 16].unsqueeze(1).to_broadcast([128, 4, 16]), op=ALU.add),
             reads=[pkey, "prow"], writes=["ggm"])
        S.op("act", lambda e: e.activation(out=th[:], in_=gg[:], func=AF.Tanh, scale=1.0 / 15.0), reads=["ggm"],
             writes=["thm"])
        S.op("act", lambda e: e.mul(gt[:, :, 0:8], th[:, :, 0:8], 15.0), reads=["thm"], writes=[gtkey])
        S.op("act", lambda e: e.activation(out=ef[:], in_=th[:, :, 8:16], func=AF.Exp, scale=-15.0), reads=["thm"],
             writes=["efm"])
        S.op("act", lambda e: e.activation(out=ef[:], in_=ef[:], func=AF.Ln, bias=1.0), reads=["efm"], writes=["efm"])
        S.op("act", lambda e: e.mul(gt[:, :, 8:16], ef[:], -1.0), reads=["efm"], writes=[gtkey], accumulate=True)
        S.dma("sp", k.gts[tok0:tok0 + 512, :].rearrange("(j p) h -> p j h", p=128), gt[:], reads=[gtkey])


def phase_f1(k, es):
    nc, S = k.nc, k.S
    sb = lambda n, s, d: es.enter_context(nc.sbuf_tensor(n + k.sfx, s, d))
    ps = lambda n, s, d: es.enter_context(nc.psum_tensor(n + k.sfx, s, d))
    CF, CB, PC = k.CF, k.CB, k.PC
    q_ring = Ring(sb, "mq", 2, [128, 4, 512], BF16)
    k_ring = Ring(sb, "mk", 2, [128, 4, 512], BF16)
    kt_ring = Ring(sb, "mkt", 2, [128, 4, 512], BF16)
    vt_ring = Ring(sb, "mvt", 2, [128, 4, 1024], BF16)
    so_ring = Ring(sb, "mso", 2, [128, 8, 512], BF16)
    gl_ring = Ring(sb, "mgl", 2, [128, 4, 16], F32)
    ho_ring = Ring(sb, "mho", 2, [128, 8, 512], BF16)
    g4 = sb("mg4", [128, 512], F32)
    sm_ring = Ring(sb, "msm", 3, [128, 32], F32)
    cdp_ring = Ring(sb, "mcdp", 3, [128, 4], F32)
    dt_ring = Ring(sb, "mdt", 3, [128, 512], F32)
    eb_ring = Ring(sb, "meb", 3, [128, 512], F32)
    st_ring = Ring(sb, "mst", 3, [128, 512], BF16)
    qe_ring = Ring(sb, "mqe", 3, [128, 512], BF16)
    ad_ring = Ring(sb, "mad", 3, [128, 512], F32)
    hT_ring = Ring(sb, "mhT", 3, [128, 512], F32)
    sq_ring = Ring(sb, "msq", 3, [128, 512], BF16)
    ln_ring = Ring(sb, "mln", 3, [128, 512], F32)
    rs_ring = Ring(sb, "mrs", 3, [128, 512], F32)
    kd_ring = Ring(sb, "mkd", 3, [128, 8, 64], BF16)
    Cf = sb("mCf", [128, 4, 128], F32)
    Cb_ = sb("mCb", [128, 4, 128], BF16)
    nf = sb("mnf", [128, 4], F32)
    nb = sb("mnb", [128, 4], BF16)
    pG_ring = Ring(ps, "mpG", 1, [128, 64], F32)
    pD_ring = Ring(ps, "mpD", 2, [128, 512], F32)
    pkq_ring = Ring(ps, "mpkq", 1, [128, 512], F32)
    pN_ring = Ring(ps, "mpN", 1, [128, 512], F32)
    pden_ring = Ring(ps, "mpden", 1, [128, 512], F32)
    pss_ring = Ring(ps, "mpss", 1, [128, 512], F32)
    pC_ring = Ring(ps, "mpC", 1, [128, 4, 128], F32)

    for q in range(4):
        S.op("pool", lambda e: e.tensor_copy(g4[:, q * 128:(q + 1) * 128], CF("gt")), reads=["cf"], writes=["mg4"],
             accumulate=(q > 0))

    def load(tt):
        tok0 = tt * 512
        r = {}
        for name, ring, src in (("q", q_ring, k.mqT), ("k", k_ring, k.mkT), ("so", so_ring, k.soT)):
            t, key = ring.nxt()
            S.dma("sp", t[:], src[:, tok0:tok0 + 512].rearrange("(c p) t -> p c t", p=128), writes=[key])
            r[name] = (t, key)
        for name, ring, src in (("kt", kt_ring, k.mkt), ("vt", vt_ring, k.mvt), ("gl", gl_ring, k.gts)):
            t, key = ring.nxt()
            S.dma("sp", t[:], src[tok0:tok0 + 512, :].rearrange("(j p) d -> p j d", p=128), writes=[key])
            r[name] = (t, key)
        r["ho"] = ho_ring.nxt()
        return r

    tiles = {}

    def get_tile(tt):
        if tt not in tiles:
            tiles[tt] = load(tt)
        return tiles[tt]

    def P(c):
        cur = get_tile(c["tt"])
        j = c["j"]
        (gl, glkey), (kt, ktkey) = cur["gl"], cur["kt"]
        logi = gl[:, j, 0:8]
        logf = gl[:, j, 8:16]
        pG, pGkey = pG_ring.nxt()
        c["pG"], c["pGkey"] = pG, pGkey
        for i, cname in enumerate(("le", "gt", "ones")):
            S.op("pe", lambda e: e.matmul(pG[:, i * 8:(i + 1) * 8], lhsT=CF(cname), rhs=logf, start=True, stop=True),
                 reads=[glkey, "cf"], writes=[(pGkey, "g")], accumulate=(i > 0))
        sm, smkey = sm_ring.nxt()
        cdp, cdpkey = cdp_ring.nxt()
        c["sm"], c["smkey"], c["cdp"], c["cdpkey"] = sm, smkey, cdp, cdpkey
        S.op("dve", lambda e: e.tensor_tensor(sm[:, 0:8], logi, pG[:, 0:8], op=ALU.subtract),
             reads=[glkey, (pGkey, "g")], writes=[smkey])
        S.op("dve", lambda e: e.tensor_tensor(sm[:, 24:32], logi, pG[:, 8:16], op=ALU.add),
             reads=[glkey, (pGkey, "g")], writes=[smkey], accumulate=True)
        S.op("act", lambda e: e.activation(out=sm[:, 8:16], in_=sm[:, 24:32], func=AF.Exp), reads=[smkey], writes=[smkey])
        S.op("act", lambda e: e.activation(out=cdp[0:64, :], in_=pG[0:64, 16:24:2], func=AF.Exp),
             reads=[(pGkey, "g")], writes=[cdpkey])
        S.op("act", lambda e: e.activation(out=cdp[64:128, :], in_=pG[64:128, 17:24:2], func=AF.Exp),
             reads=[(pGkey, "g")], writes=[cdpkey], accumulate=True)
        kd, kdkey = kd_ring.nxt()
        c["kd"], c["kdkey"] = kd, kdkey
        S.op("pool", lambda e: e.tensor_tensor(kd[:], kt[:, j, :].rearrange("p (h d) -> p h d", d=64),
                                               sm[:, 8:16].unsqueeze(2).to_broadcast([128, 8, 64]), op=ALU.mult),
             reads=[ktkey, smkey], writes=[kdkey])

    def H(c):
        cur = get_tile(c["tt"])
        j = c["j"]
        js = slice(j * 128, (j + 1) * 128)
        (mq, qkey), (mk, kkey), (so, sokey) = cur["q"], cur["k"], cur["so"]
        (kt, ktkey), (vt, vtkey), (gl, glkey) = cur["kt"], cur["vt"], cur["gl"]
        ho, hokey = cur["ho"]
        sm, smkey, kd, kdkey, pG, pGkey = c["sm"], c["smkey"], c["kd"], c["kdkey"], c["pG"], c["pGkey"]
        if c["tt"] % 4 == 0 and j == 0:
            S.op("pool", lambda e: e.memset(Cf[:], 0.0), writes=["mCf"])
            S.op("pool", lambda e: e.memset(Cb_[:], 0.0), writes=["mCb"])
            S.op("pool", lambda e: e.memset(nf[:], 0.0), writes=["mnf"])
            S.op("pool", lambda e: e.memset(nb[:], 0.0), writes=["mnb"])
        pC, pCkey = pC_ring.nxt()
        c["pC"], c["pCkey"] = pC, pCkey
        CS = lambda hh: slice(hh * 128, (hh + 1) * 128)

        def front(hq):
            pDm, pDmkey = pD_ring.nxt()
            pDu, pDukey = pD_ring.nxt()
            pkq, pkqkey = pkq_ring.nxt()
            pN, pNkey = pN_ring.nxt()
            pden, pdenkey = pden_ring.nxt()
            hs = [hq * 4 + hh for hh in range(4)]
            S.op("pe", lambda e: e.matmul(pDm[:], lhsT=CF("negbig"), rhs=g4[:], start=True, stop=False),
                 reads=["cf", "mg4"], writes=[pDmkey])
            for hh, h in enumerate(hs):
                po, pr = (h % 2) * 64, h // 2
                lb = gl[:, j, 8 + h:9 + h].to_broadcast([128, 128])
                S.op("pe", lambda e: e.matmul(pDm[:, CS(hh)], lhsT=lb, rhs=CF("le"), start=False, stop=True),
                     reads=[glkey, "cf"], writes=[pDmkey], accumulate=True)
                S.op("pe", lambda e: e.matmul(pDu[:, CS(hh)], lhsT=lb, rhs=CF("le"), start=True, stop=True),
                     reads=[glkey, "cf"], writes=[pDukey], accumulate=(hh > 0))
                S.op("pe", lambda e: e.matmul(pkq[:, CS(hh)], lhsT=mk[po:po + 64, pr, js], rhs=mq[po:po + 64, pr, js],
                                              start=True, stop=True),
                     reads=[qkey, kkey], writes=[pkqkey], accumulate=(hh > 0))
            eb, ebkey = eb_ring.nxt()
            S.op("act", lambda e: e.activation(out=eb[:], in_=pDu[:], func=AF.Exp), reads=[pDukey], writes=[ebkey])
            dtl, dtkey = dt_ring.nxt()
            for hh, h in enumerate(hs):
                S.op("act", lambda e: e.activation(out=dtl[:, CS(hh)], in_=pDm[:, CS(hh)], func=AF.Exp,
                                                   bias=sm[:, h:h + 1]),
                     reads=[pDmkey, smkey], writes=[dtkey], accumulate=(hh > 0))
            st, stkey = st_ring.nxt()
            S.op("dve", lambda e: e.tensor_tensor(st[:], dtl[:], pkq[:], op=ALU.mult), reads=[dtkey, pkqkey],
                 writes=[stkey])
            qe, qekey = qe_ring.nxt()
            for hh, h in enumerate(hs):
                po, pr = (h % 2) * 64, h // 2
                S.op("dve", lambda e: e.tensor_tensor(qe[po:po + 64, CS(hh)], mq[po:po + 64, pr, js],
                                                      eb[po:po + 64, CS(hh)], op=ALU.mult),
                     reads=[qkey, ebkey], writes=[qekey], accumulate=(hh > 0))
            for hh, h in enumerate(hs):
                po, pr = (h % 2) * 64, h // 2
                S.op("pe", lambda e: e.matmul(pN[:, CS(hh)], lhsT=vt[:, j, h * 128:(h + 1) * 128], rhs=st[:, CS(hh)],
                                              start=True, stop=False), reads=[vtkey, stkey], writes=[pNkey],
                     accumulate=(hh > 0))
                S.op("pe", lambda e: e.matmul(pN[:, CS(hh)], lhsT=Cb_[po:po + 64, pr, :], rhs=qe[po:po + 64, CS(hh)],
                                              start=False, stop=True), reads=["mCb", qekey], writes=[pNkey],
                     accumulate=True)
                S.op("pe", lambda e: e.matmul(pden[:, CS(hh)], lhsT=CB("ones"), rhs=st[:, CS(hh)], start=True, stop=False),
                     reads=["cb", stkey], writes=[pdenkey], accumulate=(hh > 0))
                S.op("pe", lambda e: e.matmul(pden[:, CS(hh)], lhsT=nb[po:po + 64, pr:pr + 1].to_broadcast([64, 128]),
                                              rhs=qe[po:po + 64, CS(hh)], start=False, stop=True),
                     reads=["mnb", qekey], writes=[pdenkey], accumulate=True)
            for hh, h in enumerate(hs):
                po, pr = (h % 2) * 64, h // 2
                S.op("pe", lambda e: e.matmul(pC[po:po + 64, pr, :], lhsT=kd[:, h, :], rhs=vt[:, j, h * 128:(h + 1) * 128],
                                              start=True, stop=True), reads=[kdkey, vtkey], writes=[pCkey],
                     accumulate=(h > 0))
                S.op("pe", lambda e: e.matmul(pG[po:po + 64, 32 + pr:33 + pr], lhsT=kd[:, h, :], rhs=CB("ones")[:, 0:1],
                                              start=True, stop=True), reads=[kdkey, "cb"], writes=[(pGkey, "n")],
                     accumulate=(h > 0))
            return {"hq": hq, "hs": hs, "pN": pN, "pNkey": pNkey, "pden": pden, "pdenkey": pdenkey}

        def tailA(g):
            pN, pNkey, pden, pdenkey = g["pN"], g["pNkey"], g["pden"], g["pdenkey"]
            ad, adkey = ad_ring.nxt()
            S.op("act", lambda e: e.activation(out=ad[:], in_=pden[:], func=AF.Abs), reads=[pdenkey], writes=[adkey])
            S.op("dve", lambda e: e.tensor_scalar(ad[:], ad[:], 1.0, None, op0=ALU.max), reads=[adkey], writes=[adkey])
            S.op("dve", lambda e: e.reciprocal(ad[:], ad[:]), reads=[adkey], writes=[adkey])
            hT, hTkey = hT_ring.nxt()
            S.op("dve", lambda e: e.tensor_tensor(hT[:], pN[:], ad[:], op=ALU.mult), reads=[pNkey, adkey], writes=[hTkey])
            sq, sqkey = sq_ring.nxt()
            S.op("act", lambda e: e.activation(out=sq[:], in_=hT[:], func=AF.Square), reads=[hTkey], writes=[sqkey])
            g.update(hT=hT, hTkey=hTkey, sq=sq, sqkey=sqkey)

        def tailB(g):
            hq, hs, hT, hTkey, sq, sqkey = g["hq"], g["hs"], g["hT"], g["hTkey"], g["sq"], g["sqkey"]
            pss, psskey = pss_ring.nxt()
            S.op("pe", lambda e: e.matmul(pss[:], lhsT=CB("ones"), rhs=sq[:], start=True, stop=True),
                 reads=["cb", sqkey], writes=[psskey])
            lnb, lnkey = ln_ring.nxt()
            rs, rskey = rs_ring.nxt()
            S.op("act", lambda e: e.activation(out=lnb[:], in_=pss[:], func=AF.Ln, bias=EPS, scale=1.0 / 128),
                 reads=[psskey], writes=[lnkey])
            S.op("act", lambda e: e.activation(out=rs[:], in_=lnb[:], func=AF.Exp, scale=-0.5), reads=[lnkey],
                 writes=[rskey])
            for hh, h in enumerate(hs):
                S.op("dve", lambda e: e.scalar_tensor_tensor(out=hT[:, CS(hh)], in0=hT[:, CS(hh)], scalar=PC("mnw", h),
                                                             in1=rs[:, CS(hh)], op0=ALU.mult, op1=ALU.mult),
                     reads=[hTkey, rskey, "pcol"], writes=[hTkey])
            S.op("pool", lambda e: e.tensor_tensor(ho[:, hq * 4:(hq + 1) * 4, js], hT[:].rearrange("p (h t) -> p h t", t=128),
                                                   so[:, hq * 4:(hq + 1) * 4, js], op=ALU.mult),
                 reads=[hTkey, sokey], writes=[hokey], accumulate=(j + hq > 0))

        g0 = front(0)
        tailA(g0)
        g1 = front(1)
        tailB(g0)
        tailA(g1)
        tailB(g1)

    def E(c):
        cdp, cdpkey, pC, pCkey, pG, pGkey = c["cdp"], c["cdpkey"], c["pC"], c["pCkey"], c["pG"], c["pGkey"]
        cdb = cdp[:].unsqueeze(2).to_broadcast([128, 4, 128])
        S.op("dve", lambda e: e.tensor_tensor(Cf[:], Cf[:], cdb, op=ALU.mult), reads=["mCf", cdpkey], writes=["mCf"])
        S.op("dve", lambda e: e.tensor_tensor(Cf[:], Cf[:], pC[:], op=ALU.add), reads=["mCf", pCkey], writes=["mCf"])
        S.op("act", lambda e: e.copy(Cb_[:], Cf[:]), reads=["mCf"], writes=["mCb"])
        S.op("dve", lambda e: e.tensor_tensor(nf[:], nf[:], cdp[:], op=ALU.mult), reads=["mnf", cdpkey], writes=["mnf"])
        S.op("dve", lambda e: e.tensor_tensor(nf[:], nf[:], pG[:, 32:36], op=ALU.add), reads=["mnf", (pGkey, "n")],
             writes=["mnf"])
        S.op("act", lambda e: e.copy(nb[:], nf[:]), reads=["mnf"], writes=["mnb"])
        if c["j"] == 3:
            tok0 = c["tt"] * 512
            ho, hokey = get_tile(c["tt"])["ho"]
            S.dma("sp", k.hmT[:, tok0:tok0 + 512].rearrange("(c p) t -> p c t", p=128), ho[:], reads=[hokey])

    chunks = [{"tt": tt, "j": j} for tt in range(NTT) for j in range(4)]
    get_tile(0)
    P(chunks[0])
    for ci, c in enumerate(chunks):
        if c["j"] == 0 and c["tt"] + 1 < NTT:
            get_tile(c["tt"] + 1)
        H(c)
        if ci + 1 < len(chunks):
            P(chunks[ci + 1])
        E(c)


_NC_CACHE = {}


def kernel(**inputs):
    inp = {k_: np.asarray(v) for k_, v in inputs.items()}
    if "nc" not in _NC_CACHE:
        _NC_CACHE["nc"] = build()
    nc = _NC_CACHE["nc"]
    pc, pr = pack_params(inp)
    consts = make_consts()
    x = np.ascontiguousarray(inp["x"], dtype=np.float32)
    shared = {
        "pcol": pc, "prow": pr, "consts": consts,
        "hy_in_w": np.ascontiguousarray(inp["hy_in_w"][0], dtype=np.float32),
        "hy_out_w": np.ascontiguousarray(inp["hy_out_w"][0], dtype=np.float32),
        "ml_in_w": np.ascontiguousarray(inp["ml_in_w"][0], dtype=np.float32),
        "ml_out_w": np.ascontiguousarray(inp["ml_out_w"][0], dtype=np.float32),
        "ffn_up_w": np.ascontiguousarray(inp["ffn_up_w"], dtype=np.float32),
        "ffn_down_w": np.ascontiguousarray(inp["ffn_down_w"], dtype=np.float32),
    }
    in_maps = []
    for c in range(NCORES):
        m = dict(shared)
        m["x"] = np.ascontiguousarray(x[2 * c:2 * c + 2].reshape(NTOK, D))
        in_maps.append(m)
    res = run_bass_kernel_spmd(nc, in_maps, core_ids=list(range(NCORES)))
    out = np.concatenate([np.asarray(r["out"]).reshape(2, L, D) for r in res.results], axis=0)
    return out.astype(np.float32)
```

```python
import numpy as np
from contextlib import ExitStack
import concourse.bass as bass
import concourse.mybir as mybir
from concourse.bass_utils import run_bass_kernel_spmd

F32 = mybir.dt.float32
BF16 = mybir.dt.bfloat16
AF = mybir.ActivationFunctionType
ALU = mybir.AluOpType

NCORES = 8
NTOK = 4096
L = 2048
D = 1024
EPS = 1e-6
HYC = 5648
MLC = 3088
DFF = 2816
NTT = 8
DBGPRINT = False
OPLIMIT = None

ENGS = ("pe", "act", "dve", "pool", "sp")
NDSEM = 8


class Ins:
    __slots__ = ("eng", "fn", "deps", "is_dma", "needed", "ticket", "dslot", "dval", "waits", "q_n", "seq")

    def __init__(self, eng, fn, is_dma):
        self.eng = eng
        self.fn = fn
        self.is_dma = is_dma
        self.deps = []
        self.needed = False
        self.ticket = 0
        self.waits = []


class _Rec:
    def __getattr__(self, name):
        def f(*a, **kw):
            self.call = (name, a, kw)
        return f


class Sched:
    def __init__(self, nc, es):
        self.nc = nc
        self.sems = {}
        for e in ENGS:
            self.sems[("c", e)] = es.enter_context(nc.semaphore("c_" + e))
            for s in range(NDSEM):
                self.sems[("d", e, s)] = es.enter_context(nc.semaphore("d_%s_%d" % (e, s)))
        self.cnt = {e: 0 for e in ENGS}
        self.ndma = {e: 0 for e in ENGS}
        self.dma_hist = {e: [] for e in ENGS}
        self.hw = {e: {} for e in ENGS}
        self.nseq = 0
        self.limit = OPLIMIT
        self._reset()

    def _reset(self):
        self.streams = {e: [] for e in ENGS}
        self.all = []
        self.state = {}

    def _rec(self, ins, reads, writes, accumulate=False):
        if self.limit is not None and self.nseq >= self.limit:
            ins.deps = []
            ins.seq = self.nseq
            self.nseq += 1
            return ins
        deps = []
        for k in reads:
            st = self.state.get(k)
            if st:
                deps.extend(st[0])
        for k in writes:
            st = self.state.get(k)
            if st:
                deps.extend(st[1])
                deps.extend(st[0])
        out = []
        seen = set()
        for d in deps:
            if d is ins or id(d) in seen:
                continue
            seen.add(id(d))
            if (not d.is_dma) and (not ins.is_dma) and d.eng == ins.eng == "pe":
                continue
            out.append(d)
        last = {}
        red = []
        for d in out:
            if d.is_dma:
                red.append(d)
            elif d.eng not in last or d.seq > last[d.eng].seq:
                last[d.eng] = d
        red.extend(last.values())
        ins.deps = red
        ins.seq = self.nseq
        self.nseq += 1
        for k in reads:
            st = self.state.setdefault(k, [[], []])
            st[1].append(ins)
        for k in writes:
            st = self.state.setdefault(k, [[], []])
            if accumulate:
                st[0].append(ins)
            else:
                st[0] = [ins]
            st[1] = []
        self.all.append(ins)
        self.streams[ins.eng].append(ins)
        return ins

    def op(self, eng, fn, reads=(), writes=(), accumulate=False):
        rec = _Rec()
        fn(rec)
        name, a, kw = rec.call
        real = (lambda e, name=name, a=a, kw=kw: getattr(e, name)(*a, **kw))
        return self._rec(Ins(eng, real, False), list(reads), list(writes), accumulate)

    def dma(self, q, out, in_, reads=(), writes=()):
        ins = Ins(q, (lambda e, out=out, in_=in_: e.dma_start(out=out, in_=in_)), True)
        n = self.ndma[q]
        self.ndma[q] = n + 1
        ins.q_n = n
        ins.dslot = n % NDSEM
        ins.dval = 16 * (n // NDSEM + 1)
        hist = self.dma_hist[q]
        if self.limit is not None and self.nseq >= self.limit:
            self.ndma[q] = n
            self.nseq += 1
            return ins
        self._rec(ins, list(reads), list(writes))
        if n >= NDSEM:
            ins.deps.append(hist[n - NDSEM])
        hist.append(ins)
        return ins

    def flush(self):
        nc = self.nc
        for ins in self.all:
            for d in ins.deps:
                d.needed = True
        tails = []
        for e in ENGS:
            for ins in reversed(self.streams[e]):
                if not ins.is_dma:
                    ins.needed = True
                    tails.append(ins)
                    break
            hist = self.dma_hist[e]
            for ins in hist[max(0, len(hist) - NDSEM):]:
                tails.append(ins)
        for ins in self.all:
            if not ins.is_dma and ins.needed:
                self.cnt[ins.eng] += 1
                ins.ticket = self.cnt[ins.eng]
        hw = self.hw

        def key_val(d):
            if d.is_dma:
                return ("d", d.eng, d.dslot), d.dval
            return ("c", d.eng), d.ticket

        for ins in self.all:
            for d in ins.deps:
                key, val = key_val(d)
                if hw[ins.eng].get(key, 0) >= val:
                    continue
                hw[ins.eng][key] = val
                ins.waits.append((key, val))
        final = {e: [] for e in ENGS}
        for e in ENGS:
            for d in tails:
                key, val = key_val(d)
                if key == ("c", e) or hw[e].get(key, 0) >= val:
                    continue
                hw[e][key] = val
                final[e].append((key, val))
        sems = self.sems
        streams = self.streams

        def mk(ename):
            def body(eng):
                for ins in streams[ename]:
                    for key, val in ins.waits:
                        eng.wait_ge(sems[key], val)
                    r = ins.fn(eng)
                    if ins.is_dma:
                        r.then_inc(sems[("d", ename, ins.dslot)], 16)
                    elif ins.needed:
                        r.then_inc(sems[("c", ename)], 1)
                for key, val in final[ename]:
                    eng.wait_ge(sems[key], val)
            return body

        with nc.Block() as block:
            block.tensor(mk("pe"))
            block.scalar(mk("act"))
            block.vector(mk("dve"))
            block.gpsimd(mk("pool"))
            block.sync(mk("sp"))
        self._reset()


class Ring:
    def __init__(self, alloc, name, n, shape, dtype):
        self.tiles = [alloc("%s%d" % (name, i), shape, dtype) for i in range(n)]
        self.name = name
        self.i = 0

    def nxt(self):
        i = self.i % len(self.tiles)
        self.i += 1
        return self.tiles[i], (self.name, i)


def _col(vec):
    v = np.asarray(vec, np.float32).reshape(-1, 128)
    return np.ascontiguousarray(v.T)


PCOL = {}
PROW = {}


def _layout_tables():
    c = 0
    for name, n in (("scw", 48), ("scb", 12), ("sdd", 8), ("snw", 8), ("qw", 1), ("kw", 1),
                    ("fcw0", 132), ("fcb0", 44), ("fcw1", 132), ("fcb1", 44), ("mnw", 8)):
        PCOL[name] = (c, n)
        c += n
    PCOL["_n"] = c
    r = 0
    for name, n in (("nw00", 1024), ("nw01", 1024), ("nw10", 1024), ("nw11", 1024),
                    ("dtb", 16), ("alog", 16), ("mib", 8), ("mfb", 8)):
        PROW[name] = (r, n)
        r += n
    PROW["_n"] = r


_layout_tables()


def pack_params(inp):
    pc = np.zeros((128, PCOL["_n"]), np.float32)

    def put(name, arr):
        c0, n = PCOL[name]
        assert arr.shape == (128, n), (name, arr.shape)
        pc[:, c0:c0 + n] = arr

    cw = inp["ssd_conv_w"][0]
    put("scw", np.stack([_col(cw[k]) for k in range(4)], axis=2).reshape(128, 48))
    put("scb", _col(inp["ssd_conv_b"][0]))
    put("sdd", _col(np.repeat(inp["ssd_d"][0], 64)))
    put("snw", _col(inp["ssd_norm_w"][0]))
    put("qw", _col(np.tile(inp["sb_q_norm_w"][0], 2)))
    put("kw", _col(np.tile(inp["sb_k_norm_w"][0], 2)))
    for l in range(2):
        fw = inp["ffn_conv_w"][l]
        put("fcw%d" % l, np.stack([_col(fw[k]) for k in range(3)], axis=2).reshape(128, 132))
        put("fcb%d" % l, _col(inp["ffn_conv_b"][l]))
    put("mnw", _col(inp["ml_norm_w"][0]))
    pr = np.zeros((PROW["_n"],), np.float32)

    def putr(name, v):
        r0, n = PROW[name]
        pr[r0:r0 + n] = np.asarray(v, np.float32).reshape(n)

    putr("nw00", inp["norm_w"][0, 0])
    putr("nw01", inp["norm_w"][0, 1])
    putr("nw10", inp["norm_w"][1, 0])
    putr("nw11", inp["norm_w"][1, 1])
    putr("dtb", inp["ssd_dt_bias"][0])
    putr("alog", inp["ssd_a_log"][0])
    putr("mib", inp["ml_i_b"][0])
    putr("mfb", inp["ml_f_b"][0])
    prow = np.ascontiguousarray(np.broadcast_to(pr[None, :], (128, pr.size)))
    return pc, prow


def make_consts():
    i = np.arange(128)
    eye = (i[:, None] == i[None, :]).astype(np.float32)
    le = (i[:, None] <= i[None, :]).astype(np.float32)
    gt = (i[:, None] > i[None, :]).astype(np.float32)
    ge = (i[:, None] >= i[None, :]).astype(np.float32)
    ones = np.ones((128, 128), np.float32)
    bd = ((i[:, None] // 64) == (i[None, :] // 64)).astype(np.float32)
    negbig = -30000.0 * eye
    negu = -ge
    return np.ascontiguousarray(np.concatenate([eye, le, gt, ge, ones, bd, negbig, negu], axis=1))


CI = {"eye": 0, "le": 1, "gt": 2, "ge": 3, "ones": 4, "bd": 5, "negbig": 6, "negu": 7}


class K:
    pass


def build(nphase=99, dbg=False):
    nc = bass.Bass("TRN2", target_bir_lowering=False)
    k = K()
    k.nc = nc
    k.sfx = ""
    ein = lambda n, s, d=F32: nc.dram_tensor(n, s, d, kind="ExternalInput").ap()
    skind = "ExternalOutput" if dbg else "Internal"
    scr = lambda n, s, d: nc.dram_tensor(n, s, d, kind=skind).ap()
    k.x = ein("x", [NTOK, D])
    k.pcol_d = ein("pcol", [128, PCOL["_n"]])
    k.prow_d = ein("prow", [128, PROW["_n"]])
    k.consts_d = ein("consts", [128, 8 * 128])
    k.hy_in_w = ein("hy_in_w", [D, HYC])
    k.hy_out_w = ein("hy_out_w", [2048, D])
    k.ml_in_w = ein("ml_in_w", [D, MLC])
    k.ml_out_w = ein("ml_out_w", [D, D])
    k.ffn_up_w = ein("ffn_up_w", [2, D, 2 * DFF])
    k.ffn_down_w = ein("ffn_down_w", [2, DFF, D])
    k.out = nc.dram_tensor("out", [NTOK, D], F32, kind="ExternalOutput").ap()
    k.szT = scr("szT", [1024, NTOK], BF16)
    k.xbcT = scr("xbcT", [1536, NTOK], BF16)
    k.dts = scr("dts", [NTOK, 16], F32)
    k.qT = scr("qT", [1024, NTOK], BF16)
    k.kT = scr("kT", [1024, NTOK], BF16)
    k.vtok = scr("vtok", [NTOK, 1024], BF16)
    k.ycatT = scr("ycatT", [2048, NTOK], BF16)
    k.h1 = scr("h1", [NTOK, D], F32)
    k.h2 = scr("h2", [NTOK, D], F32)
    k.h3 = scr("h3", [NTOK, D], F32)
    k.gT = scr("gT", [DFF, NTOK], BF16)
    k.mqT = scr("mqT", [512, NTOK], BF16)
    k.mkT = scr("mkT", [512, NTOK], BF16)
    k.soT = scr("soT", [1024, NTOK], BF16)
    k.mkt = scr("mkt", [NTOK, 512], BF16)
    k.mvt = scr("mvt", [NTOK, 1024], BF16)
    k.gts = scr("gts", [NTOK, 16], F32)
    k.hmT = scr("hmT", [1024, NTOK], BF16)

    with ExitStack() as es:
        S = Sched(nc, es)
        k.S = S
        sb = lambda n, s, d: es.enter_context(nc.sbuf_tensor(n, s, d))
        k.cf = sb("cf", [128, 8 * 128], F32)
        k.cb = sb("cb", [128, 8 * 128], BF16)
        k.pcol = sb("pcolt", [128, PCOL["_n"]], F32)
        k.prow = sb("prowt", [128, PROW["_n"]], F32)
        S.dma("sp", k.cf[:], k.consts_d, writes=["cf"])
        S.dma("pool", k.cb[:], k.consts_d, writes=["cb"])
        S.dma("sp", k.pcol[:], k.pcol_d, writes=["pcol"])
        S.dma("sp", k.prow[:], k.prow_d, writes=["prow"])
        k.CF = lambda name: k.cf[:, CI[name] * 128:(CI[name] + 1) * 128]
        k.CB = lambda name: k.cb[:, CI[name] * 128:(CI[name] + 1) * 128]
        k.PC = lambda name, i=0, n=1: k.pcol[:, PCOL[name][0] + i:PCOL[name][0] + i + n]
        k.PR = lambda name: k.prow[:, PROW[name][0]:PROW[name][0] + PROW[name][1]]

        phases = [phase_a0, phase_b0, phase_c0,
                  lambda k, pes: phase_proj_res(k, pes, k.hy_out_w, 16, k.ycatT, k.x, k.h1),
                  lambda k, pes: phase_ffn_up(k, pes, 0, k.h1, k.gT),
                  lambda k, pes: phase_proj_res(k, pes, k.ffn_down_w[0], 22, k.gT, k.h1, k.h2),
                  phase_e1, phase_f1,
                  lambda k, pes: phase_proj_res(k, pes, k.ml_out_w, 8, k.hmT, k.h2, k.h3),
                  lambda k, pes: phase_ffn_up(k, pes, 1, k.h3, k.gT),
                  lambda k, pes: phase_proj_res(k, pes, k.ffn_down_w[1], 22, k.gT, k.h3, k.out)]
        for pi, ph in enumerate(phases[:nphase]):
            k.sfx = "_p%d" % pi
            with ExitStack() as pes:
                ph(k, pes)
                S.flush()
    return nc


def norm_transpose(k, S, xt, xkey, nw, hn_ring, hnT, hnT_key, ptr_ring, junk, small):
    ssq, lnv, rstd = small
    for j in range(4):
        S.op("act", lambda e, j=j: e.activation(out=junk[:], in_=xt[:, j, :], func=AF.Square,
                                                accum_out=ssq[:, j:j + 1]),
             reads=[xkey], writes=["junk", "ssq"])
    S.op("act", lambda e: e.activation(out=lnv[:], in_=ssq[:], func=AF.Ln, bias=EPS, scale=1.0 / D),
         reads=["ssq"], writes=["lnv"])
    S.op("act", lambda e: e.activation(out=rstd[:], in_=lnv[:], func=AF.Exp, scale=-0.5),
         reads=["lnv"], writes=["rstd"])
    hn, hkey = hn_ring.nxt()
    for j in range(4):
        S.op("dve", lambda e, j=j: e.scalar_tensor_tensor(out=hn[:, j, :], in0=xt[:, j, :], scalar=rstd[:, j:j + 1],
                                                          in1=nw, op0=ALU.mult, op1=ALU.mult),
             reads=[xkey, "rstd", "prow"], writes=[(hkey, j)])
    for j in range(4):
        ptr, pkey = ptr_ring.nxt()
        for kc in range(8):
            S.op("pe", lambda e, j=j, kc=kc, ptr=ptr: e.transpose(out=ptr[:, kc * 128:(kc + 1) * 128],
                                                                  in_=hn[:, j, kc * 128:(kc + 1) * 128],
                                                                  identity=k.CB("eye")),
                 reads=[(hkey, j), "cb"], writes=[pkey], accumulate=(kc > 0))
        eng = "act" if j % 2 == 0 else "dve"
        if eng == "act":
            S.op("act", lambda e, j=j, ptr=ptr: e.copy(hnT[:, :, j * 128:(j + 1) * 128],
                                                       ptr[:].rearrange("p (c t) -> p c t", t=128)),
                 reads=[pkey], writes=[hnT_key])
        else:
            S.op("dve", lambda e, j=j, ptr=ptr: e.tensor_copy(hnT[:, :, j * 128:(j + 1) * 128],
                                                              ptr[:].rearrange("p (c t) -> p c t", t=128)),
                 reads=[pkey], writes=[hnT_key])


def phase_a0(k, es):
    nc, S = k.nc, k.S
    sb = lambda n, s, d: es.enter_context(nc.sbuf_tensor(n + k.sfx, s, d))
    ps = lambda n, s, d: es.enter_context(nc.psum_tensor(n + k.sfx, s, d))
    win = sb("win", [128, 8, HYC], BF16)
    for kc in range(8):
        S.dma("pool", win[:, kc, :], k.hy_in_w[kc * 128:(kc + 1) * 128, :], writes=[("win", kc)])
    winkeys = [("win", kc) for kc in range(8)]
    xt_ring = Ring(sb, "xt", 2, [128, 4, D], F32)
    hn_ring = Ring(sb, "hn", 1, [128, 4, D], BF16)
    hnT_ring = Ring(sb, "hnT", 2, [128, 8, 512], BF16)
    ptr_ring = Ring(ps, "ptr", 1, [128, 1024], BF16)
    pb_ring = Ring(ps, "pb", 5, [128, 512], F32)
    pss_ring = Ring(ps, "pss", 2, [128, 512], F32)
    junk = sb("junk", [128, D], BF16)
    small = (sb("ssq", [128, 4], F32), sb("lnv", [128, 4], F32), sb("rstd", [128, 4], F32))
    ob_ring = Ring(sb, "ob", 4, [128, 512], BF16)
    raw_ring = Ring(sb, "raw", 3, [128, 515], F32)
    acc_ring = Ring(sb, "acc", 4, [128, 512], F32)
    sq_ring = Ring(sb, "sqb", 3, [128, 512], BF16)
    ta_ring = Ring(sb, "ta", 2, [128, 512], F32)
    tb_ring = Ring(sb, "tb", 3, [128, 512], F32)
    halo = sb("halo", [128, 12, 3], F32)
    qws = sb("qws", [128, 1], F32)
    dtt_ring = Ring(sb, "dtt", 2, [128, 4, 16], F32)
    dte = sb("dte", [128, 64], F32)
    S.op("act", lambda e: e.mul(qws[:], k.PC("qw"), 0.125), reads=["pcol"], writes=["qws"])

    def load_x(tt):
        xt, xkey = xt_ring.nxt()
        S.dma("sp", xt[:], k.x[tt * 512:(tt + 1) * 512, :].rearrange("(j p) d -> p j d", p=128), writes=[xkey])
        return xt, xkey

    def prologue(xx):
        xt, xkey = xx
        hnT, hkey = hnT_ring.nxt()
        norm_transpose(k, S, xt, xkey, k.PR("nw00"), hn_ring, hnT, hkey, ptr_ring, junk, small)
        return hnT, hkey

    xs = {0: load_x(0)}
    if NTT > 1:
        xs[1] = load_x(1)
    pro = {0: prologue(xs[0])}
    for tt in range(NTT):
        if tt + 2 < NTT:
            xs[tt + 2] = load_x(tt + 2)
        hnT, hkey = pro[tt]
        tok0 = tt * 512
        seq_start = (tt % 4 == 0)

        def proj_fm(col0):
            pb, pkey = pb_ring.nxt()
            for kc in range(8):
                S.op("pe", lambda e, kc=kc, pb=pb: e.matmul(pb[:], lhsT=win[:, kc, col0:col0 + 128], rhs=hnT[:, kc, :],
                                                            start=(kc == 0), stop=(kc == 7)),
                     reads=[hkey, ("win", kc)], writes=[pkey], accumulate=(kc > 0))
            return pb, pkey

        for c in range(8):
            pb, pkey = proj_fm(c * 128)
            ob, okey = ob_ring.nxt()
            S.op("act", lambda e, pb=pb, ob=ob: e.activation(out=ob[:], in_=pb[:], func=AF.Silu),
                 reads=[pkey], writes=[okey])
            S.dma("sp", k.szT[c * 128:(c + 1) * 128, tok0:tok0 + 512], ob[:], reads=[okey])
        if DBGPRINT: print('a0 after z', S.nseq)
        def x1(c):
            pb, pkey = proj_fm(1024 + c * 128)
            raw, rkey = raw_ring.nxt()
            acc, akey = acc_ring.nxt()
            if seq_start:
                S.op("pool", lambda e: e.memset(raw[:, 0:3], 0.0), writes=[(rkey, "h")])
            else:
                S.op("pool", lambda e: e.tensor_copy(raw[:, 0:3], halo[:, c, :]), reads=[("halo", c)],
                     writes=[(rkey, "h")])
            S.op("act", lambda e: e.copy(raw[:, 3:515], pb[:]), reads=[pkey], writes=[rkey])
            S.op("act", lambda e: e.activation(out=acc[:], in_=pb[:], func=AF.Identity, bias=k.PC("scb", c),
                                               scale=k.PC("scw", c * 4 + 3)), reads=[pkey, "pcol"], writes=[akey])
            return (raw, rkey, acc, akey, c)

        def xtap(t, tp):
            raw, rkey, acc, akey, c = t
            S.op("dve", lambda e: e.scalar_tensor_tensor(out=acc[:], in0=raw[:, tp:tp + 512],
                                                         scalar=k.PC("scw", c * 4 + tp), in1=acc[:], op0=ALU.mult,
                                                         op1=ALU.add), reads=[rkey, (rkey, "h"), akey, "pcol"], writes=[akey])

        def xhalo(t):
            raw, rkey, acc, akey, c = t
            S.op("pool", lambda e: e.tensor_copy(halo[:, c, :], raw[:, 512:515]), reads=[rkey], writes=[("halo", c)])

        def x3(t):
            raw, rkey, acc, akey, c = t
            ob, okey = ob_ring.nxt()
            S.op("act", lambda e: e.activation(out=ob[:], in_=acc[:], func=AF.Silu), reads=[akey], writes=[okey])
            S.dma("sp", k.xbcT[c * 128:(c + 1) * 128, tok0:tok0 + 512], ob[:], reads=[okey])

        its = {}
        for n in range(12 + 3):
            if n < 12:
                its[n] = x1(n)
            a_ok, b_ok = 0 <= n - 1 < 12, 0 <= n - 2 < 12
            if a_ok:
                xtap(its[n - 1], 0)
            if b_ok:
                xtap(its[n - 2], 2)
                xhalo(its[n - 2])
            if a_ok:
                xtap(its[n - 1], 1)
            if 0 <= n - 3 < 12:
                x3(its[n - 3])
        if tt + 1 < NTT:
            pro[tt + 1] = prologue(xs[tt + 1])

        def q1(n):
            which, c = n // 8, n % 8
            pb, pkey = proj_fm((2576 if which == 0 else 3600) + c * 128)
            sq, sqkey = sq_ring.nxt()
            S.op("act", lambda e: e.activation(out=sq[:], in_=pb[:], func=AF.Square), reads=[pkey], writes=[sqkey])
            return {"pb": pb, "pkey": pkey, "sq": sq, "sqkey": sqkey, "which": which, "c": c}

        def q2(t):
            pss, psskey = pss_ring.nxt()
            sq, sqkey = t["sq"], t["sqkey"]
            S.op("pe", lambda e: e.matmul(pss[:], lhsT=k.CB("bd"), rhs=sq[:], start=True, stop=True),
                 reads=[sqkey, "cb"], writes=[psskey])
            ta, takey = ta_ring.nxt()
            tb, tbkey = tb_ring.nxt()
            S.op("act", lambda e: e.activation(out=ta[:], in_=pss[:], func=AF.Ln, bias=EPS, scale=1.0 / 64),
                 reads=[psskey], writes=[takey])
            S.op("act", lambda e: e.activation(out=tb[:], in_=ta[:], func=AF.Exp, scale=-0.5), reads=[takey], writes=[tbkey])
            t["tb"], t["tbkey"] = tb, tbkey

        def q3(t):
            ob, okey = ob_ring.nxt()
            pb, pkey, tb, tbkey, which, c = t["pb"], t["pkey"], t["tb"], t["tbkey"], t["which"], t["c"]
            wcol = qws[:, 0:1] if which == 0 else k.PC("kw")
            S.op("dve", lambda e: e.scalar_tensor_tensor(out=ob[:], in0=pb[:], scalar=wcol, in1=tb[:], op0=ALU.mult,
                                                         op1=ALU.mult), reads=[pkey, tbkey, "qws", "pcol"], writes=[okey])
            dst = k.qT if which == 0 else k.kT
            S.dma("sp", dst[c * 128:(c + 1) * 128, tok0:tok0 + 512], ob[:], reads=[okey])

        its = {}
        for n in range(16 + 2):
            if n < 16:
                its[n] = q1(n)
            if 0 <= n - 1 < 16:
                q2(its[n - 1])
            if 0 <= n - 2 < 16:
                q3(its[n - 2])
        if DBGPRINT: print('a0 after qk', S.nseq)
        pb, pkey = pb_ring.nxt()
        for j in range(4):
            for kc in range(8):
                S.op("pe", lambda e, j=j, kc=kc, pb=pb: e.matmul(pb[:, j * 16:(j + 1) * 16],
                                                                lhsT=hnT[:, kc, j * 128:(j + 1) * 128],
                                                                rhs=win[:, kc, 2560:2576], start=(kc == 0), stop=(kc == 7)),
                     reads=[hkey, ("win", kc)], writes=[pkey], accumulate=(j + kc > 0))
        dtt, dkey = dtt_ring.nxt()
        S.op("dve", lambda e, pb=pb: e.tensor_tensor(dte[:].rearrange("p (j h) -> p j h", h=16),
                                                     pb[:, 0:64].rearrange("p (j h) -> p j h", h=16),
                                                     k.PR("dtb").unsqueeze(1).to_broadcast([128, 4, 16]), op=ALU.add),
             reads=[pkey, "prow"], writes=["dte"])
        S.op("act", lambda e: e.activation(out=dte[:], in_=dte[:], func=AF.Exp), reads=["dte"], writes=["dte"])
        S.op("act", lambda e, dtt=dtt: e.activation(out=dtt[:].rearrange("p j h -> p (j h)"), in_=dte[:], func=AF.Ln,
                                                    bias=1.0),
             reads=["dte"], writes=[dkey])
        S.dma("sp", k.dts[tok0:tok0 + 512, :].rearrange("(j p) h -> p j h", p=128), dtt[:], reads=[dkey])
        if DBGPRINT: print('a0 after dt', S.nseq)
        for j in range(4):
            for half in range(2):
                pb, pkey = pb_ring.nxt()
                for kc in range(8):
                    S.op("pe", lambda e, j=j, kc=kc, pb=pb, half=half: e.matmul(
                        pb[:], lhsT=hnT[:, kc, j * 128:(j + 1) * 128],
                        rhs=win[:, kc, 4624 + half * 512:4624 + (half + 1) * 512], start=(kc == 0), stop=(kc == 7)),
                        reads=[hkey, ("win", kc)], writes=[pkey], accumulate=(kc > 0))
                ob, okey = ob_ring.nxt()
                if half == 0:
                    S.op("act", lambda e, pb=pb, ob=ob: e.copy(ob[:], pb[:]), reads=[pkey], writes=[okey])
                else:
                    S.op("dve", lambda e, pb=pb, ob=ob: e.tensor_copy(ob[:], pb[:]), reads=[pkey], writes=[okey])
                S.dma("sp", k.vtok[tok0 + j * 128:tok0 + (j + 1) * 128, half * 512:(half + 1) * 512], ob[:],
                      reads=[okey])


def phase_b0(k, es):
    nc, S = k.nc, k.S
    sb = lambda n, s, d: es.enter_context(nc.sbuf_tensor(n + k.sfx, s, d))
    ps = lambda n, s, d: es.enter_context(nc.psum_tensor(n + k.sfx, s, d))
    CF, CB, PC, PR = k.CF, k.CB, k.PC, k.PR
    xb_ring = Ring(sb, "xb", 2, [128, 12, 512], BF16)
    sz_ring = Ring(sb, "szt", 2, [128, 8, 512], BF16)
    dtl_ring = Ring(sb, "dtl", 2, [128, 4, 16], F32)
    yo_ring = Ring(sb, "yo", 2, [128, 8, 512], BF16)
    da_ring = Ring(sb, "da", 3, [128, 16], F32)
    sm_ring = Ring(sb, "sm", 3, [128, 48], F32)
    xtil_ring = Ring(sb, "xtil", 3, [128, 1024], BF16)
    xtd_ring = Ring(sb, "xtd", 3, [128, 1024], BF16)
    bmt_ring = Ring(sb, "bmt", 3, [128, 256], BF16)
    cbm_ring = Ring(sb, "cbm", 3, [128, 2, 128], F32)
    dec_ring = Ring(sb, "dec", 2, [128, 512], F32)
    eb_ring = Ring(sb, "eb", 2, [128, 512], F32)
    mt_ring = Ring(sb, "mt", 9, [128, 512], BF16)
    ce_ring = Ring(sb, "ce", 9, [128, 512], BF16)
    gv_ring = Ring(sb, "gv", 3, [128, 8, 128], F32)
    sq_ring = Ring(sb, "ssq2", 3, [128, 8, 128], BF16)
    rs_ring = Ring(sb, "rs", 2, [128, 128], F32)
    ln_ring = Ring(sb, "lnb", 2, [128, 128], F32)
    ab = sb("ab", [128, 16], F32)
    g4 = sb("g4", [128, 512], F32)
    prev_f = sb("prev_f", [128, 1024], F32)
    prev_b = sb("prev_b", [128, 1024], BF16)
    pmA_ring = Ring(ps, "pmA", 1, [128, 512], F32)
    ptx_ring = Ring(ps, "ptx", 1, [128, 1024], BF16)
    ptb_ring = Ring(ps, "ptb", 1, [128, 256], BF16)
    pw_ring = Ring(ps, "pw", 3, [128, 512], F32)
    py_ring = Ring(ps, "py", 2, [128, 512], F32)

    S.op("act", lambda e: e.activation(out=ab[:], in_=PR("alog"), func=AF.Exp), reads=["prow"], writes=["ab"])
    S.op("act", lambda e: e.mul(ab[:], ab[:], -1.0), reads=["ab"], writes=["ab"])
    for q in range(4):
        S.op("pool", lambda e, q=q: e.tensor_copy(g4[:, q * 128:(q + 1) * 128], CF("gt")), reads=["cf"], writes=["g4"],
             accumulate=(q > 0))

    def load(tt):
        tok0 = tt * 512
        xb, xkey = xb_ring.nxt()
        szt, skey = sz_ring.nxt()
        dtl, dkey = dtl_ring.nxt()
        S.dma("sp", xb[:], k.xbcT[:, tok0:tok0 + 512].rearrange("(c p) t -> p c t", p=128), writes=[xkey])
        S.dma("sp", szt[:], k.szT[:, tok0:tok0 + 512].rearrange("(c p) t -> p c t", p=128), writes=[skey])
        S.dma("sp", dtl[:], k.dts[tok0:tok0 + 512, :].rearrange("(j p) h -> p j h", p=128), writes=[dkey])
        return xb, xkey, szt, skey, dtl, dkey

    tiles = {}

    def get_tile(tt):
        if tt not in tiles:
            tiles[tt] = load(tt) + yo_ring.nxt()
        return tiles[tt]

    def P(c):
        xb, xkey, szt, skey, dtl, dkey, yo, yokey = get_tile(c["tt"])
        j = c["j"]
        js = slice(j * 128, (j + 1) * 128)
        da, dakey = da_ring.nxt()
        S.op("dve", lambda e: e.tensor_tensor(da[:], dtl[:, j, :], ab[:], op=ALU.mult), reads=[dkey, "ab"], writes=[dakey])
        pmA, pmkey = pmA_ring.nxt()
        for i, cname in enumerate(("le", "gt", "ones")):
            S.op("pe", lambda e: e.matmul(pmA[:, i * 16:(i + 1) * 16], lhsT=CF(cname), rhs=da[:], start=True, stop=True),
                 reads=[dakey, "cf"], writes=[(pmkey, "c")], accumulate=(i > 0))
        sm, smkey = sm_ring.nxt()
        S.op("act", lambda e: e.mul(sm[:, 0:16], pmA[:, 0:16], -1.0), reads=[(pmkey, "c")], writes=[smkey])
        S.op("act", lambda e: e.activation(out=sm[:, 16:48], in_=pmA[:, 16:48], func=AF.Exp), reads=[(pmkey, "c")],
             writes=[smkey], accumulate=True)
        ptx, ptxkey = ptx_ring.nxt()
        for fc in range(8):
            S.op("pe", lambda e: e.transpose(out=ptx[:, fc * 128:(fc + 1) * 128], in_=xb[:, fc, js], identity=CB("eye")),
                 reads=[xkey, "cb"], writes=[ptxkey], accumulate=(fc > 0))
        xtil, xtkey = xtil_ring.nxt()
        xtd, xdkey = xtd_ring.nxt()
        S.op("dve", lambda e: e.tensor_tensor(
            xtil[:].rearrange("p (h d) -> p h d", d=64), ptx[:].rearrange("p (h d) -> p h d", d=64),
            dtl[:, j, :].unsqueeze(2).to_broadcast([128, 16, 64]), op=ALU.mult), reads=[ptxkey, dkey], writes=[xtkey])
        S.op("pool", lambda e: e.tensor_tensor(
            xtd[:].rearrange("p (h d) -> p h d", d=64), xtil[:].rearrange("p (h d) -> p h d", d=64),
            sm[:, 16:32].unsqueeze(2).to_broadcast([128, 16, 64]), op=ALU.mult), reads=[xtkey, smkey], writes=[xdkey])
        ptb, ptbkey = ptb_ring.nxt()
        for g in range(2):
            S.op("pe", lambda e: e.transpose(out=ptb[:, g * 128:(g + 1) * 128], in_=xb[:, 8 + g, js], identity=CB("eye")),
                 reads=[xkey, "cb"], writes=[ptbkey], accumulate=(g > 0))
        bmt, bmkey = bmt_ring.nxt()
        S.op("act", lambda e: e.copy(bmt[:], ptb[:]), reads=[ptbkey], writes=[bmkey])
        for g in range(2):
            S.op("pe", lambda e: e.matmul(pmA[:, 64 + g * 128:64 + (g + 1) * 128], lhsT=xb[:, 8 + g, js],
                                          rhs=xb[:, 10 + g, js], start=True, stop=True),
                 reads=[xkey], writes=[(pmkey, "cb")], accumulate=(g > 0))
        cbm, cbkey = cbm_ring.nxt()
        S.op("dve", lambda e: e.tensor_tensor(cbm[:], pmA[:, 64:320].rearrange("p (g t) -> p g t", t=128),
                                              CF("le").unsqueeze(1).to_broadcast([128, 2, 128]), op=ALU.mult),
             reads=[(pmkey, "cb"), "cf"], writes=[cbkey])
        c.update(da=da, dakey=dakey, pmA=pmA, pmkey=pmkey, sm=sm, smkey=smkey, xtil=xtil, xtkey=xtkey, xtd=xtd,
                 xdkey=xdkey, bmt=bmt, bmkey=bmkey, cbm=cbm, cbkey=cbkey)
        c["mt"], c["ce"] = [], []
        for hq in range(4):
            g = hq // 2
            pam, pamkey = pw_ring.nxt()
            pau, paukey = pw_ring.nxt()
            S.op("pe", lambda e: e.matmul(pam[:], lhsT=CF("negbig"), rhs=g4[:], start=True, stop=False),
                 reads=["cf", "g4"], writes=[pamkey])
            for hh in range(4):
                h = hq * 4 + hh
                cs = slice(hh * 128, (hh + 1) * 128)
                lb = da[:, h:h + 1].to_broadcast([128, 128])
                S.op("pe", lambda e: e.matmul(pam[:, cs], lhsT=lb, rhs=CF("le"), start=False, stop=True),
                     reads=[dakey, "cf"], writes=[pamkey], accumulate=True)
                S.op("pe", lambda e: e.matmul(pau[:, cs], lhsT=lb, rhs=CF("le"), start=True, stop=True),
                     reads=[dakey, "cf"], writes=[paukey], accumulate=(hh > 0))
            eb, ebkey = eb_ring.nxt()
            S.op("act", lambda e: e.activation(out=eb[:], in_=pau[:], func=AF.Exp), reads=[paukey], writes=[ebkey])
            dec, deckey = dec_ring.nxt()
            for hh in range(4):
                h = hq * 4 + hh
                cs = slice(hh * 128, (hh + 1) * 128)
                S.op("act", lambda e: e.activation(out=dec[:, cs], in_=pam[:, cs], func=AF.Exp, bias=sm[:, h:h + 1]),
                     reads=[pamkey, smkey], writes=[deckey], accumulate=(hh > 0))
            mt, mtkey = mt_ring.nxt()
            S.op("dve", lambda e: e.tensor_tensor(mt[:].rearrange("p (h t) -> p h t", t=128),
                                                  dec[:].rearrange("p (h t) -> p h t", t=128),
                                                  cbm[:, g, :].unsqueeze(1).to_broadcast([128, 4, 128]), op=ALU.mult),
                 reads=[deckey, cbkey], writes=[mtkey])
            ce, cekey = ce_ring.nxt()
            S.op("pool", lambda e: e.tensor_tensor(ce[:].rearrange("p (h t) -> p h t", t=128),
                                                   eb[:].rearrange("p (h t) -> p h t", t=128),
                                                   xb[:, 10 + g, js].unsqueeze(1).to_broadcast([128, 4, 128]), op=ALU.mult),
                 reads=[xkey, ebkey], writes=[cekey])
            c["mt"].append((mt, mtkey))
            c["ce"].append((ce, cekey))

    def H(c):
        xtil, xtkey = c["xtil"], c["xtkey"]
        if c["tt"] % 4 == 0 and c["j"] == 0:
            S.op("pool", lambda e: e.memset(prev_f[:], 0.0), writes=["prev_f"])
            S.op("pool", lambda e: e.memset(prev_b[:], 0.0), writes=["prev_b"])
        pys = [py_ring.nxt(), py_ring.nxt()]
        c["pys"] = pys
        for h in range(16):
            mt, mtkey = c["mt"][h // 4]
            ce, cekey = c["ce"][h // 4]
            cs = slice((h % 4) * 128, (h % 4 + 1) * 128)
            py, pykey = pys[h // 8]
            fcl = (h // 2) % 4
            po = (h % 2) * 64
            outap = py[po:po + 64, fcl * 128:(fcl + 1) * 128]
            S.op("pe", lambda e: e.matmul(outap, lhsT=xtil[:, h * 64:(h + 1) * 64], rhs=mt[:, cs], start=True, stop=False),
                 reads=[xtkey, mtkey], writes=[pykey], accumulate=(h % 8 > 0))
            S.op("pe", lambda e: e.matmul(outap, lhsT=prev_b[:, h * 64:(h + 1) * 64], rhs=ce[:, cs], start=False, stop=True),
                 reads=["prev_b", cekey], writes=[pykey], accumulate=True)
        sm, smkey, bmt, bmkey, xtd, xdkey = c["sm"], c["smkey"], c["bmt"], c["bmkey"], c["xtd"], c["xdkey"]
        psts = [pw_ring.nxt(), pw_ring.nxt()]
        for g in range(2):
            pst, pstkey = psts[g]
            S.op("pe", lambda e: e.matmul(pst[:], lhsT=bmt[:, g * 128:(g + 1) * 128], rhs=xtd[:, g * 512:(g + 1) * 512],
                                          start=True, stop=True), reads=[bmkey, xdkey], writes=[pstkey])
        S.op("dve", lambda e: e.tensor_tensor(
            prev_f[:].rearrange("p (h d) -> p h d", d=64), prev_f[:].rearrange("p (h d) -> p h d", d=64),
            sm[:, 32:48].unsqueeze(2).to_broadcast([128, 16, 64]), op=ALU.mult),
            reads=["prev_f", smkey], writes=["prev_f"])
        for g in range(2):
            pst, pstkey = psts[g]
            S.op("dve", lambda e: e.tensor_tensor(prev_f[:, g * 512:(g + 1) * 512], prev_f[:, g * 512:(g + 1) * 512],
                                                  pst[:], op=ALU.add), reads=["prev_f", pstkey], writes=["prev_f"])
        S.op("act", lambda e: e.copy(prev_b[:], prev_f[:]), reads=["prev_f"], writes=["prev_b"])

    def E(c):
        xb, xkey, szt, skey, dtl, dkey, yo, yokey = get_tile(c["tt"])
        j = c["j"]
        js = slice(j * 128, (j + 1) * 128)
        pys, pmA, pmkey = c["pys"], c["pmA"], c["pmkey"]
        gv, gvkey = gv_ring.nxt()
        for fc in range(8):
            py, pykey = pys[fc // 4]
            S.op("dve", lambda e: e.scalar_tensor_tensor(
                out=gv[:, fc, :], in0=xb[:, fc, js], scalar=PC("sdd", fc), in1=py[:, (fc % 4) * 128:(fc % 4 + 1) * 128],
                op0=ALU.mult, op1=ALU.add), reads=[xkey, pykey, "pcol"], writes=[gvkey], accumulate=(fc > 0))
        S.op("pool", lambda e: e.tensor_tensor(gv[:], gv[:], szt[:, :, js], op=ALU.mult), reads=[gvkey, skey],
             writes=[gvkey])
        sq, sqkey = sq_ring.nxt()
        S.op("act", lambda e: e.activation(out=sq[:], in_=gv[:], func=AF.Square), reads=[gvkey], writes=[sqkey])
        c.update(gv=gv, gvkey=gvkey, sq=sq, sqkey=sqkey)

    def E2(c):
        xb, xkey, szt, skey, dtl, dkey, yo, yokey = get_tile(c["tt"])
        j = c["j"]
        js = slice(j * 128, (j + 1) * 128)
        pmA, pmkey, gv, gvkey, sq, sqkey = c["pmA"], c["pmkey"], c["gv"], c["gvkey"], c["sq"], c["sqkey"]
        for fc in range(8):
            S.op("pe", lambda e: e.matmul(pmA[:, 384:512], lhsT=CB("ones"), rhs=sq[:, fc, :], start=(fc == 0),
                                          stop=(fc == 7)), reads=[sqkey, "cb"], writes=[(pmkey, "ss")], accumulate=(fc > 0))
        lnb, lnkey = ln_ring.nxt()
        rs, rskey = rs_ring.nxt()
        S.op("act", lambda e: e.activation(out=lnb[:], in_=pmA[:, 384:512], func=AF.Ln, bias=EPS, scale=1.0 / 1024),
             reads=[(pmkey, "ss")], writes=[lnkey])
        S.op("act", lambda e: e.activation(out=rs[:], in_=lnb[:], func=AF.Exp, scale=-0.5), reads=[lnkey], writes=[rskey])
        S.op("dve", lambda e: e.tensor_tensor(gv[:], gv[:], rs[:].unsqueeze(1).to_broadcast([128, 8, 128]), op=ALU.mult),
             reads=[gvkey, rskey], writes=[gvkey])
        S.op("pool", lambda e: e.tensor_tensor(yo[:, :, js], gv[:], PC("snw", 0, 8).unsqueeze(2).to_broadcast([128, 8, 128]),
                                               op=ALU.mult), reads=[gvkey, "pcol"], writes=[yokey], accumulate=(j > 0))
        if j == 3:
            tok0 = c["tt"] * 512
            S.dma("sp", k.ycatT[0:1024, tok0:tok0 + 512].rearrange("(c p) t -> p c t", p=128), yo[:], reads=[yokey])

    chunks = [{"tt": tt, "j": j} for tt in range(NTT) for j in range(4)]
    get_tile(0)
    P(chunks[0])
    for ci, c in enumerate(chunks):
        if c["j"] == 0 and c["tt"] + 1 < NTT:
            get_tile(c["tt"] + 1)
        H(c)
        if ci > 0:
            E2(chunks[ci - 1])
        if ci + 1 < len(chunks):
            P(chunks[ci + 1])
        E(c)
    E2(chunks[-1])


def phase_c0(k, es):
    nc, S = k.nc, k.S
    sb = lambda n, s, d: es.enter_context(nc.sbuf_tensor(n + k.sfx, s, d))
    ps = lambda n, s, d: es.enter_context(nc.psum_tensor(n + k.sfx, s, d))
    CF, CB = k.CF, k.CB
    q_ring = Ring(sb, "qp", 2, [128, L], BF16)
    k_ring = Ring(sb, "kp", 2, [128, L], BF16)
    vb_ring = Ring(sb, "vb", 1, [128, 16, 1024], BF16)
    e_ring = Ring(sb, "ee", 3, [128, 512], F32)
    sp_ring = Ring(sb, "spp", 5, [128, 512], BF16)
    bt_ring = Ring(sb, "btt", 3, [128, 512], BF16)
    r_ring = Ring(sb, "rr", 3, [128, 4], F32)
    accs = [sb("accA", [128, 4, 64], F32), sb("accB", [128, 4, 64], F32)]
    osb_ring = Ring(sb, "osb", 2, [128, 4, 128], BF16)
    oT_ring = Ring(sb, "oT", 2, [128, 512], BF16)
    pz_ring = Ring(ps, "pz", 2, [128, 512], F32)
    py_ring = Ring(ps, "pyy", 3, [128, 512], F32)
    pov_ring = Ring(ps, "pov", 2, [128, 4, 65], F32)
    ptr_ring = Ring(ps, "ptc", 1, [128, 512], BF16)

    for b in range(2):
        vb, vkey = vb_ring.nxt()
        S.dma("sp", vb[:], k.vtok[b * L:(b + 1) * L, :].rearrange("(i p) d -> p i d", p=128), writes=[vkey])
        for hp in range(8):
            qp, qkey = q_ring.nxt()
            kp, kkey = k_ring.nxt()
            S.dma("sp", qp[:], k.qT[hp * 128:(hp + 1) * 128, b * L:(b + 1) * L], writes=[qkey])
            S.dma("sp", kp[:], k.kT[hp * 128:(hp + 1) * 128, b * L:(b + 1) * L], writes=[kkey])
            for g in range(4):
                items = []
                for i in range(4 * g + 4):
                    for hh in range(2):
                        items.append({"i": i, "hh": hh})
                osb, oskey = osb_ring.nxt()
                for hh in range(2):
                    S.op("pool", lambda e, hh=hh: e.memset(accs[hh][:], 0.0), writes=[("acc", hh)])

                def geom(it):
                    i = it["i"]
                    qlo = max(0, i - 4 * g)
                    n = (4 - qlo) * 128
                    t0 = (4 * g + qlo) * 128
                    po = it["hh"] * 64
                    return i, qlo, n, t0, po

                def zmm(it, pt, pkey):
                    i, qlo, n, t0, po = geom(it)
                    diag = i >= 4 * g
                    S.op("pe", lambda e: e.matmul(pt[:, 0:n], lhsT=kp[po:po + 64, i * 128:(i + 1) * 128],
                                                  rhs=qp[po:po + 64, t0:t0 + n], start=True, stop=False),
                         reads=[qkey, kkey], writes=[pkey])
                    if diag:
                        S.op("pe", lambda e: e.matmul(pt[:, 0:128], lhsT=CB("negbig"), rhs=CB("ge"), start=False,
                                                      stop=False), reads=["cb"], writes=[pkey], accumulate=True)

                def s1(it):
                    it["pz"], it["pzkey"] = pz_ring.nxt()
                    zmm(it, it["pz"], it["pzkey"])
                    return

                def s2(it):
                    i, qlo, n, t0, po = geom(it)
                    ee, ekey = e_ring.nxt()
                    sp, spkey = sp_ring.nxt()
                    it["sp"], it["spkey"] = sp, spkey
                    pz, pzkey = it["pz"], it["pzkey"]
                    S.op("act", lambda e: e.activation(out=ee[:, 0:n], in_=pz[:, 0:n], func=AF.Exp),
                         reads=[pzkey], writes=[ekey])
                    S.op("act", lambda e: e.activation(out=sp[:, 0:n], in_=ee[:, 0:n], func=AF.Ln, bias=1.0),
                         reads=[ekey], writes=[spkey])

                def s3(it):
                    i, qlo, n, t0, po = geom(it)
                    it["py"], it["pykey"] = py_ring.nxt()
                    zmm(it, it["py"], it["pykey"])
                    py, sp = it["py"], it["sp"]
                    S.op("pe", lambda e: e.matmul(py[:, 0:n], lhsT=CB("negu"), rhs=sp[:, 0:n], start=False, stop=True),
                         reads=[it["spkey"], "cb"], writes=[it["pykey"]], accumulate=True)

                def s4(it):
                    i, qlo, n, t0, po = geom(it)
                    bt, btkey = bt_ring.nxt()
                    it["bt"], it["btkey"] = bt, btkey
                    py = it["py"]
                    S.op("act", lambda e: e.activation(out=bt[:, 0:n], in_=py[:, 0:n], func=AF.Exp),
                         reads=[it["pykey"]], writes=[btkey])

                def s5(it):
                    i, qlo, n, t0, po = geom(it)
                    pov, povkey = pov_ring.nxt()
                    it["pov"], it["povkey"] = pov, povkey
                    bt, sp = it["bt"], it["sp"]
                    h = hp * 2 + it["hh"]
                    first = True
                    for qq in range(qlo, 4):
                        cs = slice((qq - qlo) * 128, (qq - qlo + 1) * 128)
                        S.op("pe", lambda e, qq=qq, cs=cs: e.matmul(pov[:, qq, 0:64], lhsT=bt[:, cs],
                                                                    rhs=vb[:, i, h * 64:(h + 1) * 64], start=True, stop=True),
                             reads=[it["btkey"], vkey], writes=[povkey], accumulate=(not first))
                        first = False
                        S.op("pe", lambda e, qq=qq, cs=cs: e.matmul(pov[:, qq, 64:65], lhsT=sp[:, cs], rhs=CB("ones")[:, 0:1],
                                                                    start=True, stop=True),
                             reads=[it["spkey"], "cb"], writes=[povkey], accumulate=True)

                def s6(it):
                    i, qlo, n, t0, po = geom(it)
                    rr, rkey = r_ring.nxt()
                    pov, povkey = it["pov"], it["povkey"]
                    hh = it["hh"]
                    acc = accs[hh]
                    nq = 4 - qlo
                    S.op("act", lambda e: e.activation(out=rr[:, qlo:4].unsqueeze(2), in_=pov[:, qlo:4, 64:65], func=AF.Exp,
                                                       scale=-1.0), reads=[povkey], writes=[rkey])
                    S.op("dve", lambda e: e.tensor_tensor(acc[:, qlo:4, :], acc[:, qlo:4, :],
                                                          rr[:, qlo:4].unsqueeze(2).to_broadcast([128, nq, 64]), op=ALU.mult),
                         reads=[rkey, ("acc", hh)], writes=[("acc", hh)])
                    S.op("dve", lambda e: e.tensor_tensor(acc[:, qlo:4, :], acc[:, qlo:4, :], pov[:, qlo:4, 0:64], op=ALU.add),
                         reads=[povkey, ("acc", hh)], writes=[("acc", hh)])

                stages = [s1, s2, s3, s4, s5, s6]
                lag = [0, 0, 1, 1, 2, 2]
                for n in range(len(items) + 2):
                    for st, lg in zip(stages, lag):
                        m = n - lg
                        if 0 <= m < len(items):
                            st(items[m])
                for hh in range(2):
                    S.op("pool", lambda e, hh=hh: e.tensor_copy(osb[:, :, hh * 64:(hh + 1) * 64], accs[hh][:]),
                         reads=[("acc", hh)], writes=[oskey], accumulate=(hh > 0))
                ptc, ptckey = ptr_ring.nxt()
                for qq in range(4):
                    S.op("pe", lambda e, qq=qq: e.transpose(out=ptc[:, qq * 128:(qq + 1) * 128], in_=osb[:, qq, :],
                                                            identity=CB("eye")),
                         reads=[oskey, "cb"], writes=[ptckey], accumulate=(qq > 0))
                oT, oTkey = oT_ring.nxt()
                S.op("dve", lambda e: e.tensor_copy(oT[:], ptc[:]), reads=[ptckey], writes=[oTkey])
                tok0 = b * L + g * 512
                S.dma("sp", k.ycatT[1024 + hp * 128:1024 + (hp + 1) * 128, tok0:tok0 + 512], oT[:], reads=[oTkey])


def phase_proj_res(k, es, w_dram, kc_n, srcT, h_in, h_out):
    nc, S = k.nc, k.S
    sb = lambda n, s, d: es.enter_context(nc.sbuf_tensor(n + k.sfx, s, d))
    ps = lambda n, s, d: es.enter_context(nc.psum_tensor(n + k.sfx, s, d))
    w = sb("wres", [128, kc_n, D], BF16)
    for c in range(kc_n):
        S.dma("pool", w[:, c, :], w_dram[c * 128:(c + 1) * 128, :], writes=[("wres", c)])
    src_ring = Ring(sb, "srct", 2, [128, kc_n, 512], BF16)
    h_ring = Ring(sb, "hres", 2, [128, 4, D], F32)
    po_ring = Ring(ps, "pres", 4, [128, 512], F32)

    def load(tt):
        src, skey = src_ring.nxt()
        ht, hkey = h_ring.nxt()
        S.dma("sp", src[:], srcT[:, tt * 512:(tt + 1) * 512].rearrange("(c p) t -> p c t", p=128), writes=[skey])
        S.dma("sp", ht[:], h_in[tt * 512:(tt + 1) * 512, :].rearrange("(j p) d -> p j d", p=128), writes=[hkey])
        return src, skey, ht, hkey

    nxt = load(0)
    for tt in range(NTT):
        src, skey, ht, hkey = nxt
        if tt + 1 < NTT:
            nxt = load(tt + 1)
        for j in range(4):
            for half in range(2):
                po, pkey = po_ring.nxt()
                for c in range(kc_n):
                    S.op("pe", lambda e: e.matmul(po[:], lhsT=src[:, c, j * 128:(j + 1) * 128],
                                                  rhs=w[:, c, half * 512:(half + 1) * 512], start=(c == 0),
                                                  stop=(c == kc_n - 1)),
                         reads=[skey, ("wres", c)], writes=[pkey], accumulate=(c > 0))
                S.op("dve", lambda e: e.tensor_tensor(ht[:, j, half * 512:(half + 1) * 512],
                                                      ht[:, j, half * 512:(half + 1) * 512], po[:], op=ALU.add),
                     reads=[pkey, hkey], writes=[hkey])
        S.dma("sp", h_out[tt * 512:(tt + 1) * 512, :].rearrange("(j p) d -> p j d", p=128), ht[:], reads=[hkey])


def phase_ffn_up(k, es, layer, h_in, gT):
    nc, S = k.nc, k.S
    sb = lambda n, s, d: es.enter_context(nc.sbuf_tensor(n + k.sfx, s, d))
    ps = lambda n, s, d: es.enter_context(nc.psum_tensor(n + k.sfx, s, d))
    PC, PR = k.PC, k.PR
    wup = sb("wup", [128, 8, 2 * DFF], BF16)
    for cb_ in range(4):
        for kc in range(8):
            S.dma("pool", wup[:, kc, cb_ * 1408:(cb_ + 1) * 1408],
                  k.ffn_up_w[layer, kc * 128:(kc + 1) * 128, cb_ * 1408:(cb_ + 1) * 1408], writes=[("wup", kc, cb_)])
    xt_ring = Ring(sb, "xtf", 2, [128, 4, D], F32)
    hn_ring = Ring(sb, "hnf", 1, [128, 4, D], BF16)
    hnT_ring = Ring(sb, "hnTf", 2, [128, 8, 512], BF16)
    ptr_ring = Ring(ps, "ptrf", 2, [128, 1024], BF16)
    pb_ring = Ring(ps, "pbf", 6, [128, 512], F32)
    junk = sb("junkf", [128, D], BF16)
    small = (sb("ssqf", [128, 4], F32), sb("lnvf", [128, 4], F32), sb("rstdf", [128, 4], F32))
    raw_ring = Ring(sb, "rawf", 6, [128, 514], F32)
    acc_ring = Ring(sb, "accf", 6, [128, 512], F32)
    sg_ring = Ring(sb, "sgf", 2, [128, 512], F32)
    ob_ring = Ring(sb, "obf", 3, [128, 512], BF16)
    halo = sb("halof", [128, 44, 2], F32)
    cwn, cbn, nwn = "fcw%d" % layer, "fcb%d" % layer, "nw%d1" % layer

    def load_x(tt):
        xt, xkey = xt_ring.nxt()
        S.dma("sp", xt[:], h_in[tt * 512:(tt + 1) * 512, :].rearrange("(j p) d -> p j d", p=128), writes=[xkey])
        return xt, xkey

    def prologue(xx):
        xt, xkey = xx
        hnT, hkey = hnT_ring.nxt()
        norm_transpose(k, S, xt, xkey, PR(nwn), hn_ring, hnT, hkey, ptr_ring, junk, small)
        return hnT, hkey

    xs = {0: load_x(0)}
    if NTT > 1:
        xs[1] = load_x(1)
    pro = {0: prologue(xs[0])}
    for tt in range(NTT):
        if tt + 2 < NTT:
            xs[tt + 2] = load_x(tt + 2)
        hnT, hkey = pro[tt]
        tok0 = tt * 512
        seq_start = (tt % 4 == 0)

        def st1(cc):
            pb, pkey = pb_ring.nxt()
            for kc in range(8):
                S.op("pe", lambda e: e.matmul(pb[:], lhsT=wup[:, kc, cc * 128:(cc + 1) * 128], rhs=hnT[:, kc, :],
                                              start=(kc == 0), stop=(kc == 7)),
                     reads=[hkey, ("wup", kc, (cc * 128) // 1408)], writes=[pkey], accumulate=(kc > 0))
            raw, rkey = raw_ring.nxt()
            acc, akey = acc_ring.nxt()
            if seq_start:
                S.op("pool", lambda e: e.memset(raw[:, 0:2], 0.0), writes=[(rkey, "h")])
            else:
                S.op("pool", lambda e: e.tensor_copy(raw[:, 0:2], halo[:, cc, :]), reads=[("halof", cc)],
                     writes=[(rkey, "h")])
            S.op("act", lambda e: e.copy(raw[:, 2:514], pb[:]), reads=[pkey], writes=[rkey])
            S.op("act", lambda e: e.activation(out=acc[:], in_=pb[:], func=AF.Identity, bias=PC(cbn, cc),
                                               scale=PC(cwn, cc * 3 + 2)), reads=[pkey, "pcol"], writes=[akey])
            return (raw, rkey, acc, akey, cc)

        def st2(ta, tb_):
            for tp in range(2):
                for (raw, rkey, acc, akey, cc) in (ta, tb_):
                    S.op("dve", lambda e: e.scalar_tensor_tensor(out=acc[:], in0=raw[:, tp:tp + 512],
                                                                 scalar=PC(cwn, cc * 3 + tp), in1=acc[:], op0=ALU.mult,
                                                                 op1=ALU.add), reads=[rkey, (rkey, "h"), akey, "pcol"],
                         writes=[akey])
            for (raw, rkey, acc, akey, cc) in (ta, tb_):
                S.op("pool", lambda e: e.tensor_copy(halo[:, cc, :], raw[:, 512:514]), reads=[rkey], writes=[("halof", cc)])

        def st3(tg, tv, c):
            ag, agkey = tg[2], tg[3]
            av, avkey = tv[2], tv[3]
            sg, sgkey = sg_ring.nxt()
            S.op("act", lambda e: e.activation(out=sg[:], in_=ag[:], func=AF.Silu), reads=[agkey], writes=[sgkey])
            ob, okey = ob_ring.nxt()
            S.op("pool", lambda e: e.tensor_tensor(ob[:], sg[:], av[:], op=ALU.mult), reads=[sgkey, avkey], writes=[okey])
            S.dma("sp", gT[c * 128:(c + 1) * 128, tok0:tok0 + 512], ob[:], reads=[okey])

        its = {}
        for n in range(22 + 2):
            if n == 11 and tt + 1 < NTT:
                pro[tt + 1] = prologue(xs[tt + 1])
            if n < 22:
                its[n] = (st1(n), st1(22 + n))
            if 0 <= n - 1 < 22:
                st2(its[n - 1][0], its[n - 1][1])
            if 0 <= n - 2 < 22:
                st3(its[n - 2][0], its[n - 2][1], n - 2)


def phase_e1(k, es):
    nc, S = k.nc, k.S
    sb = lambda n, s, d: es.enter_context(nc.sbuf_tensor(n + k.sfx, s, d))
    ps = lambda n, s, d: es.enter_context(nc.psum_tensor(n + k.sfx, s, d))
    PC, PR = k.PC, k.PR
    win = sb("winm", [128, 8, MLC], BF16)
    for kc in range(8):
        S.dma("pool", win[:, kc, :], k.ml_in_w[kc * 128:(kc + 1) * 128, :], writes=[("winm", kc)])
    xt_ring = Ring(sb, "xtm", 2, [128, 4, D], F32)
    hn_ring = Ring(sb, "hnm", 1, [128, 4, D], BF16)
    hnT_ring = Ring(sb, "hnTm", 2, [128, 8, 512], BF16)
    ptr_ring = Ring(ps, "ptrm", 2, [128, 1024], BF16)
    pb_ring = Ring(ps, "pbm", 5, [128, 512], F32)
    junk = sb("junkm", [128, D], BF16)
    small = (sb("ssqm", [128, 4], F32), sb("lnvm", [128, 4], F32), sb("rstdm", [128, 4], F32))
    ob_ring = Ring(sb, "obm", 4, [128, 512], BF16)
    gg = sb("ggm", [128, 4, 16], F32)
    th = sb("thm", [128, 4, 16], F32)
    ef = sb("efm", [128, 4, 8], F32)
    gt_ring = Ring(sb, "gtm", 2, [128, 4, 16], F32)

    def load_x(tt):
        xt, xkey = xt_ring.nxt()
        S.dma("sp", xt[:], k.h2[tt * 512:(tt + 1) * 512, :].rearrange("(j p) d -> p j d", p=128), writes=[xkey])
        return xt, xkey

    def prologue(xx):
        xt, xkey = xx
        hnT, hkey = hnT_ring.nxt()
        norm_transpose(k, S, xt, xkey, PR("nw10"), hn_ring, hnT, hkey, ptr_ring, junk, small)
        return hnT, hkey

    xs = {0: load_x(0)}
    if NTT > 1:
        xs[1] = load_x(1)
    pro = {0: prologue(xs[0])}
    for tt in range(NTT):
        if tt + 2 < NTT:
            xs[tt + 2] = load_x(tt + 2)
        hnT, hkey = pro[tt]
        tok0 = tt * 512

        def proj_fm(col0):
            pb, pkey = pb_ring.nxt()
            for kc in range(8):
                S.op("pe", lambda e: e.matmul(pb[:], lhsT=win[:, kc, col0:col0 + 128], rhs=hnT[:, kc, :],
                                              start=(kc == 0), stop=(kc == 7)),
                     reads=[hkey, ("winm", kc)], writes=[pkey], accumulate=(kc > 0))
            return pb, pkey

        def proj_tm(j, col0, n):
            pb, pkey = pb_ring.nxt()
            for kc in range(8):
                S.op("pe", lambda e: e.matmul(pb[:, 0:n], lhsT=hnT[:, kc, j * 128:(j + 1) * 128],
                                              rhs=win[:, kc, col0:col0 + n], start=(kc == 0), stop=(kc == 7)),
                     reads=[hkey, ("winm", kc)], writes=[pkey], accumulate=(kc > 0))
            return pb, pkey

        for c in range(4):
            pb, pkey = proj_fm(c * 128)
            ob, okey = ob_ring.nxt()
            S.op("act", lambda e: e.mul(ob[:], pb[:], 0.125), reads=[pkey], writes=[okey])
            S.dma("sp", k.mqT[c * 128:(c + 1) * 128, tok0:tok0 + 512], ob[:], reads=[okey])
        for c in range(4):
            pb, pkey = proj_fm(512 + c * 128)
            ob, okey = ob_ring.nxt()
            S.op("dve", lambda e: e.tensor_copy(ob[:], pb[:]), reads=[pkey], writes=[okey])
            S.dma("sp", k.mkT[c * 128:(c + 1) * 128, tok0:tok0 + 512], ob[:], reads=[okey])
        for c in range(8):
            pb, pkey = proj_fm(2048 + c * 128)
            ob, okey = ob_ring.nxt()
            S.op("act", lambda e: e.activation(out=ob[:], in_=pb[:], func=AF.Sigmoid), reads=[pkey], writes=[okey])
            S.dma("sp", k.soT[c * 128:(c + 1) * 128, tok0:tok0 + 512], ob[:], reads=[okey])
        if tt + 1 < NTT:
            pro[tt + 1] = prologue(xs[tt + 1])
        for j in range(4):
            for part in range(3):
                col0 = 512 if part == 0 else 1024 + (part - 1) * 512
                pb, pkey = proj_tm(j, col0, 512)
                ob, okey = ob_ring.nxt()
                if part == 1:
                    S.op("act", lambda e: e.copy(ob[:], pb[:]), reads=[pkey], writes=[okey])
                else:
                    S.op("dve", lambda e: e.tensor_copy(ob[:], pb[:]), reads=[pkey], writes=[okey])
                if part == 0:
                    S.dma("sp", k.mkt[tok0 + j * 128:tok0 + (j + 1) * 128, :], ob[:], reads=[okey])
                else:
                    S.dma("sp", k.mvt[tok0 + j * 128:tok0 + (j + 1) * 128, (part - 1) * 512:part * 512], ob[:],
                          reads=[okey])
        pb, pkey = pb_ring.nxt()
        for j in range(4):
            for kc in range(8):
                S.op("pe", lambda e: e.matmul(pb[:, j * 16:(j + 1) * 16], lhsT=hnT[:, kc, j * 128:(j + 1) * 128],
                                              rhs=win[:, kc, 3072:3088], start=(kc == 0), stop=(kc == 7)),
                     reads=[hkey, ("winm", kc)], writes=[pkey], accumulate=(j + kc > 0))
        gt, gtkey = gt_ring.nxt()
        r0 = PROW["mib"][0]
        S.op("dve", lambda e: e.tensor_tensor(gg[:], pb[:, 0:64].rearrange("p (j h) -> p j h", h=16),
                                              k.prow[:, r0:r0 + 16].unsqueeze(1).to_broadcast([128, 4, 16]), op=ALU.add),
             reads=[pkey, "prow"], writes=["ggm"])
        S.op("act", lambda e: e.activation(out=th[:], in_=gg[:], func=AF.Tanh, scale=1.0 / 15.0), reads=["ggm"],
             writes=["thm"])
        S.op("act", lambda e: e.mul(gt[:, :, 0:8], th[:, :, 0:8], 15.0), reads=["thm"], writes=[gtkey])
        S.op("act", lambda e: e.activation(out=ef[:], in_=th[:, :, 8:16], func=AF.Exp, scale=-15.0), reads=["thm"],
             writes=["efm"])
        S.op("act", lambda e: e.activation(out=ef[:], in_=ef[:], func=AF.Ln, bias=1.0), reads=["efm"], writes=["efm"])
        S.op("act", lambda e: e.mul(gt[:, :, 8:16], ef[:], -1.0), reads=["efm"], writes=[gtkey], accumulate=True)
        S.dma("sp", k.gts[tok0:tok0 + 512, :].rearrange("(j p) h -> p j h", p=128), gt[:], reads=[gtkey])


def phase_f1(k, es):
    nc, S = k.nc, k.S
    sb = lambda n, s, d: es.enter_context(nc.sbuf_tensor(n + k.sfx, s, d))
    ps = lambda n, s, d: es.enter_context(nc.psum_tensor(n + k.sfx, s, d))
    CF, CB, PC = k.CF, k.CB, k.PC
    q_ring = Ring(sb, "mq", 2, [128, 4, 512], BF16)
    k_ring = Ring(sb, "mk", 2, [128, 4, 512], BF16)
    kt_ring = Ring(sb, "mkt", 2, [128, 4, 512], BF16)
    vt_ring = Ring(sb, "mvt", 2, [128, 4, 1024], BF16)
    so_ring = Ring(sb, "mso", 2, [128, 8, 512], BF16)
    gl_ring = Ring(sb, "mgl", 2, [128, 4, 16], F32)
    ho_ring = Ring(sb, "mho", 2, [128, 8, 512], BF16)
    g4 = sb("mg4", [128, 512], F32)
    sm_ring = Ring(sb, "msm", 3, [128, 32], F32)
    cdp_ring = Ring(sb, "mcdp", 3, [128, 4], F32)
    dt_ring = Ring(sb, "mdt", 3, [128, 512], F32)
    eb_ring = Ring(sb, "meb", 3, [128, 512], F32)
    st_ring = Ring(sb, "mst", 3, [128, 512], BF16)
    qe_ring = Ring(sb, "mqe", 3, [128, 512], BF16)
    ad_ring = Ring(sb, "mad", 3, [128, 512], F32)
    hT_ring = Ring(sb, "mhT", 3, [128, 512], F32)
    sq_ring = Ring(sb, "msq", 3, [128, 512], BF16)
    ln_ring = Ring(sb, "mln", 3, [128, 512], F32)
    rs_ring = Ring(sb, "mrs", 3, [128, 512], F32)
    kd_ring = Ring(sb, "mkd", 3, [128, 8, 64], BF16)
    Cf = sb("mCf", [128, 4, 128], F32)
    Cb_ = sb("mCb", [128, 4, 128], BF16)
    nf = sb("mnf", [128, 4], F32)
    nb = sb("mnb", [128, 4], BF16)
    pG_ring = Ring(ps, "mpG", 1, [128, 64], F32)
    pD_ring = Ring(ps, "mpD", 2, [128, 512], F32)
    pkq_ring = Ring(ps, "mpkq", 1, [128, 512], F32)
    pN_ring = Ring(ps, "mpN", 1, [128, 512], F32)
    pden_ring = Ring(ps, "mpden", 1, [128, 512], F32)
    pss_ring = Ring(ps, "mpss", 1, [128, 512], F32)
    pC_ring = Ring(ps, "mpC", 1, [128, 4, 128], F32)

    for q in range(4):
        S.op("pool", lambda e: e.tensor_copy(g4[:, q * 128:(q + 1) * 128], CF("gt")), reads=["cf"], writes=["mg4"],
             accumulate=(q > 0))

    def load(tt):
        tok0 = tt * 512
        r = {}
        for name, ring, src in (("q", q_ring, k.mqT), ("k", k_ring, k.mkT), ("so", so_ring, k.soT)):
            t, key = ring.nxt()
            S.dma("sp", t[:], src[:, tok0:tok0 + 512].rearrange("(c p) t -> p c t", p=128), writes=[key])
            r[name] = (t, key)
        for name, ring, src in (("kt", kt_ring, k.mkt), ("vt", vt_ring, k.mvt), ("gl", gl_ring, k.gts)):
            t, key = ring.nxt()
            S.dma("sp", t[:], src[tok0:tok0 + 512, :].rearrange("(j p) d -> p j d", p=128), writes=[key])
            r[name] = (t, key)
        r["ho"] = ho_ring.nxt()
        return r

    tiles = {}

    def get_tile(tt):
        if tt not in tiles:
            tiles[tt] = load(tt)
        return tiles[tt]

    def P(c):
        cur = get_tile(c["tt"])
        j = c["j"]
        (gl, glkey), (kt, ktkey) = cur["gl"], cur["kt"]
        logi = gl[:, j, 0:8]
        logf = gl[:, j, 8:16]
        pG, pGkey = pG_ring.nxt()
        c["pG"], c["pGkey"] = pG, pGkey
        for i, cname in enumerate(("le", "gt", "ones")):
            S.op("pe", lambda e: e.matmul(pG[:, i * 8:(i + 1) * 8], lhsT=CF(cname), rhs=logf, start=True, stop=True),
                 reads=[glkey, "cf"], writes=[(pGkey, "g")], accumulate=(i > 0))
        sm, smkey = sm_ring.nxt()
        cdp, cdpkey = cdp_ring.nxt()
        c["sm"], c["smkey"], c["cdp"], c["cdpkey"] = sm, smkey, cdp, cdpkey
        S.op("dve", lambda e: e.tensor_tensor(sm[:, 0:8], logi, pG[:, 0:8], op=ALU.subtract),
             reads=[glkey, (pGkey, "g")], writes=[smkey])
        S.op("dve", lambda e: e.tensor_tensor(sm[:, 24:32], logi, pG[:, 8:16], op=ALU.add),
             reads=[glkey, (pGkey, "g")], writes=[smkey], accumulate=True)
        S.op("act", lambda e: e.activation(out=sm[:, 8:16], in_=sm[:, 24:32], func=AF.Exp), reads=[smkey], writes=[smkey])
        S.op("act", lambda e: e.activation(out=cdp[0:64, :], in_=pG[0:64, 16:24:2], func=AF.Exp),
             reads=[(pGkey, "g")], writes=[cdpkey])
        S.op("act", lambda e: e.activation(out=cdp[64:128, :], in_=pG[64:128, 17:24:2], func=AF.Exp),
             reads=[(pGkey, "g")], writes=[cdpkey], accumulate=True)
        kd, kdkey = kd_ring.nxt()
        c["kd"], c["kdkey"] = kd, kdkey
        S.op("pool", lambda e: e.tensor_tensor(kd[:], kt[:, j, :].rearrange("p (h d) -> p h d", d=64),
                                               sm[:, 8:16].unsqueeze(2).to_broadcast([128, 8, 64]), op=ALU.mult),
             reads=[ktkey, smkey], writes=[kdkey])

    def H(c):
        cur = get_tile(c["tt"])
        j = c["j"]
        js = slice(j * 128, (j + 1) * 128)
        (mq, qkey), (mk, kkey), (so, sokey) = cur["q"], cur["k"], cur["so"]
        (kt, ktkey), (vt, vtkey), (gl, glkey) = cur["kt"], cur["vt"], cur["gl"]
        ho, hokey = cur["ho"]
        sm, smkey, kd, kdkey, pG, pGkey = c["sm"], c["smkey"], c["kd"], c["kdkey"], c["pG"], c["pGkey"]
        if c["tt"] % 4 == 0 and j == 0:
            S.op("pool", lambda e: e.memset(Cf[:], 0.0), writes=["mCf"])
            S.op("pool", lambda e: e.memset(Cb_[:], 0.0), writes=["mCb"])
            S.op("pool", lambda e: e.memset(nf[:], 0.0), writes=["mnf"])
            S.op("pool", lambda e: e.memset(nb[:], 0.0), writes=["mnb"])
        pC, pCkey = pC_ring.nxt()
        c["pC"], c["pCkey"] = pC, pCkey
        CS = lambda hh: slice(hh * 128, (hh + 1) * 128)

        def front(hq):
            pDm, pDmkey = pD_ring.nxt()
            pDu, pDukey = pD_ring.nxt()
            pkq, pkqkey = pkq_ring.nxt()
            pN, pNkey = pN_ring.nxt()
            pden, pdenkey = pden_ring.nxt()
            hs = [hq * 4 + hh for hh in range(4)]
            S.op("pe", lambda e: e.matmul(pDm[:], lhsT=CF("negbig"), rhs=g4[:], start=True, stop=False),
                 reads=["cf", "mg4"], writes=[pDmkey])
            for hh, h in enumerate(hs):
                po, pr = (h % 2) * 64, h // 2
                lb = gl[:, j, 8 + h:9 + h].to_broadcast([128, 128])
                S.op("pe", lambda e: e.matmul(pDm[:, CS(hh)], lhsT=lb, rhs=CF("le"), start=False, stop=True),
                     reads=[glkey, "cf"], writes=[pDmkey], accumulate=True)
                S.op("pe", lambda e: e.matmul(pDu[:, CS(hh)], lhsT=lb, rhs=CF("le"), start=True, stop=True),
                     reads=[glkey, "cf"], writes=[pDukey], accumulate=(hh > 0))
                S.op("pe", lambda e: e.matmul(pkq[:, CS(hh)], lhsT=mk[po:po + 64, pr, js], rhs=mq[po:po + 64, pr, js],
                                              start=True, stop=True),
                     reads=[qkey, kkey], writes=[pkqkey], accumulate=(hh > 0))
            eb, ebkey = eb_ring.nxt()
            S.op("act", lambda e: e.activation(out=eb[:], in_=pDu[:], func=AF.Exp), reads=[pDukey], writes=[ebkey])
            dtl, dtkey = dt_ring.nxt()
            for hh, h in enumerate(hs):
                S.op("act", lambda e: e.activation(out=dtl[:, CS(hh)], in_=pDm[:, CS(hh)], func=AF.Exp,
                                                   bias=sm[:, h:h + 1]),
                     reads=[pDmkey, smkey], writes=[dtkey], accumulate=(hh > 0))
            st, stkey = st_ring.nxt()
            S.op("dve", lambda e: e.tensor_tensor(st[:], dtl[:], pkq[:], op=ALU.mult), reads=[dtkey, pkqkey],
                 writes=[stkey])
            qe, qekey = qe_ring.nxt()
            for hh, h in enumerate(hs):
                po, pr = (h % 2) * 64, h // 2
                S.op("pool", lambda e: e.tensor_tensor(qe[po:po + 64, CS(hh)], mq[po:po + 64, pr, js],
                                                       eb[po:po + 64, CS(hh)], op=ALU.mult),
                     reads=[qkey, ebkey], writes=[qekey], accumulate=(hh > 0))
            for hh, h in enumerate(hs):
                po, pr = (h % 2) * 64, h // 2
                S.op("pe", lambda e: e.matmul(pN[:, CS(hh)], lhsT=vt[:, j, h * 128:(h + 1) * 128], rhs=st[:, CS(hh)],
                                              start=True, stop=False), reads=[vtkey, stkey], writes=[pNkey],
                     accumulate=(hh > 0))
                S.op("pe", lambda e: e.matmul(pN[:, CS(hh)], lhsT=Cb_[po:po + 64, pr, :], rhs=qe[po:po + 64, CS(hh)],
                                              start=False, stop=True), reads=["mCb", qekey], writes=[pNkey],
                     accumulate=True)
                S.op("pe", lambda e: e.matmul(pden[:, CS(hh)], lhsT=CB("ones"), rhs=st[:, CS(hh)], start=True, stop=False),
                     reads=["cb", stkey], writes=[pdenkey], accumulate=(hh > 0))
                S.op("pe", lambda e: e.matmul(pden[:, CS(hh)], lhsT=nb[po:po + 64, pr:pr + 1].to_broadcast([64, 128]),
                                              rhs=qe[po:po + 64, CS(hh)], start=False, stop=True),
                     reads=["mnb", qekey], writes=[pdenkey], accumulate=True)
            for hh, h in enumerate(hs):
                po, pr = (h % 2) * 64, h // 2
                S.op("pe", lambda e: e.matmul(pC[po:po + 64, pr, :], lhsT=kd[:, h, :], rhs=vt[:, j, h * 128:(h + 1) * 128],
                                              start=True, stop=True), reads=[kdkey, vtkey], writes=[pCkey],
                     accumulate=(h > 0))
                S.op("pe", lambda e: e.matmul(pG[po:po + 64, 32 + pr:33 + pr], lhsT=kd[:, h, :], rhs=CB("ones")[:, 0:1],
                                              start=True, stop=True), reads=[kdkey, "cb"], writes=[(pGkey, "n")],
                     accumulate=(h > 0))
            return {"hq": hq, "hs": hs, "pN": pN, "pNkey": pNkey, "pden": pden, "pdenkey": pdenkey}

        def tailA(g):
            pN, pNkey, pden, pdenkey = g["pN"], g["pNkey"], g["pden"], g["pdenkey"]
            ad, adkey = ad_ring.nxt()
            S.op("act", lambda e: e.activation(out=ad[:], in_=pden[:], func=AF.Abs), reads=[pdenkey], writes=[adkey])
            S.op("dve", lambda e: e.tensor_scalar(ad[:], ad[:], 1.0, None, op0=ALU.max), reads=[adkey], writes=[adkey])
            S.op("dve", lambda e: e.reciprocal(ad[:], ad[:]), reads=[adkey], writes=[adkey])
            hT, hTkey = hT_ring.nxt()
            S.op("dve", lambda e: e.tensor_tensor(hT[:], pN[:], ad[:], op=ALU.mult), reads=[pNkey, adkey], writes=[hTkey])
            sq, sqkey = sq_ring.nxt()
            S.op("act", lambda e: e.activation(out=sq[:], in_=hT[:], func=AF.Square), reads=[hTkey], writes=[sqkey])
            g.update(hT=hT, hTkey=hTkey, sq=sq, sqkey=sqkey)

        def tailB(g):
            hq, hs, hT, hTkey, sq, sqkey = g["hq"], g["hs"], g["hT"], g["hTkey"], g["sq"], g["sqkey"]
            pss, psskey = pss_ring.nxt()
            S.op("pe", lambda e: e.matmul(pss[:], lhsT=CB("ones"), rhs=sq[:], start=True, stop=True),
                 reads=["cb", sqkey], writes=[psskey])
            lnb, lnkey = ln_ring.nxt()
            rs, rskey = rs_ring.nxt()
            S.op("act", lambda e: e.activation(out=lnb[:], in_=pss[:], func=AF.Ln, bias=EPS, scale=1.0 / 128),
                 reads=[psskey], writes=[lnkey])
            S.op("act", lambda e: e.activation(out=rs[:], in_=lnb[:], func=AF.Exp, scale=-0.5), reads=[lnkey],
                 writes=[rskey])
            for hh, h in enumerate(hs):
                S.op("dve", lambda e: e.scalar_tensor_tensor(out=hT[:, CS(hh)], in0=hT[:, CS(hh)], scalar=PC("mnw", h),
                                                             in1=rs[:, CS(hh)], op0=ALU.mult, op1=ALU.mult),
                     reads=[hTkey, rskey, "pcol"], writes=[hTkey])
            S.op("pool", lambda e: e.tensor_tensor(ho[:, hq * 4:(hq + 1) * 4, js], hT[:].rearrange("p (h t) -> p h t", t=128),
                                                   so[:, hq * 4:(hq + 1) * 4, js], op=ALU.mult),
                 reads=[hTkey, sokey], writes=[hokey], accumulate=(j + hq > 0))

        g0 = front(0)
        tailA(g0)
        g1 = front(1)
        tailB(g0)
        tailA(g1)
        tailB(g1)

    def E(c):
        cdp, cdpkey, pC, pCkey, pG, pGkey = c["cdp"], c["cdpkey"], c["pC"], c["pCkey"], c["pG"], c["pGkey"]
        cdb = cdp[:].unsqueeze(2).to_broadcast([128, 4, 128])
        S.op("dve", lambda e: e.tensor_tensor(Cf[:], Cf[:], cdb, op=ALU.mult), reads=["mCf", cdpkey], writes=["mCf"])
        S.op("dve", lambda e: e.tensor_tensor(Cf[:], Cf[:], pC[:], op=ALU.add), reads=["mCf", pCkey], writes=["mCf"])
        S.op("act", lambda e: e.copy(Cb_[:], Cf[:]), reads=["mCf"], writes=["mCb"])
        S.op("dve", lambda e: e.tensor_tensor(nf[:], nf[:], cdp[:], op=ALU.mult), reads=["mnf", cdpkey], writes=["mnf"])
        S.op("dve", lambda e: e.tensor_tensor(nf[:], nf[:], pG[:, 32:36], op=ALU.add), reads=["mnf", (pGkey, "n")],
             writes=["mnf"])
        S.op("act", lambda e: e.copy(nb[:], nf[:]), reads=["mnf"], writes=["mnb"])
        if c["j"] == 3:
            tok0 = c["tt"] * 512
            ho, hokey = get_tile(c["tt"])["ho"]
            S.dma("sp", k.hmT[:, tok0:tok0 + 512].rearrange("(c p) t -> p c t", p=128), ho[:], reads=[hokey])

    chunks = [{"tt": tt, "j": j} for tt in range(NTT) for j in range(4)]
    get_tile(0)
    P(chunks[0])
    for ci, c in enumerate(chunks):
        if c["j"] == 0 and c["tt"] + 1 < NTT:
            get_tile(c["tt"] + 1)
        H(c)
        if ci + 1 < len(chunks):
            P(chunks[ci + 1])
        E(c)


_NC_CACHE = {}


def kernel(**inputs):
    inp = {k_: np.asarray(v) for k_, v in inputs.items()}
    if "nc" not in _NC_CACHE:
        _NC_CACHE["nc"] = build()
    nc = _NC_CACHE["nc"]
    pc, pr = pack_params(inp)
    consts = make_consts()
    x = np.ascontiguousarray(inp["x"], dtype=np.float32)
    shared = {
        "pcol": pc, "prow": pr, "consts": consts,
        "hy_in_w": np.ascontiguousarray(inp["hy_in_w"][0], dtype=np.float32),
        "hy_out_w": np.ascontiguousarray(inp["hy_out_w"][0], dtype=np.float32),
        "ml_in_w": np.ascontiguousarray(inp["ml_in_w"][0], dtype=np.float32),
        "ml_out_w": np.ascontiguousarray(inp["ml_out_w"][0], dtype=np.float32),
        "ffn_up_w": np.ascontiguousarray(inp["ffn_up_w"], dtype=np.float32),
        "ffn_down_w": np.ascontiguousarray(inp["ffn_down_w"], dtype=np.float32),
    }
    in_maps = []
    for c in range(NCORES):
        m = dict(shared)
        m["x"] = np.ascontiguousarray(x[2 * c:2 * c + 2].reshape(NTOK, D))
        in_maps.append(m)
    res = run_bass_kernel_spmd(nc, in_maps, core_ids=list(range(NCORES)))
    out = np.concatenate([np.asarray(r["out"]).reshape(2, L, D) for r in res.results], axis=0)
    return out.astype(np.float32)
```

```python
import numpy as np
from contextlib import ExitStack
import concourse.bass as bass
import concourse.mybir as mybir
from concourse.bass_utils import run_bass_kernel_spmd

F32 = mybir.dt.float32
BF16 = mybir.dt.bfloat16
AF = mybir.ActivationFunctionType
ALU = mybir.AluOpType

NCORES = 8
NTOK = 4096
L = 2048
D = 1024
EPS = 1e-6
HYC = 5648
MLC = 3088
DFF = 2816
NTT = 8
DBGPRINT = False
OPLIMIT = None

ENGS = ("pe", "act", "dve", "pool", "sp")
NDSEM = 8


class Ins:
    __slots__ = ("eng", "fn", "deps", "is_dma", "needed", "ticket", "dslot", "dval", "waits", "q_n", "seq")

    def __init__(self, eng, fn, is_dma):
        self.eng = eng
        self.fn = fn
        self.is_dma = is_dma
        self.deps = []
        self.needed = False
        self.ticket = 0
        self.waits = []


class _Rec:
    def __getattr__(self, name):
        def f(*a, **kw):
            self.call = (name, a, kw)
        return f


class Sched:
    def __init__(self, nc, es):
        self.nc = nc
        self.sems = {}
        for e in ENGS:
            self.sems[("c", e)] = es.enter_context(nc.semaphore("c_" + e))
            for s in range(NDSEM):
                self.sems[("d", e, s)] = es.enter_context(nc.semaphore("d_%s_%d" % (e, s)))
        self.cnt = {e: 0 for e in ENGS}
        self.ndma = {e: 0 for e in ENGS}
        self.dma_hist = {e: [] for e in ENGS}
        self.hw = {e: {} for e in ENGS}
        self.nseq = 0
        self.limit = OPLIMIT
        self._reset()

    def _reset(self):
        self.streams = {e: [] for e in ENGS}
        self.all = []
        self.state = {}

    def _rec(self, ins, reads, writes, accumulate=False):
        if self.limit is not None and self.nseq >= self.limit:
            ins.deps = []
            ins.seq = self.nseq
            self.nseq += 1
            return ins
        deps = []
        for k in reads:
            st = self.state.get(k)
            if st:
                deps.extend(st[0])
        for k in writes:
            st = self.state.get(k)
            if st:
                deps.extend(st[1])
                deps.extend(st[0])
        out = []
        seen = set()
        for d in deps:
            if d is ins or id(d) in seen:
                continue
            seen.add(id(d))
            if (not d.is_dma) and (not ins.is_dma) and d.eng == ins.eng == "pe":
                continue
            out.append(d)
        last = {}
        red = []
        for d in out:
            if d.is_dma:
                red.append(d)
            elif d.eng not in last or d.seq > last[d.eng].seq:
                last[d.eng] = d
        red.extend(last.values())
        ins.deps = red
        ins.seq = self.nseq
        self.nseq += 1
        for k in reads:
            st = self.state.setdefault(k, [[], []])
            st[1].append(ins)
        for k in writes:
            st = self.state.setdefault(k, [[], []])
            if accumulate:
                st[0].append(ins)
            else:
                st[0] = [ins]
            st[1] = []
        self.all.append(ins)
        self.streams[ins.eng].append(ins)
        return ins

    def op(self, eng, fn, reads=(), writes=(), accumulate=False):
        rec = _Rec()
        fn(rec)
        name, a, kw = rec.call
        real = (lambda e, name=name, a=a, kw=kw: getattr(e, name)(*a, **kw))
        return self._rec(Ins(eng, real, False), list(reads), list(writes), accumulate)

    def dma(self, q, out, in_, reads=(), writes=()):
        ins = Ins(q, (lambda e, out=out, in_=in_: e.dma_start(out=out, in_=in_)), True)
        n = self.ndma[q]
        self.ndma[q] = n + 1
        ins.q_n = n
        ins.dslot = n % NDSEM
        ins.dval = 16 * (n // NDSEM + 1)
        hist = self.dma_hist[q]
        if self.limit is not None and self.nseq >= self.limit:
            self.ndma[q] = n
            self.nseq += 1
            return ins
        self._rec(ins, list(reads), list(writes))
        if n >= NDSEM:
            ins.deps.append(hist[n - NDSEM])
        hist.append(ins)
        return ins

    def flush(self):
        nc = self.nc
        for ins in self.all:
            for d in ins.deps:
                d.needed = True
        tails = []
        for e in ENGS:
            for ins in reversed(self.streams[e]):
                if not ins.is_dma:
                    ins.needed = True
                    tails.append(ins)
                    break
            hist = self.dma_hist[e]
            for ins in hist[max(0, len(hist) - NDSEM):]:
                tails.append(ins)
        for ins in self.all:
            if not ins.is_dma and ins.needed:
                self.cnt[ins.eng] += 1
                ins.ticket = self.cnt[ins.eng]
        hw = self.hw

        def key_val(d):
            if d.is_dma:
                return ("d", d.eng, d.dslot), d.dval
            return ("c", d.eng), d.ticket

        for ins in self.all:
            for d in ins.deps:
                key, val = key_val(d)
                if hw[ins.eng].get(key, 0) >= val:
                    continue
                hw[ins.eng][key] = val
                ins.waits.append((key, val))
        final = {e: [] for e in ENGS}
        for e in ENGS:
            for d in tails:
                key, val = key_val(d)
                if key == ("c", e) or hw[e].get(key, 0) >= val:
                    continue
                hw[e][key] = val
                final[e].append((key, val))
        sems = self.sems
        streams = self.streams

        def mk(ename):
            def body(eng):
                for ins in streams[ename]:
                    for key, val in ins.waits:
                        eng.wait_ge(sems[key], val)
                    r = ins.fn(eng)
                    if ins.is_dma:
                        r.then_inc(sems[("d", ename, ins.dslot)], 16)
                    elif ins.needed:
                        r.then_inc(sems[("c", ename)], 1)
                for key, val in final[ename]:
                    eng.wait_ge(sems[key], val)
            return body

        with nc.Block() as block:
            block.tensor(mk("pe"))
            block.scalar(mk("act"))
            block.vector(mk("dve"))
            block.gpsimd(mk("pool"))
            block.sync(mk("sp"))
        self._reset()


class Ring:
    def __init__(self, alloc, name, n, shape, dtype):
        self.tiles = [alloc("%s%d" % (name, i), shape, dtype) for i in range(n)]
        self.name = name
        self.i = 0

    def nxt(self):
        i = self.i % len(self.tiles)
        self.i += 1
        return self.tiles[i], (self.name, i)


def _col(vec):
    v = np.asarray(vec, np.float32).reshape(-1, 128)
    return np.ascontiguousarray(v.T)


PCOL = {}
PROW = {}


def _layout_tables():
    c = 0
    for name, n in (("scw", 48), ("scb", 12), ("sdd", 8), ("snw", 8), ("qw", 1), ("kw", 1),
                    ("fcw0", 132), ("fcb0", 44), ("fcw1", 132), ("fcb1", 44), ("mnw", 8)):
        PCOL[name] = (c, n)
        c += n
    PCOL["_n"] = c
    r = 0
    for name, n in (("nw00", 1024), ("nw01", 1024), ("nw10", 1024), ("nw11", 1024),
                    ("dtb", 16), ("alog", 16), ("mib", 8), ("mfb", 8)):
        PROW[name] = (r, n)
        r += n
    PROW["_n"] = r


_layout_tables()


def pack_params(inp):
    pc = np.zeros((128, PCOL["_n"]), np.float32)

    def put(name, arr):
        c0, n = PCOL[name]
        assert arr.shape == (128, n), (name, arr.shape)
        pc[:, c0:c0 + n] = arr

    cw = inp["ssd_conv_w"][0]
    put("scw", np.stack([_col(cw[k]) for k in range(4)], axis=2).reshape(128, 48))
    put("scb", _col(inp["ssd_conv_b"][0]))
    put("sdd", _col(np.repeat(inp["ssd_d"][0], 64)))
    put("snw", _col(inp["ssd_norm_w"][0]))
    put("qw", _col(np.tile(inp["sb_q_norm_w"][0], 2)))
    put("kw", _col(np.tile(inp["sb_k_norm_w"][0], 2)))
    for l in range(2):
        fw = inp["ffn_conv_w"][l]
        put("fcw%d" % l, np.stack([_col(fw[k]) for k in range(3)], axis=2).reshape(128, 132))
        put("fcb%d" % l, _col(inp["ffn_conv_b"][l]))
    put("mnw", _col(inp["ml_norm_w"][0]))
    pr = np.zeros((PROW["_n"],), np.float32)

    def putr(name, v):
        r0, n = PROW[name]
        pr[r0:r0 + n] = np.asarray(v, np.float32).reshape(n)

    putr("nw00", inp["norm_w"][0, 0])
    putr("nw01", inp["norm_w"][0, 1])
    putr("nw10", inp["norm_w"][1, 0])
    putr("nw11", inp["norm_w"][1, 1])
    putr("dtb", inp["ssd_dt_bias"][0])
    putr("alog", inp["ssd_a_log"][0])
    putr("mib", inp["ml_i_b"][0])
    putr("mfb", inp["ml_f_b"][0])
    prow = np.ascontiguousarray(np.broadcast_to(pr[None, :], (128, pr.size)))
    return pc, prow


def make_consts():
    i = np.arange(128)
    eye = (i[:, None] == i[None, :]).astype(np.float32)
    le = (i[:, None] <= i[None, :]).astype(np.float32)
    gt = (i[:, None] > i[None, :]).astype(np.float32)
    ge = (i[:, None] >= i[None, :]).astype(np.float32)
    ones = np.ones((128, 128), np.float32)
    bd = ((i[:, None] // 64) == (i[None, :] // 64)).astype(np.float32)
    negbig = -30000.0 * eye
    negu = -ge
    return np.ascontiguousarray(np.concatenate([eye, le, gt, ge, ones, bd, negbig, negu], axis=1))


CI = {"eye": 0, "le": 1, "gt": 2, "ge": 3, "ones": 4, "bd": 5, "negbig": 6, "negu": 7}


class K:
    pass


def build(nphase=99, dbg=False):
    nc = bass.Bass("TRN2", target_bir_lowering=False)
    k = K()
    k.nc = nc
    k.sfx = ""
    ein = lambda n, s, d=F32: nc.dram_tensor(n, s, d, kind="ExternalInput").ap()
    skind = "ExternalOutput" if dbg else "Internal"
    scr = lambda n, s, d: nc.dram_tensor(n, s, d, kind=skind).ap()
    k.x = ein("x", [NTOK, D])
    k.pcol_d = ein("pcol", [128, PCOL["_n"]])
    k.prow_d = ein("prow", [128, PROW["_n"]])
    k.consts_d = ein("consts", [128, 8 * 128])
    k.hy_in_w = ein("hy_in_w", [D, HYC])
    k.hy_out_w = ein("hy_out_w", [2048, D])
    k.ml_in_w = ein("ml_in_w", [D, MLC])
    k.ml_out_w = ein("ml_out_w", [D, D])
    k.ffn_up_w = ein("ffn_up_w", [2, D, 2 * DFF])
    k.ffn_down_w = ein("ffn_down_w", [2, DFF, D])
    k.out = nc.dram_tensor("out", [NTOK, D], F32, kind="ExternalOutput").ap()
    k.szT = scr("szT", [1024, NTOK], BF16)
    k.xbcT = scr("xbcT", [1536, NTOK], BF16)
    k.dts = scr("dts", [NTOK, 16], F32)
    k.qT = scr("qT", [1024, NTOK], BF16)
    k.kT = scr("kT", [1024, NTOK], BF16)
    k.vtok = scr("vtok", [NTOK, 1024], BF16)
    k.ycatT = scr("ycatT", [2048, NTOK], BF16)
    k.h1 = scr("h1", [NTOK, D], F32)
    k.h2 = scr("h2", [NTOK, D], F32)
    k.h3 = scr("h3", [NTOK, D], F32)
    k.gT = scr("gT", [DFF, NTOK], BF16)
    k.mqT = scr("mqT", [512, NTOK], BF16)
    k.mkT = scr("mkT", [512, NTOK], BF16)
    k.soT = scr("soT", [1024, NTOK], BF16)
    k.mkt = scr("mkt", [NTOK, 512], BF16)
    k.mvt = scr("mvt", [NTOK, 1024], BF16)
    k.gts = scr("gts", [NTOK, 16], F32)
    k.hmT = scr("hmT", [1024, NTOK], BF16)

    with ExitStack() as es:
        S = Sched(nc, es)
        k.S = S
        sb = lambda n, s, d: es.enter_context(nc.sbuf_tensor(n, s, d))
        k.cf = sb("cf", [128, 8 * 128], F32)
        k.cb = sb("cb", [128, 8 * 128], BF16)
        k.pcol = sb("pcolt", [128, PCOL["_n"]], F32)
        k.prow = sb("prowt", [128, PROW["_n"]], F32)
        S.dma("sp", k.cf[:], k.consts_d, writes=["cf"])
        S.dma("pool", k.cb[:], k.consts_d, writes=["cb"])
        S.dma("sp", k.pcol[:], k.pcol_d, writes=["pcol"])
        S.dma("sp", k.prow[:], k.prow_d, writes=["prow"])
        k.CF = lambda name: k.cf[:, CI[name] * 128:(CI[name] + 1) * 128]
        k.CB = lambda name: k.cb[:, CI[name] * 128:(CI[name] + 1) * 128]
        k.PC = lambda name, i=0, n=1: k.pcol[:, PCOL[name][0] + i:PCOL[name][0] + i + n]
        k.PR = lambda name: k.prow[:, PROW[name][0]:PROW[name][0] + PROW[name][1]]

        phases = [phase_a0, phase_b0, phase_c0,
                  lambda k, pes: phase_proj_res(k, pes, k.hy_out_w, 16, k.ycatT, k.x, k.h1),
                  lambda k, pes: phase_ffn_up(k, pes, 0, k.h1, k.gT),
                  lambda k, pes: phase_proj_res(k, pes, k.ffn_down_w[0], 22, k.gT, k.h1, k.h2),
                  phase_e1, phase_f1,
                  lambda k, pes: phase_proj_res(k, pes, k.ml_out_w, 8, k.hmT, k.h2, k.h3),
                  lambda k, pes: phase_ffn_up(k, pes, 1, k.h3, k.gT),
                  lambda k, pes: phase_proj_res(k, pes, k.ffn_down_w[1], 22, k.gT, k.h3, k.out)]
        for pi, ph in enumerate(phases[:nphase]):
            k.sfx = "_p%d" % pi
            with ExitStack() as pes:
                ph(k, pes)
                S.flush()
    return nc


def norm_transpose(k, S, xt, xkey, nw, hn_ring, hnT, hnT_key, ptr_ring, junk, small):
    ssq, lnv, rstd = small
    for j in range(4):
        S.op("act", lambda e, j=j: e.activation(out=junk[:], in_=xt[:, j, :], func=AF.Square,
                                                accum_out=ssq[:, j:j + 1]),
             reads=[xkey], writes=["junk", "ssq"])
    S.op("act", lambda e: e.activation(out=lnv[:], in_=ssq[:], func=AF.Ln, bias=EPS, scale=1.0 / D),
         reads=["ssq"], writes=["lnv"])
    S.op("act", lambda e: e.activation(out=rstd[:], in_=lnv[:], func=AF.Exp, scale=-0.5),
         reads=["lnv"], writes=["rstd"])
    hn, hkey = hn_ring.nxt()
    for j in range(4):
        S.op("dve", lambda e, j=j: e.scalar_tensor_tensor(out=hn[:, j, :], in0=xt[:, j, :], scalar=rstd[:, j:j + 1],
                                                          in1=nw, op0=ALU.mult, op1=ALU.mult),
             reads=[xkey, "rstd", "prow"], writes=[(hkey, j)])
    for j in range(4):
        ptr, pkey = ptr_ring.nxt()
        for kc in range(8):
            S.op("pe", lambda e, j=j, kc=kc, ptr=ptr: e.transpose(out=ptr[:, kc * 128:(kc + 1) * 128],
                                                                  in_=hn[:, j, kc * 128:(kc + 1) * 128],
                                                                  identity=k.CB("eye")),
                 reads=[(hkey, j), "cb"], writes=[pkey], accumulate=(kc > 0))
        eng = "act" if j % 2 == 0 else "dve"
        if eng == "act":
            S.op("act", lambda e, j=j, ptr=ptr: e.copy(hnT[:, :, j * 128:(j + 1) * 128],
                                                       ptr[:].rearrange("p (c t) -> p c t", t=128)),
                 reads=[pkey], writes=[hnT_key])
        else:
            S.op("dve", lambda e, j=j, ptr=ptr: e.tensor_copy(hnT[:, :, j * 128:(j + 1) * 128],
                                                              ptr[:].rearrange("p (c t) -> p c t", t=128)),
                 reads=[pkey], writes=[hnT_key])


def phase_a0(k, es):
    nc, S = k.nc, k.S
    sb = lambda n, s, d: es.enter_context(nc.sbuf_tensor(n + k.sfx, s, d))
    ps = lambda n, s, d: es.enter_context(nc.psum_tensor(n + k.sfx, s, d))
    win = sb("win", [128, 8, HYC], BF16)
    BW = 706

    def wk(kc, c0, c1):
        return [("win", kc, b) for b in range(c0 // BW, (c1 - 1) // BW + 1)]

    for b in range(8):
        for kc in range(8):
            S.dma("pool", win[:, kc, b * BW:(b + 1) * BW], k.hy_in_w[kc * 128:(kc + 1) * 128, b * BW:(b + 1) * BW],
                  writes=[("win", kc, b)])
    xt_ring = Ring(sb, "xt", 2, [128, 4, D], F32)
    hn_ring = Ring(sb, "hn", 1, [128, 4, D], BF16)
    hnT_ring = Ring(sb, "hnT", 2, [128, 8, 512], BF16)
    ptr_ring = Ring(ps, "ptr", 1, [128, 1024], BF16)
    pb_ring = Ring(ps, "pb", 5, [128, 512], F32)
    pss_ring = Ring(ps, "pss", 2, [128, 512], F32)
    junk = sb("junk", [128, D], BF16)
    small = (sb("ssq", [128, 4], F32), sb("lnv", [128, 4], F32), sb("rstd", [128, 4], F32))
    ob_ring = Ring(sb, "ob", 4, [128, 512], BF16)
    raw_ring = Ring(sb, "raw", 3, [128, 515], F32)
    acc_ring = Ring(sb, "acc", 4, [128, 512], F32)
    sq_ring = Ring(sb, "sqb", 3, [128, 512], BF16)
    ta_ring = Ring(sb, "ta", 2, [128, 512], F32)
    tb_ring = Ring(sb, "tb", 3, [128, 512], F32)
    halo = sb("halo", [128, 12, 3], F32)
    qws = sb("qws", [128, 1], F32)
    dtt_ring = Ring(sb, "dtt", 2, [128, 4, 16], F32)
    dte = sb("dte", [128, 64], F32)
    S.op("act", lambda e: e.mul(qws[:], k.PC("qw"), 0.125), reads=["pcol"], writes=["qws"])

    def load_x(tt):
        xt, xkey = xt_ring.nxt()
        S.dma("sp", xt[:], k.x[tt * 512:(tt + 1) * 512, :].rearrange("(j p) d -> p j d", p=128), writes=[xkey])
        return xt, xkey

    def prologue(xx):
        xt, xkey = xx
        hnT, hkey = hnT_ring.nxt()
        norm_transpose(k, S, xt, xkey, k.PR("nw00"), hn_ring, hnT, hkey, ptr_ring, junk, small)
        return hnT, hkey

    xs = {0: load_x(0)}
    if NTT > 1:
        xs[1] = load_x(1)
    pro = {0: prologue(xs[0])}
    for tt in range(NTT):
        if tt + 2 < NTT:
            xs[tt + 2] = load_x(tt + 2)
        hnT, hkey = pro[tt]
        tok0 = tt * 512
        seq_start = (tt % 4 == 0)

        def proj_fm(col0):
            pb, pkey = pb_ring.nxt()
            for kc in range(8):
                S.op("pe", lambda e, kc=kc, pb=pb: e.matmul(pb[:], lhsT=win[:, kc, col0:col0 + 128], rhs=hnT[:, kc, :],
                                                            start=(kc == 0), stop=(kc == 7)),
                     reads=[hkey] + wk(kc, col0, col0 + 128), writes=[pkey], accumulate=(kc > 0))
            return pb, pkey

        for c in range(8):
            pb, pkey = proj_fm(c * 128)
            ob, okey = ob_ring.nxt()
            S.op("act", lambda e, pb=pb, ob=ob: e.activation(out=ob[:], in_=pb[:], func=AF.Silu),
                 reads=[pkey], writes=[okey])
            S.dma("sp", k.szT[c * 128:(c + 1) * 128, tok0:tok0 + 512], ob[:], reads=[okey])
        if DBGPRINT: print('a0 after z', S.nseq)
        def x1(c):
            pb, pkey = proj_fm(1024 + c * 128)
            raw, rkey = raw_ring.nxt()
            acc, akey = acc_ring.nxt()
            if seq_start:
                S.op("pool", lambda e: e.memset(raw[:, 0:3], 0.0), writes=[(rkey, "h")])
            else:
                S.op("pool", lambda e: e.tensor_copy(raw[:, 0:3], halo[:, c, :]), reads=[("halo", c)],
                     writes=[(rkey, "h")])
            S.op("act", lambda e: e.copy(raw[:, 3:515], pb[:]), reads=[pkey], writes=[rkey])
            S.op("act", lambda e: e.activation(out=acc[:], in_=pb[:], func=AF.Identity, bias=k.PC("scb", c),
                                               scale=k.PC("scw", c * 4 + 3)), reads=[pkey, "pcol"], writes=[akey])
            return (raw, rkey, acc, akey, c)

        def xtap(t, tp):
            raw, rkey, acc, akey, c = t
            S.op("dve", lambda e: e.scalar_tensor_tensor(out=acc[:], in0=raw[:, tp:tp + 512],
                                                         scalar=k.PC("scw", c * 4 + tp), in1=acc[:], op0=ALU.mult,
                                                         op1=ALU.add), reads=[rkey, (rkey, "h"), akey, "pcol"], writes=[akey])

        def xhalo(t):
            raw, rkey, acc, akey, c = t
            S.op("pool", lambda e: e.tensor_copy(halo[:, c, :], raw[:, 512:515]), reads=[rkey], writes=[("halo", c)])

        def x3(t):
            raw, rkey, acc, akey, c = t
            ob, okey = ob_ring.nxt()
            S.op("act", lambda e: e.activation(out=ob[:], in_=acc[:], func=AF.Silu), reads=[akey], writes=[okey])
            S.dma("sp", k.xbcT[c * 128:(c + 1) * 128, tok0:tok0 + 512], ob[:], reads=[okey])

        its = {}
        for n in range(12 + 3):
            if n < 12:
                its[n] = x1(n)
            a_ok, b_ok = 0 <= n - 1 < 12, 0 <= n - 2 < 12
            if a_ok:
                xtap(its[n - 1], 0)
            if b_ok:
                xtap(its[n - 2], 2)
                xhalo(its[n - 2])
            if a_ok:
                xtap(its[n - 1], 1)
            if 0 <= n - 3 < 12:
                x3(its[n - 3])
        if tt + 1 < NTT:
            pro[tt + 1] = prologue(xs[tt + 1])

        def q1(n):
            which, c = n // 8, n % 8
            pb, pkey = proj_fm((2576 if which == 0 else 3600) + c * 128)
            sq, sqkey = sq_ring.nxt()
            S.op("act", lambda e: e.activation(out=sq[:], in_=pb[:], func=AF.Square), reads=[pkey], writes=[sqkey])
            return {"pb": pb, "pkey": pkey, "sq": sq, "sqkey": sqkey, "which": which, "c": c}

        def q2(t):
            pss, psskey = pss_ring.nxt()
            sq, sqkey = t["sq"], t["sqkey"]
            S.op("pe", lambda e: e.matmul(pss[:], lhsT=k.CB("bd"), rhs=sq[:], start=True, stop=True),
                 reads=[sqkey, "cb"], writes=[psskey])
            ta, takey = ta_ring.nxt()
            tb, tbkey = tb_ring.nxt()
            S.op("act", lambda e: e.activation(out=ta[:], in_=pss[:], func=AF.Ln, bias=EPS, scale=1.0 / 64),
                 reads=[psskey], writes=[takey])
            S.op("act", lambda e: e.activation(out=tb[:], in_=ta[:], func=AF.Exp, scale=-0.5), reads=[takey], writes=[tbkey])
            t["tb"], t["tbkey"] = tb, tbkey

        def q3(t):
            ob, okey = ob_ring.nxt()
            pb, pkey, tb, tbkey, which, c = t["pb"], t["pkey"], t["tb"], t["tbkey"], t["which"], t["c"]
            wcol = qws[:, 0:1] if which == 0 else k.PC("kw")
            S.op("dve", lambda e: e.scalar_tensor_tensor(out=ob[:], in0=pb[:], scalar=wcol, in1=tb[:], op0=ALU.mult,
                                                         op1=ALU.mult), reads=[pkey, tbkey, "qws", "pcol"], writes=[okey])
            dst = k.qT if which == 0 else k.kT
            S.dma("sp", dst[c * 128:(c + 1) * 128, tok0:tok0 + 512], ob[:], reads=[okey])

        its = {}
        for n in range(16 + 2):
            if n < 16:
                its[n] = q1(n)
            if 0 <= n - 1 < 16:
                q2(its[n - 1])
            if 0 <= n - 2 < 16:
                q3(its[n - 2])
        if DBGPRINT: print('a0 after qk', S.nseq)
        pb, pkey = pb_ring.nxt()
        for j in range(4):
            for kc in range(8):
                S.op("pe", lambda e, j=j, kc=kc, pb=pb: e.matmul(pb[:, j * 16:(j + 1) * 16],
                                                                lhsT=hnT[:, kc, j * 128:(j + 1) * 128],
                                                                rhs=win[:, kc, 2560:2576], start=(kc == 0), stop=(kc == 7)),
                     reads=[hkey] + wk(kc, 2560, 2576), writes=[pkey], accumulate=(j + kc > 0))
        dtt, dkey = dtt_ring.nxt()
        S.op("dve", lambda e, pb=pb: e.tensor_tensor(dte[:].rearrange("p (j h) -> p j h", h=16),
                                                     pb[:, 0:64].rearrange("p (j h) -> p j h", h=16),
                                                     k.PR("dtb").unsqueeze(1).to_broadcast([128, 4, 16]), op=ALU.add),
             reads=[pkey, "prow"], writes=["dte"])
        S.op("act", lambda e: e.activation(out=dte[:], in_=dte[:], func=AF.Exp), reads=["dte"], writes=["dte"])
        S.op("act", lambda e, dtt=dtt: e.activation(out=dtt[:].rearrange("p j h -> p (j h)"), in_=dte[:], func=AF.Ln,
                                                    bias=1.0),
             reads=["dte"], writes=[dkey])
        S.dma("sp", k.dts[tok0:tok0 + 512, :].rearrange("(j p) h -> p j h", p=128), dtt[:], reads=[dkey])
        if DBGPRINT: print('a0 after dt', S.nseq)
        for j in range(4):
            for half in range(2):
                pb, pkey = pb_ring.nxt()
                for kc in range(8):
                    S.op("pe", lambda e, j=j, kc=kc, pb=pb, half=half: e.matmul(
                        pb[:], lhsT=hnT[:, kc, j * 128:(j + 1) * 128],
                        rhs=win[:, kc, 4624 + half * 512:4624 + (half + 1) * 512], start=(kc == 0), stop=(kc == 7)),
                        reads=[hkey] + wk(kc, 4624 + half * 512, 4624 + (half + 1) * 512), writes=[pkey],
                        accumulate=(kc > 0))
                ob, okey = ob_ring.nxt()
                if half == 0:
                    S.op("act", lambda e, pb=pb, ob=ob: e.copy(ob[:], pb[:]), reads=[pkey], writes=[okey])
                else:
                    S.op("dve", lambda e, pb=pb, ob=ob: e.tensor_copy(ob[:], pb[:]), reads=[pkey], writes=[okey])
                S.dma("sp", k.vtok[tok0 + j * 128:tok0 + (j + 1) * 128, half * 512:(half + 1) * 512], ob[:],
                      reads=[okey])


def phase_b0(k, es):
    nc, S = k.nc, k.S
    sb = lambda n, s, d: es.enter_context(nc.sbuf_tensor(n + k.sfx, s, d))
    ps = lambda n, s, d: es.enter_context(nc.psum_tensor(n + k.sfx, s, d))
    CF, CB, PC, PR = k.CF, k.CB, k.PC, k.PR
    xb_ring = Ring(sb, "xb", 2, [128, 12, 512], BF16)
    sz_ring = Ring(sb, "szt", 2, [128, 8, 512], BF16)
    dtl_ring = Ring(sb, "dtl", 2, [128, 4, 16], F32)
    yo_ring = Ring(sb, "yo", 2, [128, 8, 512], BF16)
    da_ring = Ring(sb, "da", 3, [128, 16], F32)
    sm_ring = Ring(sb, "sm", 3, [128, 48], F32)
    xtil_ring = Ring(sb, "xtil", 3, [128, 1024], BF16)
    xtd_ring = Ring(sb, "xtd", 3, [128, 1024], BF16)
    bmt_ring = Ring(sb, "bmt", 3, [128, 256], BF16)
    cbm_ring = Ring(sb, "cbm", 3, [128, 2, 128], F32)
    dec_ring = Ring(sb, "dec", 2, [128, 512], F32)
    eb_ring = Ring(sb, "eb", 2, [128, 512], F32)
    mt_ring = Ring(sb, "mt", 9, [128, 512], BF16)
    ce_ring = Ring(sb, "ce", 9, [128, 512], BF16)
    gv_ring = Ring(sb, "gv", 3, [128, 8, 128], F32)
    sq_ring = Ring(sb, "ssq2", 3, [128, 8, 128], BF16)
    rs_ring = Ring(sb, "rs", 2, [128, 128], F32)
    ln_ring = Ring(sb, "lnb", 2, [128, 128], F32)
    ab = sb("ab", [128, 16], F32)
    g4 = sb("g4", [128, 512], F32)
    prev_f = sb("prev_f", [128, 1024], F32)
    prev_b = sb("prev_b", [128, 1024], BF16)
    pmA_ring = Ring(ps, "pmA", 1, [128, 512], F32)
    ptx_ring = Ring(ps, "ptx", 1, [128, 1024], BF16)
    ptb_ring = Ring(ps, "ptb", 1, [128, 256], BF16)
    pw_ring = Ring(ps, "pw", 3, [128, 512], F32)
    py_ring = Ring(ps, "py", 2, [128, 512], F32)

    S.op("act", lambda e: e.activation(out=ab[:], in_=PR("alog"), func=AF.Exp), reads=["prow"], writes=["ab"])
    S.op("act", lambda e: e.mul(ab[:], ab[:], -1.0), reads=["ab"], writes=["ab"])
    for q in range(4):
        S.op("pool", lambda e, q=q: e.tensor_copy(g4[:, q * 128:(q + 1) * 128], CF("gt")), reads=["cf"], writes=["g4"],
             accumulate=(q > 0))

    def load(tt):
        tok0 = tt * 512
        xb, xkey = xb_ring.nxt()
        szt, skey = sz_ring.nxt()
        dtl, dkey = dtl_ring.nxt()
        S.dma("sp", xb[:], k.xbcT[:, tok0:tok0 + 512].rearrange("(c p) t -> p c t", p=128), writes=[xkey])
        S.dma("sp", szt[:], k.szT[:, tok0:tok0 + 512].rearrange("(c p) t -> p c t", p=128), writes=[skey])
        S.dma("sp", dtl[:], k.dts[tok0:tok0 + 512, :].rearrange("(j p) h -> p j h", p=128), writes=[dkey])
        return xb, xkey, szt, skey, dtl, dkey

    tiles = {}

    def get_tile(tt):
        if tt not in tiles:
            tiles[tt] = load(tt) + yo_ring.nxt()
        return tiles[tt]

    def P(c):
        xb, xkey, szt, skey, dtl, dkey, yo, yokey = get_tile(c["tt"])
        j = c["j"]
        js = slice(j * 128, (j + 1) * 128)
        da, dakey = da_ring.nxt()
        S.op("dve", lambda e: e.tensor_tensor(da[:], dtl[:, j, :], ab[:], op=ALU.mult), reads=[dkey, "ab"], writes=[dakey])
        pmA, pmkey = pmA_ring.nxt()
        for i, cname in enumerate(("le", "gt", "ones")):
            S.op("pe", lambda e: e.matmul(pmA[:, i * 16:(i + 1) * 16], lhsT=CF(cname), rhs=da[:], start=True, stop=True),
                 reads=[dakey, "cf"], writes=[(pmkey, "c")], accumulate=(i > 0))
        sm, smkey = sm_ring.nxt()
        S.op("act", lambda e: e.mul(sm[:, 0:16], pmA[:, 0:16], -1.0), reads=[(pmkey, "c")], writes=[smkey])
        S.op("act", lambda e: e.activation(out=sm[:, 16:48], in_=pmA[:, 16:48], func=AF.Exp), reads=[(pmkey, "c")],
             writes=[smkey], accumulate=True)
        ptx, ptxkey = ptx_ring.nxt()
        for fc in range(8):
            S.op("pe", lambda e: e.transpose(out=ptx[:, fc * 128:(fc + 1) * 128], in_=xb[:, fc, js], identity=CB("eye")),
                 reads=[xkey, "cb"], writes=[ptxkey], accumulate=(fc > 0))
        xtil, xtkey = xtil_ring.nxt()
        xtd, xdkey = xtd_ring.nxt()
        S.op("dve", lambda e: e.tensor_tensor(
            xtil[:].rearrange("p (h d) -> p h d", d=64), ptx[:].rearrange("p (h d) -> p h d", d=64),
            dtl[:, j, :].unsqueeze(2).to_broadcast([128, 16, 64]), op=ALU.mult), reads=[ptxkey, dkey], writes=[xtkey])
        S.op("pool", lambda e: e.tensor_tensor(
            xtd[:].rearrange("p (h d) -> p h d", d=64), xtil[:].rearrange("p (h d) -> p h d", d=64),
            sm[:, 16:32].unsqueeze(2).to_broadcast([128, 16, 64]), op=ALU.mult), reads=[xtkey, smkey], writes=[xdkey])
        ptb, ptbkey = ptb_ring.nxt()
        for g in range(2):
            S.op("pe", lambda e: e.transpose(out=ptb[:, g * 128:(g + 1) * 128], in_=xb[:, 8 + g, js], identity=CB("eye")),
                 reads=[xkey, "cb"], writes=[ptbkey], accumulate=(g > 0))
        bmt, bmkey = bmt_ring.nxt()
        S.op("act", lambda e: e.copy(bmt[:], ptb[:]), reads=[ptbkey], writes=[bmkey])
        for g in range(2):
            S.op("pe", lambda e: e.matmul(pmA[:, 64 + g * 128:64 + (g + 1) * 128], lhsT=xb[:, 8 + g, js],
                                          rhs=xb[:, 10 + g, js], start=True, stop=True),
                 reads=[xkey], writes=[(pmkey, "cb")], accumulate=(g > 0))
        cbm, cbkey = cbm_ring.nxt()
        S.op("dve", lambda e: e.tensor_tensor(cbm[:], pmA[:, 64:320].rearrange("p (g t) -> p g t", t=128),
                                              CF("le").unsqueeze(1).to_broadcast([128, 2, 128]), op=ALU.mult),
             reads=[(pmkey, "cb"), "cf"], writes=[cbkey])
        c.update(da=da, dakey=dakey, pmA=pmA, pmkey=pmkey, sm=sm, smkey=smkey, xtil=xtil, xtkey=xtkey, xtd=xtd,
                 xdkey=xdkey, bmt=bmt, bmkey=bmkey, cbm=cbm, cbkey=cbkey)
        c["mt"], c["ce"] = [], []
        for hq in range(4):
            g = hq // 2
            pam, pamkey = pw_ring.nxt()
            pau, paukey = pw_ring.nxt()
            S.op("pe", lambda e: e.matmul(pam[:], lhsT=CF("negbig"), rhs=g4[:], start=True, stop=False),
                 reads=["cf", "g4"], writes=[pamkey])
            for hh in range(4):
                h = hq * 4 + hh
                cs = slice(hh * 128, (hh + 1) * 128)
                lb = da[:, h:h + 1].to_broadcast([128, 128])
                S.op("pe", lambda e: e.matmul(pam[:, cs], lhsT=lb, rhs=CF("le"), start=False, stop=True),
                     reads=[dakey, "cf"], writes=[pamkey], accumulate=True)
                S.op("pe", lambda e: e.matmul(pau[:, cs], lhsT=lb, rhs=CF("le"), start=True, stop=True),
                     reads=[dakey, "cf"], writes=[paukey], accumulate=(hh > 0))
            eb, ebkey = eb_ring.nxt()
            S.op("act", lambda e: e.activation(out=eb[:], in_=pau[:], func=AF.Exp), reads=[paukey], writes=[ebkey])
            dec, deckey = dec_ring.nxt()
            for hh in range(4):
                h = hq * 4 + hh
                cs = slice(hh * 128, (hh + 1) * 128)
                S.op("act", lambda e: e.activation(out=dec[:, cs], in_=pam[:, cs], func=AF.Exp, bias=sm[:, h:h + 1]),
                     reads=[pamkey, smkey], writes=[deckey], accumulate=(hh > 0))
            mt, mtkey = mt_ring.nxt()
            S.op("dve", lambda e: e.tensor_tensor(mt[:].rearrange("p (h t) -> p h t", t=128),
                                                  dec[:].rearrange("p (h t) -> p h t", t=128),
                                                  cbm[:, g, :].unsqueeze(1).to_broadcast([128, 4, 128]), op=ALU.mult),
                 reads=[deckey, cbkey], writes=[mtkey])
            ce, cekey = ce_ring.nxt()
            S.op("pool", lambda e: e.tensor_tensor(ce[:].rearrange("p (h t) -> p h t", t=128),
                                                   eb[:].rearrange("p (h t) -> p h t", t=128),
                                                   xb[:, 10 + g, js].unsqueeze(1).to_broadcast([128, 4, 128]), op=ALU.mult),
                 reads=[xkey, ebkey], writes=[cekey])
            c["mt"].append((mt, mtkey))
            c["ce"].append((ce, cekey))

    def H(c):
        xtil, xtkey = c["xtil"], c["xtkey"]
        if c["tt"] % 4 == 0 and c["j"] == 0:
            S.op("pool", lambda e: e.memset(prev_f[:], 0.0), writes=["prev_f"])
            S.op("pool", lambda e: e.memset(prev_b[:], 0.0), writes=["prev_b"])
        pys = [py_ring.nxt(), py_ring.nxt()]
        c["pys"] = pys
        for h in range(16):
            mt, mtkey = c["mt"][h // 4]
            ce, cekey = c["ce"][h // 4]
            cs = slice((h % 4) * 128, (h % 4 + 1) * 128)
            py, pykey = pys[h // 8]
            fcl = (h // 2) % 4
            po = (h % 2) * 64
            outap = py[po:po + 64, fcl * 128:(fcl + 1) * 128]
            S.op("pe", lambda e: e.matmul(outap, lhsT=xtil[:, h * 64:(h + 1) * 64], rhs=mt[:, cs], start=True, stop=False),
                 reads=[xtkey, mtkey], writes=[pykey], accumulate=(h % 8 > 0))
            S.op("pe", lambda e: e.matmul(outap, lhsT=prev_b[:, h * 64:(h + 1) * 64], rhs=ce[:, cs], start=False, stop=True),
                 reads=["prev_b", cekey], writes=[pykey], accumulate=True)
        sm, smkey, bmt, bmkey, xtd, xdkey = c["sm"], c["smkey"], c["bmt"], c["bmkey"], c["xtd"], c["xdkey"]
        psts = [pw_ring.nxt(), pw_ring.nxt()]
        for g in range(2):
            pst, pstkey = psts[g]
            S.op("pe", lambda e: e.matmul(pst[:], lhsT=bmt[:, g * 128:(g + 1) * 128], rhs=xtd[:, g * 512:(g + 1) * 512],
                                          start=True, stop=True), reads=[bmkey, xdkey], writes=[pstkey])
        S.op("dve", lambda e: e.tensor_tensor(
            prev_f[:].rearrange("p (h d) -> p h d", d=64), prev_f[:].rearrange("p (h d) -> p h d", d=64),
            sm[:, 32:48].unsqueeze(2).to_broadcast([128, 16, 64]), op=ALU.mult),
            reads=["prev_f", smkey], writes=["prev_f"])
        for g in range(2):
            pst, pstkey = psts[g]
            S.op("dve", lambda e: e.tensor_tensor(prev_f[:, g * 512:(g + 1) * 512], prev_f[:, g * 512:(g + 1) * 512],
                                                  pst[:], op=ALU.add), reads=["prev_f", pstkey], writes=["prev_f"])
        S.op("act", lambda e: e.copy(prev_b[:], prev_f[:]), reads=["prev_f"], writes=["prev_b"])

    def E(c):
        xb, xkey, szt, skey, dtl, dkey, yo, yokey = get_tile(c["tt"])
        j = c["j"]
        js = slice(j * 128, (j + 1) * 128)
        pys, pmA, pmkey = c["pys"], c["pmA"], c["pmkey"]
        gv, gvkey = gv_ring.nxt()
        for fc in range(8):
            py, pykey = pys[fc // 4]
            S.op("dve", lambda e: e.scalar_tensor_tensor(
                out=gv[:, fc, :], in0=xb[:, fc, js], scalar=PC("sdd", fc), in1=py[:, (fc % 4) * 128:(fc % 4 + 1) * 128],
                op0=ALU.mult, op1=ALU.add), reads=[xkey, pykey, "pcol"], writes=[gvkey], accumulate=(fc > 0))
        S.op("pool", lambda e: e.tensor_tensor(gv[:], gv[:], szt[:, :, js], op=ALU.mult), reads=[gvkey, skey],
             writes=[gvkey])
        sq, sqkey = sq_ring.nxt()
        S.op("act", lambda e: e.activation(out=sq[:], in_=gv[:], func=AF.Square), reads=[gvkey], writes=[sqkey])
        c.update(gv=gv, gvkey=gvkey, sq=sq, sqkey=sqkey)

    def E2(c):
        xb, xkey, szt, skey, dtl, dkey, yo, yokey = get_tile(c["tt"])
        j = c["j"]
        js = slice(j * 128, (j + 1) * 128)
        pmA, pmkey, gv, gvkey, sq, sqkey = c["pmA"], c["pmkey"], c["gv"], c["gvkey"], c["sq"], c["sqkey"]
        for fc in range(8):
            S.op("pe", lambda e: e.matmul(pmA[:, 384:512], lhsT=CB("ones"), rhs=sq[:, fc, :], start=(fc == 0),
                                          stop=(fc == 7)), reads=[sqkey, "cb"], writes=[(pmkey, "ss")], accumulate=(fc > 0))
        lnb, lnkey = ln_ring.nxt()
        rs, rskey = rs_ring.nxt()
        S.op("act", lambda e: e.activation(out=lnb[:], in_=pmA[:, 384:512], func=AF.Ln, bias=EPS, scale=1.0 / 1024),
             reads=[(pmkey, "ss")], writes=[lnkey])
        S.op("act", lambda e: e.activation(out=rs[:], in_=lnb[:], func=AF.Exp, scale=-0.5), reads=[lnkey], writes=[rskey])
        S.op("dve", lambda e: e.tensor_tensor(gv[:], gv[:], rs[:].unsqueeze(1).to_broadcast([128, 8, 128]), op=ALU.mult),
             reads=[gvkey, rskey], writes=[gvkey])
        S.op("pool", lambda e: e.tensor_tensor(yo[:, :, js], gv[:], PC("snw", 0, 8).unsqueeze(2).to_broadcast([128, 8, 128]),
                                               op=ALU.mult), reads=[gvkey, "pcol"], writes=[yokey], accumulate=(j > 0))
        if j == 3:
            tok0 = c["tt"] * 512
            S.dma("sp", k.ycatT[0:1024, tok0:tok0 + 512].rearrange("(c p) t -> p c t", p=128), yo[:], reads=[yokey])

    chunks = [{"tt": tt, "j": j} for tt in range(NTT) for j in range(4)]
    get_tile(0)
    P(chunks[0])
    for ci, c in enumerate(chunks):
        if c["j"] == 0 and c["tt"] + 1 < NTT:
            get_tile(c["tt"] + 1)
        H(c)
        if ci > 0:
            E2(chunks[ci - 1])
        if ci + 1 < len(chunks):
            P(chunks[ci + 1])
        E(c)
    E2(chunks[-1])


def phase_c0(k, es):
    nc, S = k.nc, k.S
    sb = lambda n, s, d: es.enter_context(nc.sbuf_tensor(n + k.sfx, s, d))
    ps = lambda n, s, d: es.enter_context(nc.psum_tensor(n + k.sfx, s, d))
    CF, CB = k.CF, k.CB
    q_ring = Ring(sb, "qp", 2, [128, L], BF16)
    k_ring = Ring(sb, "kp", 2, [128, L], BF16)
    vb_ring = Ring(sb, "vb", 1, [128, 16, 1024], BF16)
    e_ring = Ring(sb, "ee", 3, [128, 512], F32)
    sp_ring = Ring(sb, "spp", 5, [128, 512], BF16)
    bt_ring = Ring(sb, "btt", 3, [128, 512], BF16)
    r_ring = Ring(sb, "rr", 3, [128, 4], F32)
    accs = [sb("accA", [128, 4, 64], F32), sb("accB", [128, 4, 64], F32)]
    osb_ring = Ring(sb, "osb", 2, [128, 4, 128], BF16)
    oT_ring = Ring(sb, "oT", 2, [128, 512], BF16)
    pz_ring = Ring(ps, "pz", 2, [128, 512], F32)
    py_ring = Ring(ps, "pyy", 3, [128, 512], F32)
    pov_ring = Ring(ps, "pov", 2, [128, 4, 65], F32)
    ptr_ring = Ring(ps, "ptc", 1, [128, 512], BF16)

    for b in range(2):
        vb, vkey = vb_ring.nxt()
        S.dma("sp", vb[:], k.vtok[b * L:(b + 1) * L, :].rearrange("(i p) d -> p i d", p=128), writes=[vkey])
        for hp in range(8):
            qp, qkey = q_ring.nxt()
            kp, kkey = k_ring.nxt()
            S.dma("sp", qp[:], k.qT[hp * 128:(hp + 1) * 128, b * L:(b + 1) * L], writes=[qkey])
            S.dma("sp", kp[:], k.kT[hp * 128:(hp + 1) * 128, b * L:(b + 1) * L], writes=[kkey])
            for g in range(4):
                items = []
                for i in range(4 * g + 4):
                    for hh in range(2):
                        items.append({"i": i, "hh": hh})
                osb, oskey = osb_ring.nxt()
                for hh in range(2):
                    S.op("pool", lambda e, hh=hh: e.memset(accs[hh][:], 0.0), writes=[("acc", hh)])

                def geom(it):
                    i = it["i"]
                    qlo = max(0, i - 4 * g)
                    n = (4 - qlo) * 128
                    t0 = (4 * g + qlo) * 128
                    po = it["hh"] * 64
                    return i, qlo, n, t0, po

                def zmm(it, pt, pkey):
                    i, qlo, n, t0, po = geom(it)
                    diag = i >= 4 * g
                    S.op("pe", lambda e: e.matmul(pt[:, 0:n], lhsT=kp[po:po + 64, i * 128:(i + 1) * 128],
                                                  rhs=qp[po:po + 64, t0:t0 + n], start=True, stop=False),
                         reads=[qkey, kkey], writes=[pkey])
                    if diag:
                        S.op("pe", lambda e: e.matmul(pt[:, 0:128], lhsT=CB("negbig"), rhs=CB("ge"), start=False,
                                                      stop=False), reads=["cb"], writes=[pkey], accumulate=True)

                def s1(it):
                    it["pz"], it["pzkey"] = pz_ring.nxt()
                    zmm(it, it["pz"], it["pzkey"])
                    return

                def s2(it):
                    i, qlo, n, t0, po = geom(it)
                    ee, ekey = e_ring.nxt()
                    sp, spkey = sp_ring.nxt()
                    it["sp"], it["spkey"] = sp, spkey
                    pz, pzkey = it["pz"], it["pzkey"]
                    S.op("act", lambda e: e.activation(out=ee[:, 0:n], in_=pz[:, 0:n], func=AF.Exp),
                         reads=[pzkey], writes=[ekey])
                    S.op("act", lambda e: e.activation(out=sp[:, 0:n], in_=ee[:, 0:n], func=AF.Ln, bias=1.0),
                         reads=[ekey], writes=[spkey])

                def s3(it):
                    i, qlo, n, t0, po = geom(it)
                    it["py"], it["pykey"] = py_ring.nxt()
                    zmm(it, it["py"], it["pykey"])
                    py, sp = it["py"], it["sp"]
                    S.op("pe", lambda e: e.matmul(py[:, 0:n], lhsT=CB("negu"), rhs=sp[:, 0:n], start=False, stop=True),
                         reads=[it["spkey"], "cb"], writes=[it["pykey"]], accumulate=True)

                def s4(it):
                    i, qlo, n, t0, po = geom(it)
                    bt, btkey = bt_ring.nxt()
                    it["bt"], it["btkey"] = bt, btkey
                    py = it["py"]
                    S.op("act", lambda e: e.activation(out=bt[:, 0:n], in_=py[:, 0:n], func=AF.Exp),
                         reads=[it["pykey"]], writes=[btkey])

                def s5(it):
                    i, qlo, n, t0, po = geom(it)
                    pov, povkey = pov_ring.nxt()
                    it["pov"], it["povkey"] = pov, povkey
                    bt, sp = it["bt"], it["sp"]
                    h = hp * 2 + it["hh"]
                    first = True
                    for qq in range(qlo, 4):
                        cs = slice((qq - qlo) * 128, (qq - qlo + 1) * 128)
                        S.op("pe", lambda e, qq=qq, cs=cs: e.matmul(pov[:, qq, 0:64], lhsT=bt[:, cs],
                                                                    rhs=vb[:, i, h * 64:(h + 1) * 64], start=True, stop=True),
                             reads=[it["btkey"], vkey], writes=[povkey], accumulate=(not first))
                        first = False
                        S.op("pe", lambda e, qq=qq, cs=cs: e.matmul(pov[:, qq, 64:65], lhsT=sp[:, cs], rhs=CB("ones")[:, 0:1],
                                                                    start=True, stop=True),
                             reads=[it["spkey"], "cb"], writes=[povkey], accumulate=True)

                def s6(it):
                    i, qlo, n, t0, po = geom(it)
                    rr, rkey = r_ring.nxt()
                    pov, povkey = it["pov"], it["povkey"]
                    hh = it["hh"]
                    acc = accs[hh]
                    nq = 4 - qlo
                    S.op("act", lambda e: e.activation(out=rr[:, qlo:4].unsqueeze(2), in_=pov[:, qlo:4, 64:65], func=AF.Exp,
                                                       scale=-1.0), reads=[povkey], writes=[rkey])
                    S.op("dve", lambda e: e.tensor_tensor(acc[:, qlo:4, :], acc[:, qlo:4, :],
                                                          rr[:, qlo:4].unsqueeze(2).to_broadcast([128, nq, 64]), op=ALU.mult),
                         reads=[rkey, ("acc", hh)], writes=[("acc", hh)])
                    S.op("dve", lambda e: e.tensor_tensor(acc[:, qlo:4, :], acc[:, qlo:4, :], pov[:, qlo:4, 0:64], op=ALU.add),
                         reads=[povkey, ("acc", hh)], writes=[("acc", hh)])

                stages = [s1, s2, s3, s4, s5, s6]
                lag = [0, 0, 1, 1, 2, 2]
                for n in range(len(items) + 2):
                    for st, lg in zip(stages, lag):
                        m = n - lg
                        if 0 <= m < len(items):
                            st(items[m])
                for hh in range(2):
                    S.op("pool", lambda e, hh=hh: e.tensor_copy(osb[:, :, hh * 64:(hh + 1) * 64], accs[hh][:]),
                         reads=[("acc", hh)], writes=[oskey], accumulate=(hh > 0))
                ptc, ptckey = ptr_ring.nxt()
                for qq in range(4):
                    S.op("pe", lambda e, qq=qq: e.transpose(out=ptc[:, qq * 128:(qq + 1) * 128], in_=osb[:, qq, :],
                                                            identity=CB("eye")),
                         reads=[oskey, "cb"], writes=[ptckey], accumulate=(qq > 0))
                oT, oTkey = oT_ring.nxt()
                S.op("dve", lambda e: e.tensor_copy(oT[:], ptc[:]), reads=[ptckey], writes=[oTkey])
                tok0 = b * L + g * 512
                S.dma("sp", k.ycatT[1024 + hp * 128:1024 + (hp + 1) * 128, tok0:tok0 + 512], oT[:], reads=[oTkey])


def phase_proj_res(k, es, w_dram, kc_n, srcT, h_in, h_out):
    nc, S = k.nc, k.S
    sb = lambda n, s, d: es.enter_context(nc.sbuf_tensor(n + k.sfx, s, d))
    ps = lambda n, s, d: es.enter_context(nc.psum_tensor(n + k.sfx, s, d))
    w = sb("wres", [128, kc_n, D], BF16)
    for c in range(kc_n):
        S.dma("pool", w[:, c, :], w_dram[c * 128:(c + 1) * 128, :], writes=[("wres", c)])
    src_ring = Ring(sb, "srct", 2, [128, kc_n, 512], BF16)
    h_ring = Ring(sb, "hres", 2, [128, 4, D], F32)
    po_ring = Ring(ps, "pres", 4, [128, 512], F32)

    def load(tt):
        src, skey = src_ring.nxt()
        ht, hkey = h_ring.nxt()
        S.dma("sp", src[:], srcT[:, tt * 512:(tt + 1) * 512].rearrange("(c p) t -> p c t", p=128), writes=[skey])
        S.dma("sp", ht[:], h_in[tt * 512:(tt + 1) * 512, :].rearrange("(j p) d -> p j d", p=128), writes=[hkey])
        return src, skey, ht, hkey

    nxt = load(0)
    for tt in range(NTT):
        src, skey, ht, hkey = nxt
        if tt + 1 < NTT:
            nxt = load(tt + 1)
        for j in range(4):
            for half in range(2):
                po, pkey = po_ring.nxt()
                for c in range(kc_n):
                    S.op("pe", lambda e: e.matmul(po[:], lhsT=src[:, c, j * 128:(j + 1) * 128],
                                                  rhs=w[:, c, half * 512:(half + 1) * 512], start=(c == 0),
                                                  stop=(c == kc_n - 1)),
                         reads=[skey, ("wres", c)], writes=[pkey], accumulate=(c > 0))
                S.op("dve", lambda e: e.tensor_tensor(ht[:, j, half * 512:(half + 1) * 512],
                                                      ht[:, j, half * 512:(half + 1) * 512], po[:], op=ALU.add),
                     reads=[pkey, hkey], writes=[hkey])
        S.dma("sp", h_out[tt * 512:(tt + 1) * 512, :].rearrange("(j p) d -> p j d", p=128), ht[:], reads=[hkey])


def phase_ffn_up(k, es, layer, h_in, gT):
    nc, S = k.nc, k.S
    sb = lambda n, s, d: es.enter_context(nc.sbuf_tensor(n + k.sfx, s, d))
    ps = lambda n, s, d: es.enter_context(nc.psum_tensor(n + k.sfx, s, d))
    PC, PR = k.PC, k.PR
    wup = sb("wup", [128, 8, 2 * DFF], BF16)
    for cb_ in range(4):
        for kc in range(8):
            S.dma("pool", wup[:, kc, cb_ * 1408:(cb_ + 1) * 1408],
                  k.ffn_up_w[layer, kc * 128:(kc + 1) * 128, cb_ * 1408:(cb_ + 1) * 1408], writes=[("wup", kc, cb_)])
    xt_ring = Ring(sb, "xtf", 2, [128, 4, D], F32)
    hn_ring = Ring(sb, "hnf", 1, [128, 4, D], BF16)
    hnT_ring = Ring(sb, "hnTf", 2, [128, 8, 512], BF16)
    ptr_ring = Ring(ps, "ptrf", 2, [128, 1024], BF16)
    pb_ring = Ring(ps, "pbf", 6, [128, 512], F32)
    junk = sb("junkf", [128, D], BF16)
    small = (sb("ssqf", [128, 4], F32), sb("lnvf", [128, 4], F32), sb("rstdf", [128, 4], F32))
    raw_ring = Ring(sb, "rawf", 6, [128, 514], F32)
    acc_ring = Ring(sb, "accf", 6, [128, 512], F32)
    sg_ring = Ring(sb, "sgf", 2, [128, 512], F32)
    ob_ring = Ring(sb, "obf", 3, [128, 512], BF16)
    halo = sb("halof", [128, 44, 2], F32)
    cwn, cbn, nwn = "fcw%d" % layer, "fcb%d" % layer, "nw%d1" % layer

    def load_x(tt):
        xt, xkey = xt_ring.nxt()
        S.dma("sp", xt[:], h_in[tt * 512:(tt + 1) * 512, :].rearrange("(j p) d -> p j d", p=128), writes=[xkey])
        return xt, xkey

    def prologue(xx):
        xt, xkey = xx
        hnT, hkey = hnT_ring.nxt()
        norm_transpose(k, S, xt, xkey, PR(nwn), hn_ring, hnT, hkey, ptr_ring, junk, small)
        return hnT, hkey

    xs = {0: load_x(0)}
    if NTT > 1:
        xs[1] = load_x(1)
    pro = {0: prologue(xs[0])}
    for tt in range(NTT):
        if tt + 2 < NTT:
            xs[tt + 2] = load_x(tt + 2)
        hnT, hkey = pro[tt]
        tok0 = tt * 512
        seq_start = (tt % 4 == 0)

        def st1(cc):
            pb, pkey = pb_ring.nxt()
            for kc in range(8):
                S.op("pe", lambda e: e.matmul(pb[:], lhsT=wup[:, kc, cc * 128:(cc + 1) * 128], rhs=hnT[:, kc, :],
                                              start=(kc == 0), stop=(kc == 7)),
                     reads=[hkey, ("wup", kc, (cc * 128) // 1408)], writes=[pkey], accumulate=(kc > 0))
            raw, rkey = raw_ring.nxt()
            acc, akey = acc_ring.nxt()
            if seq_start:
                S.op("pool", lambda e: e.memset(raw[:, 0:2], 0.0), writes=[(rkey, "h")])
            else:
                S.op("pool", lambda e: e.tensor_copy(raw[:, 0:2], halo[:, cc, :]), reads=[("halof", cc)],
                     writes=[(rkey, "h")])
            S.op("act", lambda e: e.copy(raw[:, 2:514], pb[:]), reads=[pkey], writes=[rkey])
            S.op("act", lambda e: e.activation(out=acc[:], in_=pb[:], func=AF.Identity, bias=PC(cbn, cc),
                                               scale=PC(cwn, cc * 3 + 2)), reads=[pkey, "pcol"], writes=[akey])
            return (raw, rkey, acc, akey, cc)

        def st2(ta, tb_):
            for tp in range(2):
                for (raw, rkey, acc, akey, cc) in (ta, tb_):
                    S.op("dve", lambda e: e.scalar_tensor_tensor(out=acc[:], in0=raw[:, tp:tp + 512],
                                                                 scalar=PC(cwn, cc * 3 + tp), in1=acc[:], op0=ALU.mult,
                                                                 op1=ALU.add), reads=[rkey, (rkey, "h"), akey, "pcol"],
                         writes=[akey])
            for (raw, rkey, acc, akey, cc) in (ta, tb_):
                S.op("pool", lambda e: e.tensor_copy(halo[:, cc, :], raw[:, 512:514]), reads=[rkey], writes=[("halof", cc)])

        def st3(tg, tv, c):
            ag, agkey = tg[2], tg[3]
            av, avkey = tv[2], tv[3]
            sg, sgkey = sg_ring.nxt()
            S.op("act", lambda e: e.activation(out=sg[:], in_=ag[:], func=AF.Silu), reads=[agkey], writes=[sgkey])
            ob, okey = ob_ring.nxt()
            S.op("pool", lambda e: e.tensor_tensor(ob[:], sg[:], av[:], op=ALU.mult), reads=[sgkey, avkey], writes=[okey])
            S.dma("sp", gT[c * 128:(c + 1) * 128, tok0:tok0 + 512], ob[:], reads=[okey])

        its = {}
        for n in range(22 + 2):
            if n == 11 and tt + 1 < NTT:
                pro[tt + 1] = prologue(xs[tt + 1])
            if n < 22:
                its[n] = (st1(n), st1(22 + n))
            if 0 <= n - 1 < 22:
                st2(its[n - 1][0], its[n - 1][1])
            if 0 <= n - 2 < 22:
                st3(its[n - 2][0], its[n - 2][1], n - 2)


def phase_e1(k, es):
    nc, S = k.nc, k.S
    sb = lambda n, s, d: es.enter_context(nc.sbuf_tensor(n + k.sfx, s, d))
    ps = lambda n, s, d: es.enter_context(nc.psum_tensor(n + k.sfx, s, d))
    PC, PR = k.PC, k.PR
    win = sb("winm", [128, 8, MLC], BF16)
    BW = 386

    def wk(kc, c0, c1):
        return [("winm", kc, b) for b in range(c0 // BW, (c1 - 1) // BW + 1)]

    for b in range(8):
        for kc in range(8):
            S.dma("pool", win[:, kc, b * BW:(b + 1) * BW], k.ml_in_w[kc * 128:(kc + 1) * 128, b * BW:(b + 1) * BW],
                  writes=[("winm", kc, b)])
    xt_ring = Ring(sb, "xtm", 2, [128, 4, D], F32)
    hn_ring = Ring(sb, "hnm", 1, [128, 4, D], BF16)
    hnT_ring = Ring(sb, "hnTm", 2, [128, 8, 512], BF16)
    ptr_ring = Ring(ps, "ptrm", 2, [128, 1024], BF16)
    pb_ring = Ring(ps, "pbm", 5, [128, 512], F32)
    junk = sb("junkm", [128, D], BF16)
    small = (sb("ssqm", [128, 4], F32), sb("lnvm", [128, 4], F32), sb("rstdm", [128, 4], F32))
    ob_ring = Ring(sb, "obm", 4, [128, 512], BF16)
    gg = sb("ggm", [128, 4, 16], F32)
    th = sb("thm", [128, 4, 16], F32)
    ef = sb("efm", [128, 4, 8], F32)
    gt_ring = Ring(sb, "gtm", 2, [128, 4, 16], F32)

    def load_x(tt):
        xt, xkey = xt_ring.nxt()
        S.dma("sp", xt[:], k.h2[tt * 512:(tt + 1) * 512, :].rearrange("(j p) d -> p j d", p=128), writes=[xkey])
        return xt, xkey

    def prologue(xx):
        xt, xkey = xx
        hnT, hkey = hnT_ring.nxt()
        norm_transpose(k, S, xt, xkey, PR("nw10"), hn_ring, hnT, hkey, ptr_ring, junk, small)
        return hnT, hkey

    xs = {0: load_x(0)}
    if NTT > 1:
        xs[1] = load_x(1)
    pro = {0: prologue(xs[0])}
    for tt in range(NTT):
        if tt + 2 < NTT:
            xs[tt + 2] = load_x(tt + 2)
        hnT, hkey = pro[tt]
        tok0 = tt * 512

        def proj_fm(col0):
            pb, pkey = pb_ring.nxt()
            for kc in range(8):
                S.op("pe", lambda e: e.matmul(pb[:], lhsT=win[:, kc, col0:col0 + 128], rhs=hnT[:, kc, :],
                                              start=(kc == 0), stop=(kc == 7)),
                     reads=[hkey] + wk(kc, col0, col0 + 128), writes=[pkey], accumulate=(kc > 0))
            return pb, pkey

        def proj_tm(j, col0, n):
            pb, pkey = pb_ring.nxt()
            for kc in range(8):
                S.op("pe", lambda e: e.matmul(pb[:, 0:n], lhsT=hnT[:, kc, j * 128:(j + 1) * 128],
                                              rhs=win[:, kc, col0:col0 + n], start=(kc == 0), stop=(kc == 7)),
                     reads=[hkey] + wk(kc, col0, col0 + n), writes=[pkey], accumulate=(kc > 0))
            return pb, pkey

        for c in range(4):
            pb, pkey = proj_fm(c * 128)
            ob, okey = ob_ring.nxt()
            S.op("act", lambda e: e.mul(ob[:], pb[:], 0.125), reads=[pkey], writes=[okey])
            S.dma("sp", k.mqT[c * 128:(c + 1) * 128, tok0:tok0 + 512], ob[:], reads=[okey])
        for c in range(4):
            pb, pkey = proj_fm(512 + c * 128)
            ob, okey = ob_ring.nxt()
            S.op("dve", lambda e: e.tensor_copy(ob[:], pb[:]), reads=[pkey], writes=[okey])
            S.dma("sp", k.mkT[c * 128:(c + 1) * 128, tok0:tok0 + 512], ob[:], reads=[okey])
        for c in range(8):
            pb, pkey = proj_fm(2048 + c * 128)
            ob, okey = ob_ring.nxt()
            S.op("act", lambda e: e.activation(out=ob[:], in_=pb[:], func=AF.Sigmoid), reads=[pkey], writes=[okey])
            S.dma("sp", k.soT[c * 128:(c + 1) * 128, tok0:tok0 + 512], ob[:], reads=[okey])
        if tt + 1 < NTT:
            pro[tt + 1] = prologue(xs[tt + 1])
        for j in range(4):
            for part in range(3):
                col0 = 512 if part == 0 else 1024 + (part - 1) * 512
                pb, pkey = proj_tm(j, col0, 512)
                ob, okey = ob_ring.nxt()
                if part == 1:
                    S.op("act", lambda e: e.copy(ob[:], pb[:]), reads=[pkey], writes=[okey])
                else:
                    S.op("dve", lambda e: e.tensor_copy(ob[:], pb[:]), reads=[pkey], writes=[okey])
                if part == 0:
                    S.dma("sp", k.mkt[tok0 + j * 128:tok0 + (j + 1) * 128, :], ob[:], reads=[okey])
                else:
                    S.dma("sp", k.mvt[tok0 + j * 128:tok0 + (j + 1) * 128, (part - 1) * 512:part * 512], ob[:],
                          reads=[okey])
        pb, pkey = pb_ring.nxt()
        for j in range(4):
            for kc in range(8):
                S.op("pe", lambda e: e.matmul(pb[:, j * 16:(j + 1) * 16], lhsT=hnT[:, kc, j * 128:(j + 1) * 128],
                                              rhs=win[:, kc, 3072:3088], start=(kc == 0), stop=(kc == 7)),
                     reads=[hkey] + wk(kc, 3072, 3088), writes=[pkey], accumulate=(j + kc > 0))
        gt, gtkey = gt_ring.nxt()
        r0 = PROW["mib"][0]
        S.op("dve", lambda e: e.tensor_tensor(gg[:], pb[:, 0:64].rearrange("p (j h) -> p j h", h=16),
                                              k.prow[:, r0:r0 + 16].unsqueeze(1).to_broadcast([128, 4, 16]), op=ALU.add),
             reads=[pkey, "prow"], writes=["ggm"])
        S.op("act", lambda e: e.activation(out=th[:], in_=gg[:], func=AF.Tanh, scale=1.0 / 15.0), reads=["ggm"],
             writes=["thm"])
        S.op("act", lambda e: e.mul(gt[:, :, 0:8], th[:, :, 0:8], 15.0), reads=["thm"], writes=[gtkey])
        S.op("act", lambda e: e.activation(out=ef[:], in_=th[:, :, 8:16], func=AF.Exp, scale=-15.0), reads=["thm"],
             writes=["efm"])
        S.op("act", lambda e: e.activation(out=ef[:], in_=ef[:], func=AF.Ln, bias=1.0), reads=["efm"], writes=["efm"])
        S.op("act", lambda e: e.mul(gt[:, :, 8:16], ef[:], -1.0), reads=["efm"], writes=[gtkey], accumulate=True)
        S.dma("sp", k.gts[tok0:tok0 + 512, :].rearrange("(j p) h -> p j h", p=128), gt[:], reads=[gtkey])


def phase_f1(k, es):
    nc, S = k.nc, k.S
    sb = lambda n, s, d: es.enter_context(nc.sbuf_tensor(n + k.sfx, s, d))
    ps = lambda n, s, d: es.enter_context(nc.psum_tensor(n + k.sfx, s, d))
    CF, CB, PC = k.CF, k.CB, k.PC
    q_ring = Ring(sb, "mq", 2, [128, 4, 512], BF16)
    k_ring = Ring(sb, "mk", 2, [128, 4, 512], BF16)
    kt_ring = Ring(sb, "mkt", 2, [128, 4, 512], BF16)
    vt_ring = Ring(sb, "mvt", 2, [128, 4, 1024], BF16)
    so_ring = Ring(sb, "mso", 2, [128, 8, 512], BF16)
    gl_ring = Ring(sb, "mgl", 2, [128, 4, 16], F32)
    ho_ring = Ring(sb, "mho", 2, [128, 8, 512], BF16)
    g4 = sb("mg4", [128, 512], F32)
    sm_ring = Ring(sb, "msm", 3, [128, 32], F32)
    cdp_ring = Ring(sb, "mcdp", 3, [128, 4], F32)
    dt_ring = Ring(sb, "mdt", 3, [128, 512], F32)
    eb_ring = Ring(sb, "meb", 3, [128, 512], F32)
    st_ring = Ring(sb, "mst", 3, [128, 512], BF16)
    qe_ring = Ring(sb, "mqe", 3, [128, 512], BF16)
    ad_ring = Ring(sb, "mad", 3, [128, 512], F32)
    hT_ring = Ring(sb, "mhT", 3, [128, 512], F32)
    sq_ring = Ring(sb, "msq", 3, [128, 512], BF16)
    ln_ring = Ring(sb, "mln", 3, [128, 512], F32)
    rs_ring = Ring(sb, "mrs", 3, [128, 512], F32)
    kd_ring = Ring(sb, "mkd", 3, [128, 8, 64], BF16)
    Cf = sb("mCf", [128, 4, 128], F32)
    Cb_ = sb("mCb", [128, 4, 128], BF16)
    nf = sb("mnf", [128, 4], F32)
    nb = sb("mnb", [128, 4], BF16)
    pG_ring = Ring(ps, "mpG", 1, [128, 64], F32)
    pD_ring = Ring(ps, "mpD", 2, [128, 512], F32)
    pkq_ring = Ring(ps, "mpkq", 1, [128, 512], F32)
    pN_ring = Ring(ps, "mpN", 1, [128, 512], F32)
    pden_ring = Ring(ps, "mpden", 1, [128, 512], F32)
    pss_ring = Ring(ps, "mpss", 1, [128, 512], F32)
    pC_ring = Ring(ps, "mpC", 1, [128, 4, 128], F32)

    for q in range(4):
        S.op("pool", lambda e: e.tensor_copy(g4[:, q * 128:(q + 1) * 128], CF("gt")), reads=["cf"], writes=["mg4"],
             accumulate=(q > 0))

    def load(tt):
        tok0 = tt * 512
        r = {}
        for name, ring, src in (("q", q_ring, k.mqT), ("k", k_ring, k.mkT), ("so", so_ring, k.soT)):
            t, key = ring.nxt()
            S.dma("sp", t[:], src[:, tok0:tok0 + 512].rearrange("(c p) t -> p c t", p=128), writes=[key])
            r[name] = (t, key)
        for name, ring, src in (("kt", kt_ring, k.mkt), ("vt", vt_ring, k.mvt), ("gl", gl_ring, k.gts)):
            t, key = ring.nxt()
            S.dma("sp", t[:], src[tok0:tok0 + 512, :].rearrange("(j p) d -> p j d", p=128), writes=[key])
            r[name] = (t, key)
        r["ho"] = ho_ring.nxt()
        return r

    tiles = {}

    def get_tile(tt):
        if tt not in tiles:
            tiles[tt] = load(tt)
        return tiles[tt]

    def P(c):
        cur = get_tile(c["tt"])
        j = c["j"]
        (gl, glkey), (kt, ktkey) = cur["gl"], cur["kt"]
        logi = gl[:, j, 0:8]
        logf = gl[:, j, 8:16]
        pG, pGkey = pG_ring.nxt()
        c["pG"], c["pGkey"] = pG, pGkey
        for i, cname in enumerate(("le", "gt", "ones")):
            S.op("pe", lambda e: e.matmul(pG[:, i * 8:(i + 1) * 8], lhsT=CF(cname), rhs=logf, start=True, stop=True),
                 reads=[glkey, "cf"], writes=[(pGkey, "g")], accumulate=(i > 0))
        sm, smkey = sm_ring.nxt()
        cdp, cdpkey = cdp_ring.nxt()
        c["sm"], c["smkey"], c["cdp"], c["cdpkey"] = sm, smkey, cdp, cdpkey
        S.op("dve", lambda e: e.tensor_tensor(sm[:, 0:8], logi, pG[:, 0:8], op=ALU.subtract),
             reads=[glkey, (pGkey, "g")], writes=[smkey])
        S.op("dve", lambda e: e.tensor_tensor(sm[:, 24:32], logi, pG[:, 8:16], op=ALU.add),
             reads=[glkey, (pGkey, "g")], writes=[smkey], accumulate=True)
        S.op("act", lambda e: e.activation(out=sm[:, 8:16], in_=sm[:, 24:32], func=AF.Exp), reads=[smkey], writes=[smkey])
        S.op("act", lambda e: e.activation(out=cdp[0:64, :], in_=pG[0:64, 16:24:2], func=AF.Exp),
             reads=[(pGkey, "g")], writes=[cdpkey])
        S.op("act", lambda e: e.activation(out=cdp[64:128, :], in_=pG[64:128, 17:24:2], func=AF.Exp),
             reads=[(pGkey, "g")], writes=[cdpkey], accumulate=True)
        kd, kdkey = kd_ring.nxt()
        c["kd"], c["kdkey"] = kd, kdkey
        S.op("pool", lambda e: e.tensor_tensor(kd[:], kt[:, j, :].rearrange("p (h d) -> p h d", d=64),
                                               sm[:, 8:16].unsqueeze(2).to_broadcast([128, 8, 64]), op=ALU.mult),
             reads=[ktkey, smkey], writes=[kdkey])

    def H(c):
        cur = get_tile(c["tt"])
        j = c["j"]
        js = slice(j * 128, (j + 1) * 128)
        (mq, qkey), (mk, kkey), (so, sokey) = cur["q"], cur["k"], cur["so"]
        (kt, ktkey), (vt, vtkey), (gl, glkey) = cur["kt"], cur["vt"], cur["gl"]
        ho, hokey = cur["ho"]
        sm, smkey, kd, kdkey, pG, pGkey = c["sm"], c["smkey"], c["kd"], c["kdkey"], c["pG"], c["pGkey"]
        if c["tt"] % 4 == 0 and j == 0:
            S.op("pool", lambda e: e.memset(Cf[:], 0.0), writes=["mCf"])
            S.op("pool", lambda e: e.memset(Cb_[:], 0.0), writes=["mCb"])
            S.op("pool", lambda e: e.memset(nf[:], 0.0), writes=["mnf"])
            S.op("pool", lambda e: e.memset(nb[:], 0.0), writes=["mnb"])
        pC, pCkey = pC_ring.nxt()
        c["pC"], c["pCkey"] = pC, pCkey
        CS = lambda hh: slice(hh * 128, (hh + 1) * 128)

        def front(hq):
            pDm, pDmkey = pD_ring.nxt()
            pDu, pDukey = pD_ring.nxt()
            pkq, pkqkey = pkq_ring.nxt()
            pN, pNkey = pN_ring.nxt()
            pden, pdenkey = pden_ring.nxt()
            hs = [hq * 4 + hh for hh in range(4)]
            S.op("pe", lambda e: e.matmul(pDm[:], lhsT=CF("negbig"), rhs=g4[:], start=True, stop=False),
                 reads=["cf", "mg4"], writes=[pDmkey])
            for hh, h in enumerate(hs):
                po, pr = (h % 2) * 64, h // 2
                lb = gl[:, j, 8 + h:9 + h].to_broadcast([128, 128])
                S.op("pe", lambda e: e.matmul(pDm[:, CS(hh)], lhsT=lb, rhs=CF("le"), start=False, stop=True),
                     reads=[glkey, "cf"], writes=[pDmkey], accumulate=True)
                S.op("pe", lambda e: e.matmul(pDu[:, CS(hh)], lhsT=lb, rhs=CF("le"), start=True, stop=True),
                     reads=[glkey, "cf"], writes=[pDukey], accumulate=(hh > 0))
                S.op("pe", lambda e: e.matmul(pkq[:, CS(hh)], lhsT=mk[po:po + 64, pr, js], rhs=mq[po:po + 64, pr, js],
                                              start=True, stop=True),
                     reads=[qkey, kkey], writes=[pkqkey], accumulate=(hh > 0))
            eb, ebkey = eb_ring.nxt()
            S.op("act", lambda e: e.activation(out=eb[:], in_=pDu[:], func=AF.Exp), reads=[pDukey], writes=[ebkey])
            dtl, dtkey = dt_ring.nxt()
            for hh, h in enumerate(hs):
                S.op("act", lambda e: e.activation(out=dtl[:, CS(hh)], in_=pDm[:, CS(hh)], func=AF.Exp,
                                                   bias=sm[:, h:h + 1]),
                     reads=[pDmkey, smkey], writes=[dtkey], accumulate=(hh > 0))
            st, stkey = st_ring.nxt()
            S.op("dve", lambda e: e.tensor_tensor(st[:], dtl[:], pkq[:], op=ALU.mult), reads=[dtkey, pkqkey],
                 writes=[stkey])
            qe, qekey = qe_ring.nxt()
            for hh, h in enumerate(hs):
                po, pr = (h % 2) * 64, h // 2
                S.op("pool", lambda e: e.tensor_tensor(qe[po:po + 64, CS(hh)], mq[po:po + 64, pr, js],
                                                       eb[po:po + 64, CS(hh)], op=ALU.mult),
                     reads=[qkey, ebkey], writes=[qekey], accumulate=(hh > 0))
            for hh, h in enumerate(hs):
                po, pr = (h % 2) * 64, h // 2
                S.op("pe", lambda e: e.matmul(pN[:, CS(hh)], lhsT=vt[:, j, h * 128:(h + 1) * 128], rhs=st[:, CS(hh)],
                                              start=True, stop=False), reads=[vtkey, stkey], writes=[pNkey],
                     accumulate=(hh > 0))
                S.op("pe", lambda e: e.matmul(pN[:, CS(hh)], lhsT=Cb_[po:po + 64, pr, :], rhs=qe[po:po + 64, CS(hh)],
                                              start=False, stop=True), reads=["mCb", qekey], writes=[pNkey],
                     accumulate=True)
                S.op("pe", lambda e: e.matmul(pden[:, CS(hh)], lhsT=CB("ones"), rhs=st[:, CS(hh)], start=True, stop=False),
                     reads=["cb", stkey], writes=[pdenkey], accumulate=(hh > 0))
                S.op("pe", lambda e: e.matmul(pden[:, CS(hh)], lhsT=nb[po:po + 64, pr:pr + 1].to_broadcast([64, 128]),
                                              rhs=qe[po:po + 64, CS(hh)], start=False, stop=True),
                     reads=["mnb", qekey], writes=[pdenkey], accumulate=True)
            for hh, h in enumerate(hs):
                po, pr = (h % 2) * 64, h // 2
                S.op("pe", lambda e: e.matmul(pC[po:po + 64, pr, :], lhsT=kd[:, h, :], rhs=vt[:, j, h * 128:(h + 1) * 128],
                                              start=True, stop=True), reads=[kdkey, vtkey], writes=[pCkey],
                     accumulate=(h > 0))
                S.op("pe", lambda e: e.matmul(pG[po:po + 64, 32 + pr:33 + pr], lhsT=kd[:, h, :], rhs=CB("ones")[:, 0:1],
                                              start=True, stop=True), reads=[kdkey, "cb"], writes=[(pGkey, "n")],
                     accumulate=(h > 0))
            return {"hq": hq, "hs": hs, "pN": pN, "pNkey": pNkey, "pden": pden, "pdenkey": pdenkey}

        def tailA(g):
            pN, pNkey, pden, pdenkey = g["pN"], g["pNkey"], g["pden"], g["pdenkey"]
            ad, adkey = ad_ring.nxt()
            S.op("act", lambda e: e.activation(out=ad[:], in_=pden[:], func=AF.Abs), reads=[pdenkey], writes=[adkey])
            S.op("dve", lambda e: e.tensor_scalar(ad[:], ad[:], 1.0, None, op0=ALU.max), reads=[adkey], writes=[adkey])
            S.op("dve", lambda e: e.reciprocal(ad[:], ad[:]), reads=[adkey], writes=[adkey])
            hT, hTkey = hT_ring.nxt()
            S.op("dve", lambda e: e.tensor_tensor(hT[:], pN[:], ad[:], op=ALU.mult), reads=[pNkey, adkey], writes=[hTkey])
            sq, sqkey = sq_ring.nxt()
            S.op("act", lambda e: e.activation(out=sq[:], in_=hT[:], func=AF.Square), reads=[hTkey], writes=[sqkey])
            g.update(hT=hT, hTkey=hTkey, sq=sq, sqkey=sqkey)

        def tailB(g):
            hq, hs, hT, hTkey, sq, sqkey = g["hq"], g["hs"], g["hT"], g["hTkey"], g["sq"], g["sqkey"]
            pss, psskey = pss_ring.nxt()
            S.op("pe", lambda e: e.matmul(pss[:], lhsT=CB("ones"), rhs=sq[:], start=True, stop=True),
                 reads=["cb", sqkey], writes=[psskey])
            lnb, lnkey = ln_ring.nxt()
            rs, rskey = rs_ring.nxt()
            S.op("act", lambda e: e.activation(out=lnb[:], in_=pss[:], func=AF.Ln, bias=EPS, scale=1.0 / 128),
                 reads=[psskey], writes=[lnkey])
            S.op("act", lambda e: e.activation(out=rs[:], in_=lnb[:], func=AF.Exp, scale=-0.5), reads=[lnkey],
                 writes=[rskey])
            for hh, h in enumerate(hs):
                S.op("dve", lambda e: e.scalar_tensor_tensor(out=hT[:, CS(hh)], in0=hT[:, CS(hh)], scalar=PC("mnw", h),
                                                             in1=rs[:, CS(hh)], op0=ALU.mult, op1=ALU.mult),
                     reads=[hTkey, rskey, "pcol"], writes=[hTkey])
            S.op("pool", lambda e: e.tensor_tensor(ho[:, hq * 4:(hq + 1) * 4, js], hT[:].rearrange("p (h t) -> p h t", t=128),
                                                   so[:, hq * 4:(hq + 1) * 4, js], op=ALU.mult),
                 reads=[hTkey, sokey], writes=[hokey], accumulate=(j + hq > 0))

        g0 = front(0)
        tailA(g0)
        g1 = front(1)
        tailB(g0)
        tailA(g1)
        tailB(g1)

    def E(c):
        cdp, cdpkey, pC, pCkey, pG, pGkey = c["cdp"], c["cdpkey"], c["pC"], c["pCkey"], c["pG"], c["pGkey"]
        cdb = cdp[:].unsqueeze(2).to_broadcast([128, 4, 128])
        S.op("dve", lambda e: e.tensor_tensor(Cf[:], Cf[:], cdb, op=ALU.mult), reads=["mCf", cdpkey], writes=["mCf"])
        S.op("dve", lambda e: e.tensor_tensor(Cf[:], Cf[:], pC[:], op=ALU.add), reads=["mCf", pCkey], writes=["mCf"])
        S.op("act", lambda e: e.copy(Cb_[:], Cf[:]), reads=["mCf"], writes=["mCb"])
        S.op("dve", lambda e: e.tensor_tensor(nf[:], nf[:], cdp[:], op=ALU.mult), reads=["mnf", cdpkey], writes=["mnf"])
        S.op("dve", lambda e: e.tensor_tensor(nf[:], nf[:], pG[:, 32:36], op=ALU.add), reads=["mnf", (pGkey, "n")],
             writes=["mnf"])
        S.op("act", lambda e: e.copy(nb[:], nf[:]), reads=["mnf"], writes=["mnb"])
        if c["j"] == 3:
            tok0 = c["tt"] * 512
            ho, hokey = get_tile(c["tt"])["ho"]
            S.dma("sp", k.hmT[:, tok0:tok0 + 512].rearrange("(c p) t -> p c t", p=128), ho[:], reads=[hokey])

    chunks = [{"tt": tt, "j": j} for tt in range(NTT) for j in range(4)]
    get_tile(0)
    P(chunks[0])
    for ci, c in enumerate(chunks):
        if c["j"] == 0 and c["tt"] + 1 < NTT:
            get_tile(c["tt"] + 1)
        H(c)
        if ci + 1 < len(chunks):
            P(chunks[ci + 1])
        E(c)


_NC_CACHE = {}


def kernel(**inputs):
    inp = {k_: np.asarray(v) for k_, v in inputs.items()}
    if "nc" not in _NC_CACHE:
        _NC_CACHE["nc"] = build()
    nc = _NC_CACHE["nc"]
    pc, pr = pack_params(inp)
    consts = make_consts()
    x = np.ascontiguousarray(inp["x"], dtype=np.float32)
    shared = {
        "pcol": pc, "prow": pr, "consts": consts,
        "hy_in_w": np.ascontiguousarray(inp["hy_in_w"][0], dtype=np.float32),
        "hy_out_w": np.ascontiguousarray(inp["hy_out_w"][0], dtype=np.float32),
        "ml_in_w": np.ascontiguousarray(inp["ml_in_w"][0], dtype=np.float32),
        "ml_out_w": np.ascontiguousarray(inp["ml_out_w"][0], dtype=np.float32),
        "ffn_up_w": np.ascontiguousarray(inp["ffn_up_w"], dtype=np.float32),
        "ffn_down_w": np.ascontiguousarray(inp["ffn_down_w"], dtype=np.float32),
    }
    in_maps = []
    for c in range(NCORES):
        m = dict(shared)
        m["x"] = np.ascontiguousarray(x[2 * c:2 * c + 2].reshape(NTOK, D))
        in_maps.append(m)
    res = run_bass_kernel_spmd(nc, in_maps, core_ids=list(range(NCORES)))
    out = np.concatenate([np.asarray(r["out"]).reshape(2, L, D) for r in res.results], axis=0)
    return out.astype(np.float32)
```

```python
import numpy as np
from contextlib import ExitStack
import concourse.bass as bass
import concourse.mybir as mybir
from concourse.bass_utils import run_bass_kernel_spmd

F32 = mybir.dt.float32
BF16 = mybir.dt.bfloat16
AF = mybir.ActivationFunctionType
ALU = mybir.AluOpType

NCORES = 8
NTOK = 4096
L = 2048
D = 1024
EPS = 1e-6
HYC = 5648
MLC = 3088
DFF = 2816
NTT = 8
DBGPRINT = False
OPLIMIT = None

ENGS = ("pe", "act", "dve", "pool", "sp")
NDSEM = 8


class Ins:
    __slots__ = ("eng", "fn", "deps", "is_dma", "needed", "ticket", "dslot", "dval", "waits", "q_n", "seq")

    def __init__(self, eng, fn, is_dma):
        self.eng = eng
        self.fn = fn
        self.is_dma = is_dma
        self.deps = []
        self.needed = False
        self.ticket = 0
        self.waits = []


class _Rec:
    def __getattr__(self, name):
        def f(*a, **kw):
            self.call = (name, a, kw)
        return f


class Sched:
    def __init__(self, nc, es):
        self.nc = nc
        self.sems = {}
        for e in ENGS:
            self.sems[("c", e)] = es.enter_context(nc.semaphore("c_" + e))
            for s in range(NDSEM):
                self.sems[("d", e, s)] = es.enter_context(nc.semaphore("d_%s_%d" % (e, s)))
        self.cnt = {e: 0 for e in ENGS}
        self.ndma = {e: 0 for e in ENGS}
        self.dma_hist = {e: [] for e in ENGS}
        self.hw = {e: {} for e in ENGS}
        self.nseq = 0
        self.limit = OPLIMIT
        self._reset()

    def _reset(self):
        self.streams = {e: [] for e in ENGS}
        self.all = []
        self.state = {}

    def _rec(self, ins, reads, writes, accumulate=False):
        if self.limit is not None and self.nseq >= self.limit:
            ins.deps = []
            ins.seq = self.nseq
            self.nseq += 1
            return ins
        deps = []
        for k in reads:
            st = self.state.get(k)
            if st:
                deps.extend(st[0])
        for k in writes:
            st = self.state.get(k)
            if st:
                deps.extend(st[1])
                deps.extend(st[0])
        out = []
        seen = set()
        for d in deps:
            if d is ins or id(d) in seen:
                continue
            seen.add(id(d))
            if (not d.is_dma) and (not ins.is_dma) and d.eng == ins.eng == "pe":
                continue
            out.append(d)
        last = {}
        red = []
        for d in out:
            if d.is_dma:
                red.append(d)
            elif d.eng not in last or d.seq > last[d.eng].seq:
                last[d.eng] = d
        red.extend(last.values())
        ins.deps = red
        ins.seq = self.nseq
        self.nseq += 1
        for k in reads:
            st = self.state.setdefault(k, [[], []])
            st[1].append(ins)
        for k in writes:
            st = self.state.setdefault(k, [[], []])
            if accumulate:
                st[0].append(ins)
            else:
                st[0] = [ins]
            st[1] = []
        self.all.append(ins)
        self.streams[ins.eng].append(ins)
        return ins

    def op(self, eng, fn, reads=(), writes=(), accumulate=False):
        rec = _Rec()
        fn(rec)
        name, a, kw = rec.call
        real = (lambda e, name=name, a=a, kw=kw: getattr(e, name)(*a, **kw))
        return self._rec(Ins(eng, real, False), list(reads), list(writes), accumulate)

    def dma(self, q, out, in_, reads=(), writes=()):
        ins = Ins(q, (lambda e, out=out, in_=in_: e.dma_start(out=out, in_=in_)), True)
        n = self.ndma[q]
        self.ndma[q] = n + 1
        ins.q_n = n
        ins.dslot = n % NDSEM
        ins.dval = 16 * (n // NDSEM + 1)
        hist = self.dma_hist[q]
        if self.limit is not None and self.nseq >= self.limit:
            self.ndma[q] = n
            self.nseq += 1
            return ins
        self._rec(ins, list(reads), list(writes))
        if n >= NDSEM:
            ins.deps.append(hist[n - NDSEM])
        hist.append(ins)
        return ins

    def flush(self):
        nc = self.nc
        for ins in self.all:
            for d in ins.deps:
                d.needed = True
        tails = []
        for e in ENGS:
            for ins in reversed(self.streams[e]):
                if not ins.is_dma:
                    ins.needed = True
                    tails.append(ins)
                    break
            hist = self.dma_hist[e]
            for ins in hist[max(0, len(hist) - NDSEM):]:
                tails.append(ins)
        for ins in self.all:
            if not ins.is_dma and ins.needed:
                self.cnt[ins.eng] += 1
                ins.ticket = self.cnt[ins.eng]
        hw = self.hw

        def key_val(d):
            if d.is_dma:
                return ("d", d.eng, d.dslot), d.dval
            return ("c", d.eng), d.ticket

        for ins in self.all:
            for d in ins.deps:
                key, val = key_val(d)
                if hw[ins.eng].get(key, 0) >= val:
                    continue
                hw[ins.eng][key] = val
                ins.waits.append((key, val))
        final = {e: [] for e in ENGS}
        for e in ENGS:
            for d in tails:
                key, val = key_val(d)
                if key == ("c", e) or hw[e].get(key, 0) >= val:
                    continue
                hw[e][key] = val
                final[e].append((key, val))
        sems = self.sems
        streams = self.streams

        def mk(ename):
            def body(eng):
                for ins in streams[ename]:
                    for key, val in ins.waits:
                        eng.wait_ge(sems[key], val)
                    r = ins.fn(eng)
                    if ins.is_dma:
                        r.then_inc(sems[("d", ename, ins.dslot)], 16)
                    elif ins.needed:
                        r.then_inc(sems[("c", ename)], 1)
                for key, val in final[ename]:
                    eng.wait_ge(sems[key], val)
            return body

        with nc.Block() as block:
            block.tensor(mk("pe"))
            block.scalar(mk("act"))
            block.vector(mk("dve"))
            block.gpsimd(mk("pool"))
            block.sync(mk("sp"))
        self._reset()


class Ring:
    def __init__(self, alloc, name, n, shape, dtype):
        self.tiles = [alloc("%s%d" % (name, i), shape, dtype) for i in range(n)]
        self.name = name
        self.i = 0

    def nxt(self):
        i = self.i % len(self.tiles)
        self.i += 1
        return self.tiles[i], (self.name, i)


def _col(vec):
    v = np.asarray(vec, np.float32).reshape(-1, 128)
    return np.ascontiguousarray(v.T)


PCOL = {}
PROW = {}


def _layout_tables():
    c = 0
    for name, n in (("scw", 48), ("scb", 12), ("sdd", 8), ("snw", 8), ("qw", 1), ("kw", 1),
                    ("fcw0", 132), ("fcb0", 44), ("fcw1", 132), ("fcb1", 44), ("mnw", 8)):
        PCOL[name] = (c, n)
        c += n
    PCOL["_n"] = c
    r = 0
    for name, n in (("nw00", 1024), ("nw01", 1024), ("nw10", 1024), ("nw11", 1024),
                    ("dtb", 16), ("alog", 16), ("mib", 8), ("mfb", 8)):
        PROW[name] = (r, n)
        r += n
    PROW["_n"] = r


_layout_tables()


def pack_params(inp):
    pc = np.zeros((128, PCOL["_n"]), np.float32)

    def put(name, arr):
        c0, n = PCOL[name]
        assert arr.shape == (128, n), (name, arr.shape)
        pc[:, c0:c0 + n] = arr

    cw = inp["ssd_conv_w"][0]
    put("scw", np.stack([_col(cw[k]) for k in range(4)], axis=2).reshape(128, 48))
    put("scb", _col(inp["ssd_conv_b"][0]))
    put("sdd", _col(np.repeat(inp["ssd_d"][0], 64)))
    put("snw", _col(inp["ssd_norm_w"][0]))
    put("qw", _col(np.tile(inp["sb_q_norm_w"][0], 2)))
    put("kw", _col(np.tile(inp["sb_k_norm_w"][0], 2)))
    for l in range(2):
        fw = inp["ffn_conv_w"][l]
        put("fcw%d" % l, np.stack([_col(fw[k]) for k in range(3)], axis=2).reshape(128, 132))
        put("fcb%d" % l, _col(inp["ffn_conv_b"][l]))
    put("mnw", _col(inp["ml_norm_w"][0]))
    pr = np.zeros((PROW["_n"],), np.float32)

    def putr(name, v):
        r0, n = PROW[name]
        pr[r0:r0 + n] = np.asarray(v, np.float32).reshape(n)

    putr("nw00", inp["norm_w"][0, 0])
    putr("nw01", inp["norm_w"][0, 1])
    putr("nw10", inp["norm_w"][1, 0])
    putr("nw11", inp["norm_w"][1, 1])
    putr("dtb", inp["ssd_dt_bias"][0])
    putr("alog", inp["ssd_a_log"][0])
    putr("mib", inp["ml_i_b"][0])
    putr("mfb", inp["ml_f_b"][0])
    prow = np.ascontiguousarray(np.broadcast_to(pr[None, :], (128, pr.size)))
    return pc, prow


def make_consts():
    i = np.arange(128)
    eye = (i[:, None] == i[None, :]).astype(np.float32)
    le = (i[:, None] <= i[None, :]).astype(np.float32)
    gt = (i[:, None] > i[None, :]).astype(np.float32)
    ge = (i[:, None] >= i[None, :]).astype(np.float32)
    ones = np.ones((128, 128), np.float32)
    bd = ((i[:, None] // 64) == (i[None, :] // 64)).astype(np.float32)
    negbig = -30000.0 * eye
    negu = -ge
    return np.ascontiguousarray(np.concatenate([eye, le, gt, ge, ones, bd, negbig, negu], axis=1))


CI = {"eye": 0, "le": 1, "gt": 2, "ge": 3, "ones": 4, "bd": 5, "negbig": 6, "negu": 7}


class K:
    pass


def build(nphase=99, dbg=False):
    nc = bass.Bass("TRN2", target_bir_lowering=False)
    k = K()
    k.nc = nc
    k.sfx = ""
    ein = lambda n, s, d=F32: nc.dram_tensor(n, s, d, kind="ExternalInput").ap()
    skind = "ExternalOutput" if dbg else "Internal"
    scr = lambda n, s, d: nc.dram_tensor(n, s, d, kind=skind).ap()
    k.x = ein("x", [NTOK, D])
    k.pcol_d = ein("pcol", [128, PCOL["_n"]])
    k.prow_d = ein("prow", [128, PROW["_n"]])
    k.consts_d = ein("consts", [128, 8 * 128])
    k.hy_in_w = ein("hy_in_w", [D, HYC])
    k.hy_out_w = ein("hy_out_w", [2048, D])
    k.ml_in_w = ein("ml_in_w", [D, MLC])
    k.ml_out_w = ein("ml_out_w", [D, D])
    k.ffn_up_w = ein("ffn_up_w", [2, D, 2 * DFF])
    k.ffn_down_w = ein("ffn_down_w", [2, DFF, D])
    k.out = nc.dram_tensor("out", [NTOK, D], F32, kind="ExternalOutput").ap()
    k.szT = scr("szT", [1024, NTOK], BF16)
    k.xbcT = scr("xbcT", [1536, NTOK], BF16)
    k.dts = scr("dts", [NTOK, 16], F32)
    k.qT = scr("qT", [1024, NTOK], BF16)
    k.kT = scr("kT", [1024, NTOK], BF16)
    k.vtok = scr("vtok", [NTOK, 1024], BF16)
    k.ycatT = scr("ycatT", [2048, NTOK], BF16)
    k.h1 = scr("h1", [NTOK, D], F32)
    k.h2 = scr("h2", [NTOK, D], F32)
    k.h3 = scr("h3", [NTOK, D], F32)
    k.gT = scr("gT", [DFF, NTOK], BF16)
    k.mqT = scr("mqT", [512, NTOK], BF16)
    k.mkT = scr("mkT", [512, NTOK], BF16)
    k.soT = scr("soT", [1024, NTOK], BF16)
    k.mkt = scr("mkt", [NTOK, 512], BF16)
    k.mvt = scr("mvt", [NTOK, 1024], BF16)
    k.gts = scr("gts", [NTOK, 16], F32)
    k.hmT = scr("hmT", [1024, NTOK], BF16)

    with ExitStack() as es:
        S = Sched(nc, es)
        k.S = S
        sb = lambda n, s, d: es.enter_context(nc.sbuf_tensor(n, s, d))
        k.cf = sb("cf", [128, 8 * 128], F32)
        k.cb = sb("cb", [128, 8 * 128], BF16)
        k.pcol = sb("pcolt", [128, PCOL["_n"]], F32)
        k.prow = sb("prowt", [128, PROW["_n"]], F32)
        S.dma("sp", k.cf[:], k.consts_d, writes=["cf"])
        S.dma("pool", k.cb[:], k.consts_d, writes=["cb"])
        S.dma("sp", k.pcol[:], k.pcol_d, writes=["pcol"])
        S.dma("sp", k.prow[:], k.prow_d, writes=["prow"])
        k.CF = lambda name: k.cf[:, CI[name] * 128:(CI[name] + 1) * 128]
        k.CB = lambda name: k.cb[:, CI[name] * 128:(CI[name] + 1) * 128]
        k.PC = lambda name, i=0, n=1: k.pcol[:, PCOL[name][0] + i:PCOL[name][0] + i + n]
        k.PR = lambda name: k.prow[:, PROW[name][0]:PROW[name][0] + PROW[name][1]]

        phases = [phase_a0, phase_b0, phase_c0,
                  lambda k, pes: phase_proj_res(k, pes, k.hy_out_w, 16, k.ycatT, k.x, k.h1),
                  lambda k, pes: phase_ffn_up(k, pes, 0, k.h1, k.gT),
                  lambda k, pes: phase_proj_res(k, pes, k.ffn_down_w[0], 22, k.gT, k.h1, k.h2),
                  phase_e1, phase_f1,
                  lambda k, pes: phase_proj_res(k, pes, k.ml_out_w, 8, k.hmT, k.h2, k.h3),
                  lambda k, pes: phase_ffn_up(k, pes, 1, k.h3, k.gT),
                  lambda k, pes: phase_proj_res(k, pes, k.ffn_down_w[1], 22, k.gT, k.h3, k.out)]
        for pi, ph in enumerate(phases[:nphase]):
            k.sfx = "_p%d" % pi
            with ExitStack() as pes:
                ph(k, pes)
                S.flush()
    return nc


def norm_transpose(k, S, xt, xkey, nw, hn_ring, hnT, hnT_key, ptr_ring, junk, small):
    ssq, lnv, rstd = small
    for j in range(4):
        S.op("act", lambda e, j=j: e.activation(out=junk[:], in_=xt[:, j, :], func=AF.Square,
                                                accum_out=ssq[:, j:j + 1]),
             reads=[xkey], writes=["junk", "ssq"])
    S.op("act", lambda e: e.activation(out=lnv[:], in_=ssq[:], func=AF.Ln, bias=EPS, scale=1.0 / D),
         reads=["ssq"], writes=["lnv"])
    S.op("act", lambda e: e.activation(out=rstd[:], in_=lnv[:], func=AF.Exp, scale=-0.5),
         reads=["lnv"], writes=["rstd"])
    hn, hkey = hn_ring.nxt()
    for j in range(4):
        S.op("dve", lambda e, j=j: e.scalar_tensor_tensor(out=hn[:, j, :], in0=xt[:, j, :], scalar=rstd[:, j:j + 1],
                                                          in1=nw, op0=ALU.mult, op1=ALU.mult),
             reads=[xkey, "rstd", "prow"], writes=[(hkey, j)])
    for j in range(4):
        ptr, pkey = ptr_ring.nxt()
        for kc in range(8):
            S.op("pe", lambda e, j=j, kc=kc, ptr=ptr: e.transpose(out=ptr[:, kc * 128:(kc + 1) * 128],
                                                                  in_=hn[:, j, kc * 128:(kc + 1) * 128],
                                                                  identity=k.CB("eye")),
                 reads=[(hkey, j), "cb"], writes=[pkey], accumulate=(kc > 0))
        eng = "act" if j % 2 == 0 else "dve"
        if eng == "act":
            S.op("act", lambda e, j=j, ptr=ptr: e.copy(hnT[:, :, j * 128:(j + 1) * 128],
                                                       ptr[:].rearrange("p (c t) -> p c t", t=128)),
                 reads=[pkey], writes=[hnT_key])
        else:
            S.op("dve", lambda e, j=j, ptr=ptr: e.tensor_copy(hnT[:, :, j * 128:(j + 1) * 128],
                                                              ptr[:].rearrange("p (c t) -> p c t", t=128)),
                 reads=[pkey], writes=[hnT_key])


def phase_a0(k, es):
    nc, S = k.nc, k.S
    sb = lambda n, s, d: es.enter_context(nc.sbuf_tensor(n + k.sfx, s, d))
    ps = lambda n, s, d: es.enter_context(nc.psum_tensor(n + k.sfx, s, d))
    win = sb("win", [128, 8, HYC], BF16)
    BW = 706

    def wk(kc, c0, c1):
        return [("win", kc, b) for b in range(c0 // BW, (c1 - 1) // BW + 1)]

    for b in range(8):
        for kc in range(8):
            S.dma("pool", win[:, kc, b * BW:(b + 1) * BW], k.hy_in_w[kc * 128:(kc + 1) * 128, b * BW:(b + 1) * BW],
                  writes=[("win", kc, b)])
    xt_ring = Ring(sb, "xt", 2, [128, 4, D], F32)
    hn_ring = Ring(sb, "hn", 1, [128, 4, D], BF16)
    hnT_ring = Ring(sb, "hnT", 2, [128, 8, 512], BF16)
    ptr_ring = Ring(ps, "ptr", 1, [128, 1024], BF16)
    pb_ring = Ring(ps, "pb", 5, [128, 512], F32)
    pss_ring = Ring(ps, "pss", 2, [128, 512], F32)
    junk = sb("junk", [128, D], BF16)
    small = (sb("ssq", [128, 4], F32), sb("lnv", [128, 4], F32), sb("rstd", [128, 4], F32))
    ob_ring = Ring(sb, "ob", 4, [128, 512], BF16)
    raw_ring = Ring(sb, "raw", 3, [128, 515], F32)
    acc_ring = Ring(sb, "acc", 4, [128, 512], F32)
    sq_ring = Ring(sb, "sqb", 3, [128, 512], BF16)
    ta_ring = Ring(sb, "ta", 2, [128, 512], F32)
    tb_ring = Ring(sb, "tb", 3, [128, 512], F32)
    halo = sb("halo", [128, 12, 3], F32)
    qws = sb("qws", [128, 1], F32)
    dtt_ring = Ring(sb, "dtt", 2, [128, 4, 16], F32)
    dte = sb("dte", [128, 64], F32)
    S.op("act", lambda e: e.mul(qws[:], k.PC("qw"), 0.125), reads=["pcol"], writes=["qws"])

    def load_x(tt):
        xt, xkey = xt_ring.nxt()
        S.dma("sp", xt[:], k.x[tt * 512:(tt + 1) * 512, :].rearrange("(j p) d -> p j d", p=128), writes=[xkey])
        return xt, xkey

    def prologue(xx):
        xt, xkey = xx
        hnT, hkey = hnT_ring.nxt()
        norm_transpose(k, S, xt, xkey, k.PR("nw00"), hn_ring, hnT, hkey, ptr_ring, junk, small)
        return hnT, hkey

    xs = {0: load_x(0)}
    if NTT > 1:
        xs[1] = load_x(1)
    pro = {0: prologue(xs[0])}
    for tt in range(NTT):
        if tt + 2 < NTT:
            xs[tt + 2] = load_x(tt + 2)
        hnT, hkey = pro[tt]
        tok0 = tt * 512
        seq_start = (tt % 4 == 0)

        def proj_fm(col0):
            pb, pkey = pb_ring.nxt()
            for kc in range(8):
                S.op("pe", lambda e, kc=kc, pb=pb: e.matmul(pb[:], lhsT=win[:, kc, col0:col0 + 128], rhs=hnT[:, kc, :],
                                                            start=(kc == 0), stop=(kc == 7)),
                     reads=[hkey] + wk(kc, col0, col0 + 128), writes=[pkey], accumulate=(kc > 0))
            return pb, pkey

        for c in range(8):
            pb, pkey = proj_fm(c * 128)
            ob, okey = ob_ring.nxt()
            S.op("act", lambda e, pb=pb, ob=ob: e.activation(out=ob[:], in_=pb[:], func=AF.Silu),
                 reads=[pkey], writes=[okey])
            S.dma("sp", k.szT[c * 128:(c + 1) * 128, tok0:tok0 + 512], ob[:], reads=[okey])
        if DBGPRINT: print('a0 after z', S.nseq)
        def x1(c):
            pb, pkey = proj_fm(1024 + c * 128)
            raw, rkey = raw_ring.nxt()
            acc, akey = acc_ring.nxt()
            if seq_start:
                S.op("pool", lambda e: e.memset(raw[:, 0:3], 0.0), writes=[(rkey, "h")])
            else:
                S.op("pool", lambda e: e.tensor_copy(raw[:, 0:3], halo[:, c, :]), reads=[("halo", c)],
                     writes=[(rkey, "h")])
            S.op("act", lambda e: e.copy(raw[:, 3:515], pb[:]), reads=[pkey], writes=[rkey])
            S.op("act", lambda e: e.activation(out=acc[:], in_=pb[:], func=AF.Identity, bias=k.PC("scb", c),
                                               scale=k.PC("scw", c * 4 + 3)), reads=[pkey, "pcol"], writes=[akey])
            return (raw, rkey, acc, akey, c)

        def xtap(t, tp):
            raw, rkey, acc, akey, c = t
            S.op("dve", lambda e: e.scalar_tensor_tensor(out=acc[:], in0=raw[:, tp:tp + 512],
                                                         scalar=k.PC("scw", c * 4 + tp), in1=acc[:], op0=ALU.mult,
                                                         op1=ALU.add), reads=[rkey, (rkey, "h"), akey, "pcol"], writes=[akey])

        def xhalo(t):
            raw, rkey, acc, akey, c = t
            S.op("pool", lambda e: e.tensor_copy(halo[:, c, :], raw[:, 512:515]), reads=[rkey], writes=[("halo", c)])

        def x3(t):
            raw, rkey, acc, akey, c = t
            ob, okey = ob_ring.nxt()
            S.op("act", lambda e: e.activation(out=ob[:], in_=acc[:], func=AF.Silu), reads=[akey], writes=[okey])
            S.dma("sp", k.xbcT[c * 128:(c + 1) * 128, tok0:tok0 + 512], ob[:], reads=[okey])

        its = {}
        for n in range(12 + 3):
            if n < 12:
                its[n] = x1(n)
            a_ok, b_ok = 0 <= n - 1 < 12, 0 <= n - 2 < 12
            if a_ok:
                xtap(its[n - 1], 0)
            if b_ok:
                xtap(its[n - 2], 2)
                xhalo(its[n - 2])
            if a_ok:
                xtap(its[n - 1], 1)
            if 0 <= n - 3 < 12:
                x3(its[n - 3])
        if tt + 1 < NTT:
            pro[tt + 1] = prologue(xs[tt + 1])

        def q1(n):
            which, c = n // 8, n % 8
            pb, pkey = proj_fm((2576 if which == 0 else 3600) + c * 128)
            sq, sqkey = sq_ring.nxt()
            S.op("act", lambda e: e.activation(out=sq[:], in_=pb[:], func=AF.Square), reads=[pkey], writes=[sqkey])
            return {"pb": pb, "pkey": pkey, "sq": sq, "sqkey": sqkey, "which": which, "c": c}

        def q2(t):
            pss, psskey = pss_ring.nxt()
            sq, sqkey = t["sq"], t["sqkey"]
            S.op("pe", lambda e: e.matmul(pss[:], lhsT=k.CB("bd"), rhs=sq[:], start=True, stop=True),
                 reads=[sqkey, "cb"], writes=[psskey])
            ta, takey = ta_ring.nxt()
            tb, tbkey = tb_ring.nxt()
            S.op("act", lambda e: e.activation(out=ta[:], in_=pss[:], func=AF.Ln, bias=EPS, scale=1.0 / 64),
                 reads=[psskey], writes=[takey])
            S.op("act", lambda e: e.activation(out=tb[:], in_=ta[:], func=AF.Exp, scale=-0.5), reads=[takey], writes=[tbkey])
            t["tb"], t["tbkey"] = tb, tbkey

        def q3(t):
            ob, okey = ob_ring.nxt()
            pb, pkey, tb, tbkey, which, c = t["pb"], t["pkey"], t["tb"], t["tbkey"], t["which"], t["c"]
            wcol = qws[:, 0:1] if which == 0 else k.PC("kw")
            S.op("dve", lambda e: e.scalar_tensor_tensor(out=ob[:], in0=pb[:], scalar=wcol, in1=tb[:], op0=ALU.mult,
                                                         op1=ALU.mult), reads=[pkey, tbkey, "qws", "pcol"], writes=[okey])
            dst = k.qT if which == 0 else k.kT
            S.dma("sp", dst[c * 128:(c + 1) * 128, tok0:tok0 + 512], ob[:], reads=[okey])

        its = {}
        for n in range(16 + 2):
            if n < 16:
                its[n] = q1(n)
            if 0 <= n - 1 < 16:
                q2(its[n - 1])
            if 0 <= n - 2 < 16:
                q3(its[n - 2])
        if DBGPRINT: print('a0 after qk', S.nseq)
        pb, pkey = pb_ring.nxt()
        for j in range(4):
            for kc in range(8):
                S.op("pe", lambda e, j=j, kc=kc, pb=pb: e.matmul(pb[:, j * 16:(j + 1) * 16],
                                                                lhsT=hnT[:, kc, j * 128:(j + 1) * 128],
                                                                rhs=win[:, kc, 2560:2576], start=(kc == 0), stop=(kc == 7)),
                     reads=[hkey] + wk(kc, 2560, 2576), writes=[pkey], accumulate=(j + kc > 0))
        dtt, dkey = dtt_ring.nxt()
        S.op("dve", lambda e, pb=pb: e.tensor_tensor(dte[:].rearrange("p (j h) -> p j h", h=16),
                                                     pb[:, 0:64].rearrange("p (j h) -> p j h", h=16),
                                                     k.PR("dtb").unsqueeze(1).to_broadcast([128, 4, 16]), op=ALU.add),
             reads=[pkey, "prow"], writes=["dte"])
        S.op("act", lambda e: e.activation(out=dte[:], in_=dte[:], func=AF.Exp), reads=["dte"], writes=["dte"])
        S.op("act", lambda e, dtt=dtt: e.activation(out=dtt[:].rearrange("p j h -> p (j h)"), in_=dte[:], func=AF.Ln,
                                                    bias=1.0),
             reads=["dte"], writes=[dkey])
        S.dma("sp", k.dts[tok0:tok0 + 512, :].rearrange("(j p) h -> p j h", p=128), dtt[:], reads=[dkey])
        if DBGPRINT: print('a0 after dt', S.nseq)
        for j in range(4):
            for half in range(2):
                pb, pkey = pb_ring.nxt()
                for kc in range(8):
                    S.op("pe", lambda e, j=j, kc=kc, pb=pb, half=half: e.matmul(
                        pb[:], lhsT=hnT[:, kc, j * 128:(j + 1) * 128],
                        rhs=win[:, kc, 4624 + half * 512:4624 + (half + 1) * 512], start=(kc == 0), stop=(kc == 7)),
                        reads=[hkey] + wk(kc, 4624 + half * 512, 4624 + (half + 1) * 512), writes=[pkey],
                        accumulate=(kc > 0))
                ob, okey = ob_ring.nxt()
                if half == 0:
                    S.op("act", lambda e, pb=pb, ob=ob: e.copy(ob[:], pb[:]), reads=[pkey], writes=[okey])
                else:
                    S.op("dve", lambda e, pb=pb, ob=ob: e.tensor_copy(ob[:], pb[:]), reads=[pkey], writes=[okey])
                S.dma("sp", k.vtok[tok0 + j * 128:tok0 + (j + 1) * 128, half * 512:(half + 1) * 512], ob[:],
                      reads=[okey])


def phase_b0(k, es):
    nc, S = k.nc, k.S
    sb = lambda n, s, d: es.enter_context(nc.sbuf_tensor(n + k.sfx, s, d))
    ps = lambda n, s, d: es.enter_context(nc.psum_tensor(n + k.sfx, s, d))
    CF, CB, PC, PR = k.CF, k.CB, k.PC, k.PR
    xb_ring = Ring(sb, "xb", 2, [128, 12, 512], BF16)
    sz_ring = Ring(sb, "szt", 2, [128, 8, 512], BF16)
    dtl_ring = Ring(sb, "dtl", 2, [128, 4, 16], F32)
    yo_ring = Ring(sb, "yo", 2, [128, 8, 512], BF16)
    da_ring = Ring(sb, "da", 3, [128, 16], F32)
    sm_ring = Ring(sb, "sm", 3, [128, 48], F32)
    xtil_ring = Ring(sb, "xtil", 3, [128, 1024], BF16)
    xtd_ring = Ring(sb, "xtd", 3, [128, 1024], BF16)
    bmt_ring = Ring(sb, "bmt", 3, [128, 256], BF16)
    cbm_ring = Ring(sb, "cbm", 3, [128, 2, 128], F32)
    dec_ring = Ring(sb, "dec", 2, [128, 512], F32)
    eb_ring = Ring(sb, "eb", 2, [128, 512], F32)
    mt_ring = Ring(sb, "mt", 9, [128, 512], BF16)
    ce_ring = Ring(sb, "ce", 9, [128, 512], BF16)
    gv_ring = Ring(sb, "gv", 3, [128, 8, 128], F32)
    sq_ring = Ring(sb, "ssq2", 3, [128, 8, 128], BF16)
    rs_ring = Ring(sb, "rs", 2, [128, 128], F32)
    ln_ring = Ring(sb, "lnb", 2, [128, 128], F32)
    ab = sb("ab", [128, 16], F32)
    g4 = sb("g4", [128, 512], F32)
    prev_f = sb("prev_f", [128, 1024], F32)
    prev_b = sb("prev_b", [128, 1024], BF16)
    pmA_ring = Ring(ps, "pmA", 1, [128, 512], F32)
    ptx_ring = Ring(ps, "ptx", 1, [128, 1024], BF16)
    ptb_ring = Ring(ps, "ptb", 1, [128, 256], BF16)
    pw_ring = Ring(ps, "pw", 3, [128, 512], F32)
    py_ring = Ring(ps, "py", 2, [128, 512], F32)

    S.op("act", lambda e: e.activation(out=ab[:], in_=PR("alog"), func=AF.Exp), reads=["prow"], writes=["ab"])
    S.op("act", lambda e: e.mul(ab[:], ab[:], -1.0), reads=["ab"], writes=["ab"])
    for q in range(4):
        S.op("pool", lambda e, q=q: e.tensor_copy(g4[:, q * 128:(q + 1) * 128], CF("gt")), reads=["cf"], writes=["g4"],
             accumulate=(q > 0))

    def load(tt):
        tok0 = tt * 512
        xb, xkey = xb_ring.nxt()
        szt, skey = sz_ring.nxt()
        dtl, dkey = dtl_ring.nxt()
        S.dma("sp", xb[:], k.xbcT[:, tok0:tok0 + 512].rearrange("(c p) t -> p c t", p=128), writes=[xkey])
        S.dma("sp", szt[:], k.szT[:, tok0:tok0 + 512].rearrange("(c p) t -> p c t", p=128), writes=[skey])
        S.dma("sp", dtl[:], k.dts[tok0:tok0 + 512, :].rearrange("(j p) h -> p j h", p=128), writes=[dkey])
        return xb, xkey, szt, skey, dtl, dkey

    tiles = {}

    def get_tile(tt):
        if tt not in tiles:
            tiles[tt] = load(tt) + yo_ring.nxt()
        return tiles[tt]

    def P(c):
        xb, xkey, szt, skey, dtl, dkey, yo, yokey = get_tile(c["tt"])
        j = c["j"]
        js = slice(j * 128, (j + 1) * 128)
        da, dakey = da_ring.nxt()
        S.op("dve", lambda e: e.tensor_tensor(da[:], dtl[:, j, :], ab[:], op=ALU.mult), reads=[dkey, "ab"], writes=[dakey])
        pmA, pmkey = pmA_ring.nxt()
        for i, cname in enumerate(("le", "gt", "ones")):
            S.op("pe", lambda e: e.matmul(pmA[:, i * 16:(i + 1) * 16], lhsT=CF(cname), rhs=da[:], start=True, stop=True),
                 reads=[dakey, "cf"], writes=[(pmkey, "c")], accumulate=(i > 0))
        sm, smkey = sm_ring.nxt()
        S.op("act", lambda e: e.mul(sm[:, 0:16], pmA[:, 0:16], -1.0), reads=[(pmkey, "c")], writes=[smkey])
        S.op("act", lambda e: e.activation(out=sm[:, 16:48], in_=pmA[:, 16:48], func=AF.Exp), reads=[(pmkey, "c")],
             writes=[smkey], accumulate=True)
        ptx, ptxkey = ptx_ring.nxt()
        for fc in range(8):
            S.op("pe", lambda e: e.transpose(out=ptx[:, fc * 128:(fc + 1) * 128], in_=xb[:, fc, js], identity=CB("eye")),
                 reads=[xkey, "cb"], writes=[ptxkey], accumulate=(fc > 0))
        xtil, xtkey = xtil_ring.nxt()
        xtd, xdkey = xtd_ring.nxt()
        S.op("dve", lambda e: e.tensor_tensor(
            xtil[:].rearrange("p (h d) -> p h d", d=64), ptx[:].rearrange("p (h d) -> p h d", d=64),
            dtl[:, j, :].unsqueeze(2).to_broadcast([128, 16, 64]), op=ALU.mult), reads=[ptxkey, dkey], writes=[xtkey])
        S.op("pool", lambda e: e.tensor_tensor(
            xtd[:].rearrange("p (h d) -> p h d", d=64), xtil[:].rearrange("p (h d) -> p h d", d=64),
            sm[:, 16:32].unsqueeze(2).to_broadcast([128, 16, 64]), op=ALU.mult), reads=[xtkey, smkey], writes=[xdkey])
        ptb, ptbkey = ptb_ring.nxt()
        for g in range(2):
            S.op("pe", lambda e: e.transpose(out=ptb[:, g * 128:(g + 1) * 128], in_=xb[:, 8 + g, js], identity=CB("eye")),
                 reads=[xkey, "cb"], writes=[ptbkey], accumulate=(g > 0))
        bmt, bmkey = bmt_ring.nxt()
        S.op("act", lambda e: e.copy(bmt[:], ptb[:]), reads=[ptbkey], writes=[bmkey])
        for g in range(2):
            S.op("pe", lambda e: e.matmul(pmA[:, 64 + g * 128:64 + (g + 1) * 128], lhsT=xb[:, 8 + g, js],
                                          rhs=xb[:, 10 + g, js], start=True, stop=True),
                 reads=[xkey], writes=[(pmkey, "cb")], accumulate=(g > 0))
        cbm, cbkey = cbm_ring.nxt()
        S.op("dve", lambda e: e.tensor_tensor(cbm[:], pmA[:, 64:320].rearrange("p (g t) -> p g t", t=128),
                                              CF("le").unsqueeze(1).to_broadcast([128, 2, 128]), op=ALU.mult),
             reads=[(pmkey, "cb"), "cf"], writes=[cbkey])
        c.update(da=da, dakey=dakey, pmA=pmA, pmkey=pmkey, sm=sm, smkey=smkey, xtil=xtil, xtkey=xtkey, xtd=xtd,
                 xdkey=xdkey, bmt=bmt, bmkey=bmkey, cbm=cbm, cbkey=cbkey)
        c["mt"], c["ce"] = [], []
        for hq in range(4):
            g = hq // 2
            pam, pamkey = pw_ring.nxt()
            pau, paukey = pw_ring.nxt()
            S.op("pe", lambda e: e.matmul(pam[:], lhsT=CF("negbig"), rhs=g4[:], start=True, stop=False),
                 reads=["cf", "g4"], writes=[pamkey])
            for hh in range(4):
                h = hq * 4 + hh
                cs = slice(hh * 128, (hh + 1) * 128)
                lb = da[:, h:h + 1].to_broadcast([128, 128])
                S.op("pe", lambda e: e.matmul(pam[:, cs], lhsT=lb, rhs=CF("le"), start=False, stop=True),
                     reads=[dakey, "cf"], writes=[pamkey], accumulate=True)
                S.op("pe", lambda e: e.matmul(pau[:, cs], lhsT=lb, rhs=CF("le"), start=True, stop=True),
                     reads=[dakey, "cf"], writes=[paukey], accumulate=(hh > 0))
            eb, ebkey = eb_ring.nxt()
            S.op("act", lambda e: e.activation(out=eb[:], in_=pau[:], func=AF.Exp), reads=[paukey], writes=[ebkey])
            dec, deckey = dec_ring.nxt()
            for hh in range(4):
                h = hq * 4 + hh
                cs = slice(hh * 128, (hh + 1) * 128)
                S.op("act", lambda e: e.activation(out=dec[:, cs], in_=pam[:, cs], func=AF.Exp, bias=sm[:, h:h + 1]),
                     reads=[pamkey, smkey], writes=[deckey], accumulate=(hh > 0))
            mt, mtkey = mt_ring.nxt()
            S.op("dve", lambda e: e.tensor_tensor(mt[:].rearrange("p (h t) -> p h t", t=128),
                                                  dec[:].rearrange("p (h t) -> p h t", t=128),
                                                  cbm[:, g, :].unsqueeze(1).to_broadcast([128, 4, 128]), op=ALU.mult),
                 reads=[deckey, cbkey], writes=[mtkey])
            ce, cekey = ce_ring.nxt()
            S.op("pool", lambda e: e.tensor_tensor(ce[:].rearrange("p (h t) -> p h t", t=128),
                                                   eb[:].rearrange("p (h t) -> p h t", t=128),
                                                   xb[:, 10 + g, js].unsqueeze(1).to_broadcast([128, 4, 128]), op=ALU.mult),
                 reads=[xkey, ebkey], writes=[cekey])
            c["mt"].append((mt, mtkey))
            c["ce"].append((ce, cekey))

    def H(c):
        xtil, xtkey = c["xtil"], c["xtkey"]
        if c["tt"] % 4 == 0 and c["j"] == 0:
            S.op("pool", lambda e: e.memset(prev_f[:], 0.0), writes=["prev_f"])
            S.op("pool", lambda e: e.memset(prev_b[:], 0.0), writes=["prev_b"])
        pys = [py_ring.nxt(), py_ring.nxt()]
        c["pys"] = pys
        for h in range(16):
            mt, mtkey = c["mt"][h // 4]
            ce, cekey = c["ce"][h // 4]
            cs = slice((h % 4) * 128, (h % 4 + 1) * 128)
            py, pykey = pys[h // 8]
            fcl = (h // 2) % 4
            po = (h % 2) * 64
            outap = py[po:po + 64, fcl * 128:(fcl + 1) * 128]
            S.op("pe", lambda e: e.matmul(outap, lhsT=xtil[:, h * 64:(h + 1) * 64], rhs=mt[:, cs], start=True, stop=False),
                 reads=[xtkey, mtkey], writes=[pykey], accumulate=(h % 8 > 0))
            S.op("pe", lambda e: e.matmul(outap, lhsT=prev_b[:, h * 64:(h + 1) * 64], rhs=ce[:, cs], start=False, stop=True),
                 reads=["prev_b", cekey], writes=[pykey], accumulate=True)
        sm, smkey, bmt, bmkey, xtd, xdkey = c["sm"], c["smkey"], c["bmt"], c["bmkey"], c["xtd"], c["xdkey"]
        psts = [pw_ring.nxt(), pw_ring.nxt()]
        for g in range(2):
            pst, pstkey = psts[g]
            S.op("pe", lambda e: e.matmul(pst[:], lhsT=bmt[:, g * 128:(g + 1) * 128], rhs=xtd[:, g * 512:(g + 1) * 512],
                                          start=True, stop=True), reads=[bmkey, xdkey], writes=[pstkey])
        S.op("dve", lambda e: e.tensor_tensor(
            prev_f[:].rearrange("p (h d) -> p h d", d=64), prev_f[:].rearrange("p (h d) -> p h d", d=64),
            sm[:, 32:48].unsqueeze(2).to_broadcast([128, 16, 64]), op=ALU.mult),
            reads=["prev_f", smkey], writes=["prev_f"])
        for g in range(2):
            pst, pstkey = psts[g]
            S.op("dve", lambda e: e.tensor_tensor(prev_f[:, g * 512:(g + 1) * 512], prev_f[:, g * 512:(g + 1) * 512],
                                                  pst[:], op=ALU.add), reads=["prev_f", pstkey], writes=["prev_f"])
        S.op("act", lambda e: e.copy(prev_b[:], prev_f[:]), reads=["prev_f"], writes=["prev_b"])

    def E(c):
        xb, xkey, szt, skey, dtl, dkey, yo, yokey = get_tile(c["tt"])
        j = c["j"]
        js = slice(j * 128, (j + 1) * 128)
        pys, pmA, pmkey = c["pys"], c["pmA"], c["pmkey"]
        gv, gvkey = gv_ring.nxt()
        for fc in range(8):
            py, pykey = pys[fc // 4]
            S.op("dve", lambda e: e.scalar_tensor_tensor(
                out=gv[:, fc, :], in0=xb[:, fc, js], scalar=PC("sdd", fc), in1=py[:, (fc % 4) * 128:(fc % 4 + 1) * 128],
                op0=ALU.mult, op1=ALU.add), reads=[xkey, pykey, "pcol"], writes=[gvkey], accumulate=(fc > 0))
        S.op("pool", lambda e: e.tensor_tensor(gv[:], gv[:], szt[:, :, js], op=ALU.mult), reads=[gvkey, skey],
             writes=[gvkey])
        sq, sqkey = sq_ring.nxt()
        S.op("act", lambda e: e.activation(out=sq[:], in_=gv[:], func=AF.Square), reads=[gvkey], writes=[sqkey])
        c.update(gv=gv, gvkey=gvkey, sq=sq, sqkey=sqkey)

    def E2(c):
        xb, xkey, szt, skey, dtl, dkey, yo, yokey = get_tile(c["tt"])
        j = c["j"]
        js = slice(j * 128, (j + 1) * 128)
        pmA, pmkey, gv, gvkey, sq, sqkey = c["pmA"], c["pmkey"], c["gv"], c["gvkey"], c["sq"], c["sqkey"]
        for fc in range(8):
            S.op("pe", lambda e: e.matmul(pmA[:, 384:512], lhsT=CB("ones"), rhs=sq[:, fc, :], start=(fc == 0),
                                          stop=(fc == 7)), reads=[sqkey, "cb"], writes=[(pmkey, "ss")], accumulate=(fc > 0))
        lnb, lnkey = ln_ring.nxt()
        rs, rskey = rs_ring.nxt()
        S.op("act", lambda e: e.activation(out=lnb[:], in_=pmA[:, 384:512], func=AF.Ln, bias=EPS, scale=1.0 / 1024),
             reads=[(pmkey, "ss")], writes=[lnkey])
        S.op("act", lambda e: e.activation(out=rs[:], in_=lnb[:], func=AF.Exp, scale=-0.5), reads=[lnkey], writes=[rskey])
        S.op("dve", lambda e: e.tensor_tensor(gv[:], gv[:], rs[:].unsqueeze(1).to_broadcast([128, 8, 128]), op=ALU.mult),
             reads=[gvkey, rskey], writes=[gvkey])
        S.op("pool", lambda e: e.tensor_tensor(yo[:, :, js], gv[:], PC("snw", 0, 8).unsqueeze(2).to_broadcast([128, 8, 128]),
                                               op=ALU.mult), reads=[gvkey, "pcol"], writes=[yokey], accumulate=(j > 0))
        if j == 3:
            tok0 = c["tt"] * 512
            S.dma("sp", k.ycatT[0:1024, tok0:tok0 + 512].rearrange("(c p) t -> p c t", p=128), yo[:], reads=[yokey])

    chunks = [{"tt": tt, "j": j} for tt in range(NTT) for j in range(4)]
    get_tile(0)
    P(chunks[0])
    for ci, c in enumerate(chunks):
        if c["j"] == 0 and c["tt"] + 1 < NTT:
            get_tile(c["tt"] + 1)
        H(c)
        if ci > 0:
            E2(chunks[ci - 1])
        if ci + 1 < len(chunks):
            P(chunks[ci + 1])
        E(c)
    E2(chunks[-1])


def phase_c0(k, es):
    nc, S = k.nc, k.S
    sb = lambda n, s, d: es.enter_context(nc.sbuf_tensor(n + k.sfx, s, d))
    ps = lambda n, s, d: es.enter_context(nc.psum_tensor(n + k.sfx, s, d))
    CF, CB = k.CF, k.CB
    q_ring = Ring(sb, "qp", 2, [128, L], BF16)
    k_ring = Ring(sb, "kp", 2, [128, L], BF16)
    vb_ring = Ring(sb, "vb", 2, [128, 16, 1024], BF16)
    e_ring = Ring(sb, "ee", 3, [128, 512], F32)
    sp_ring = Ring(sb, "spp", 5, [128, 512], BF16)
    bt_ring = Ring(sb, "btt", 3, [128, 512], BF16)
    r_ring = Ring(sb, "rr", 3, [128, 4], F32)
    accs = [sb("accA", [128, 4, 64], F32), sb("accB", [128, 4, 64], F32)]
    osb_ring = Ring(sb, "osb", 2, [128, 4, 128], BF16)
    oT_ring = Ring(sb, "oT", 2, [128, 512], BF16)
    pz_ring = Ring(ps, "pz", 2, [128, 512], F32)
    py_ring = Ring(ps, "pyy", 3, [128, 512], F32)
    pov_ring = Ring(ps, "pov", 2, [128, 4, 65], F32)
    ptr_ring = Ring(ps, "ptc", 1, [128, 512], BF16)

    for b in range(2):
        vb, vkey = vb_ring.nxt()
        for i_ in range(16):
            S.dma("sp", vb[:, i_, :], k.vtok[b * L + i_ * 128:b * L + (i_ + 1) * 128, :], writes=[(vkey, i_)])
        for hp in range(8):
            qp, qkey = q_ring.nxt()
            kp, kkey = k_ring.nxt()
            S.dma("sp", qp[:], k.qT[hp * 128:(hp + 1) * 128, b * L:(b + 1) * L], writes=[qkey])
            S.dma("sp", kp[:], k.kT[hp * 128:(hp + 1) * 128, b * L:(b + 1) * L], writes=[kkey])
            for g in range(4):
                items = []
                for i in range(4 * g + 4):
                    for hh in range(2):
                        items.append({"i": i, "hh": hh})
                osb, oskey = osb_ring.nxt()
                for hh in range(2):
                    S.op("pool", lambda e, hh=hh: e.memset(accs[hh][:], 0.0), writes=[("acc", hh)])

                def geom(it):
                    i = it["i"]
                    qlo = max(0, i - 4 * g)
                    n = (4 - qlo) * 128
                    t0 = (4 * g + qlo) * 128
                    po = it["hh"] * 64
                    return i, qlo, n, t0, po

                def zmm(it, pt, pkey):
                    i, qlo, n, t0, po = geom(it)
                    diag = i >= 4 * g
                    S.op("pe", lambda e: e.matmul(pt[:, 0:n], lhsT=kp[po:po + 64, i * 128:(i + 1) * 128],
                                                  rhs=qp[po:po + 64, t0:t0 + n], start=True, stop=False),
                         reads=[qkey, kkey], writes=[pkey])
                    if diag:
                        S.op("pe", lambda e: e.matmul(pt[:, 0:128], lhsT=CB("negbig"), rhs=CB("ge"), start=False,
                                                      stop=False), reads=["cb"], writes=[pkey], accumulate=True)

                def s1(it):
                    it["pz"], it["pzkey"] = pz_ring.nxt()
                    zmm(it, it["pz"], it["pzkey"])
                    return

                def s2(it):
                    i, qlo, n, t0, po = geom(it)
                    ee, ekey = e_ring.nxt()
                    sp, spkey = sp_ring.nxt()
                    it["sp"], it["spkey"] = sp, spkey
                    pz, pzkey = it["pz"], it["pzkey"]
                    S.op("act", lambda e: e.activation(out=ee[:, 0:n], in_=pz[:, 0:n], func=AF.Exp),
                         reads=[pzkey], writes=[ekey])
                    S.op("act", lambda e: e.activation(out=sp[:, 0:n], in_=ee[:, 0:n], func=AF.Ln, bias=1.0),
                         reads=[ekey], writes=[spkey])

                def s3(it):
                    i, qlo, n, t0, po = geom(it)
                    it["py"], it["pykey"] = py_ring.nxt()
                    zmm(it, it["py"], it["pykey"])
                    py, sp = it["py"], it["sp"]
                    S.op("pe", lambda e: e.matmul(py[:, 0:n], lhsT=CB("negu"), rhs=sp[:, 0:n], start=False, stop=True),
                         reads=[it["spkey"], "cb"], writes=[it["pykey"]], accumulate=True)

                def s4(it):
                    i, qlo, n, t0, po = geom(it)
                    bt, btkey = bt_ring.nxt()
                    it["bt"], it["btkey"] = bt, btkey
                    py = it["py"]
                    S.op("act", lambda e: e.activation(out=bt[:, 0:n], in_=py[:, 0:n], func=AF.Exp),
                         reads=[it["pykey"]], writes=[btkey])

                def s5(it):
                    i, qlo, n, t0, po = geom(it)
                    pov, povkey = pov_ring.nxt()
                    it["pov"], it["povkey"] = pov, povkey
                    bt, sp = it["bt"], it["sp"]
                    h = hp * 2 + it["hh"]
                    first = True
                    for qq in range(qlo, 4):
                        cs = slice((qq - qlo) * 128, (qq - qlo + 1) * 128)
                        S.op("pe", lambda e, qq=qq, cs=cs: e.matmul(pov[:, qq, 0:64], lhsT=bt[:, cs],
                                                                    rhs=vb[:, i, h * 64:(h + 1) * 64], start=True, stop=True),
                             reads=[it["btkey"], (vkey, i)], writes=[povkey], accumulate=(not first))
                        first = False
                        S.op("pe", lambda e, qq=qq, cs=cs: e.matmul(pov[:, qq, 64:65], lhsT=sp[:, cs], rhs=CB("ones")[:, 0:1],
                                                                    start=True, stop=True),
                             reads=[it["spkey"], "cb"], writes=[povkey], accumulate=True)

                def s6(it):
                    i, qlo, n, t0, po = geom(it)
                    rr, rkey = r_ring.nxt()
                    pov, povkey = it["pov"], it["povkey"]
                    hh = it["hh"]
                    acc = accs[hh]
                    nq = 4 - qlo
                    S.op("act", lambda e: e.activation(out=rr[:, qlo:4].unsqueeze(2), in_=pov[:, qlo:4, 64:65], func=AF.Exp,
                                                       scale=-1.0), reads=[povkey], writes=[rkey])
                    S.op("dve", lambda e: e.tensor_tensor(acc[:, qlo:4, :], acc[:, qlo:4, :],
                                                          rr[:, qlo:4].unsqueeze(2).to_broadcast([128, nq, 64]), op=ALU.mult),
                         reads=[rkey, ("acc", hh)], writes=[("acc", hh)])
                    S.op("dve", lambda e: e.tensor_tensor(acc[:, qlo:4, :], acc[:, qlo:4, :], pov[:, qlo:4, 0:64], op=ALU.add),
                         reads=[povkey, ("acc", hh)], writes=[("acc", hh)])

                stages = [s1, s2, s3, s4, s5, s6]
                lag = [0, 0, 1, 1, 2, 2]
                for n in range(len(items) + 2):
                    for st, lg in zip(stages, lag):
                        m = n - lg
                        if 0 <= m < len(items):
                            st(items[m])
                for hh in range(2):
                    S.op("pool", lambda e, hh=hh: e.tensor_copy(osb[:, :, hh * 64:(hh + 1) * 64], accs[hh][:]),
                         reads=[("acc", hh)], writes=[oskey], accumulate=(hh > 0))
                ptc, ptckey = ptr_ring.nxt()
                for qq in range(4):
                    S.op("pe", lambda e, qq=qq: e.transpose(out=ptc[:, qq * 128:(qq + 1) * 128], in_=osb[:, qq, :],
                                                            identity=CB("eye")),
                         reads=[oskey, "cb"], writes=[ptckey], accumulate=(qq > 0))
                oT, oTkey = oT_ring.nxt()
                S.op("dve", lambda e: e.tensor_copy(oT[:], ptc[:]), reads=[ptckey], writes=[oTkey])
                tok0 = b * L + g * 512
                S.dma("sp", k.ycatT[1024 + hp * 128:1024 + (hp + 1) * 128, tok0:tok0 + 512], oT[:], reads=[oTkey])


def phase_proj_res(k, es, w_dram, kc_n, srcT, h_in, h_out):
    nc, S = k.nc, k.S
    sb = lambda n, s, d: es.enter_context(nc.sbuf_tensor(n + k.sfx, s, d))
    ps = lambda n, s, d: es.enter_context(nc.psum_tensor(n + k.sfx, s, d))
    w = sb("wres", [128, kc_n, D], BF16)
    for c in range(kc_n):
        S.dma("pool", w[:, c, :], w_dram[c * 128:(c + 1) * 128, :], writes=[("wres", c)])
    src_ring = Ring(sb, "srct", 2, [128, kc_n, 512], BF16)
    h_ring = Ring(sb, "hres", 2, [128, 4, D], F32)
    po_ring = Ring(ps, "pres", 4, [128, 512], F32)

    def load(tt):
        src, skey = src_ring.nxt()
        ht, hkey = h_ring.nxt()
        S.dma("sp", src[:], srcT[:, tt * 512:(tt + 1) * 512].rearrange("(c p) t -> p c t", p=128), writes=[skey])
        S.dma("sp", ht[:], h_in[tt * 512:(tt + 1) * 512, :].rearrange("(j p) d -> p j d", p=128), writes=[hkey])
        return src, skey, ht, hkey

    nxt = load(0)
    for tt in range(NTT):
        src, skey, ht, hkey = nxt
        if tt + 1 < NTT:
            nxt = load(tt + 1)
        for j in range(4):
            for half in range(2):
                po, pkey = po_ring.nxt()
                for c in range(kc_n):
                    S.op("pe", lambda e: e.matmul(po[:], lhsT=src[:, c, j * 128:(j + 1) * 128],
                                                  rhs=w[:, c, half * 512:(half + 1) * 512], start=(c == 0),
                                                  stop=(c == kc_n - 1)),
                         reads=[skey, ("wres", c)], writes=[pkey], accumulate=(c > 0))
                S.op("dve", lambda e: e.tensor_tensor(ht[:, j, half * 512:(half + 1) * 512],
                                                      ht[:, j, half * 512:(half + 1) * 512], po[:], op=ALU.add),
                     reads=[pkey, hkey], writes=[hkey])
        S.dma("sp", h_out[tt * 512:(tt + 1) * 512, :].rearrange("(j p) d -> p j d", p=128), ht[:], reads=[hkey])


def phase_ffn_up(k, es, layer, h_in, gT):
    nc, S = k.nc, k.S
    sb = lambda n, s, d: es.enter_context(nc.sbuf_tensor(n + k.sfx, s, d))
    ps = lambda n, s, d: es.enter_context(nc.psum_tensor(n + k.sfx, s, d))
    PC, PR = k.PC, k.PR
    wup = sb("wup", [128, 8, 2 * DFF], BF16)
    for cb_ in range(4):
        for kc in range(8):
            S.dma("pool", wup[:, kc, cb_ * 1408:(cb_ + 1) * 1408],
                  k.ffn_up_w[layer, kc * 128:(kc + 1) * 128, cb_ * 1408:(cb_ + 1) * 1408], writes=[("wup", kc, cb_)])
    xt_ring = Ring(sb, "xtf", 2, [128, 4, D], F32)
    hn_ring = Ring(sb, "hnf", 1, [128, 4, D], BF16)
    hnT_ring = Ring(sb, "hnTf", 2, [128, 8, 512], BF16)
    ptr_ring = Ring(ps, "ptrf", 2, [128, 1024], BF16)
    pb_ring = Ring(ps, "pbf", 6, [128, 512], F32)
    junk = sb("junkf", [128, D], BF16)
    small = (sb("ssqf", [128, 4], F32), sb("lnvf", [128, 4], F32), sb("rstdf", [128, 4], F32))
    raw_ring = Ring(sb, "rawf", 6, [128, 514], F32)
    acc_ring = Ring(sb, "accf", 6, [128, 512], F32)
    sg_ring = Ring(sb, "sgf", 2, [128, 512], F32)
    ob_ring = Ring(sb, "obf", 3, [128, 512], BF16)
    halo = sb("halof", [128, 44, 2], F32)
    cwn, cbn, nwn = "fcw%d" % layer, "fcb%d" % layer, "nw%d1" % layer

    def load_x(tt):
        xt, xkey = xt_ring.nxt()
        S.dma("sp", xt[:], h_in[tt * 512:(tt + 1) * 512, :].rearrange("(j p) d -> p j d", p=128), writes=[xkey])
        return xt, xkey

    def prologue(xx):
        xt, xkey = xx
        hnT, hkey = hnT_ring.nxt()
        norm_transpose(k, S, xt, xkey, PR(nwn), hn_ring, hnT, hkey, ptr_ring, junk, small)
        return hnT, hkey

    xs = {0: load_x(0)}
    if NTT > 1:
        xs[1] = load_x(1)
    pro = {0: prologue(xs[0])}
    for tt in range(NTT):
        if tt + 2 < NTT:
            xs[tt + 2] = load_x(tt + 2)
        hnT, hkey = pro[tt]
        tok0 = tt * 512
        seq_start = (tt % 4 == 0)

        def st1(cc):
            pb, pkey = pb_ring.nxt()
            for kc in range(8):
                S.op("pe", lambda e: e.matmul(pb[:], lhsT=wup[:, kc, cc * 128:(cc + 1) * 128], rhs=hnT[:, kc, :],
                                              start=(kc == 0), stop=(kc == 7)),
                     reads=[hkey, ("wup", kc, (cc * 128) // 1408)], writes=[pkey], accumulate=(kc > 0))
            raw, rkey = raw_ring.nxt()
            acc, akey = acc_ring.nxt()
            if seq_start:
                S.op("pool", lambda e: e.memset(raw[:, 0:2], 0.0), writes=[(rkey, "h")])
            else:
                S.op("pool", lambda e: e.tensor_copy(raw[:, 0:2], halo[:, cc, :]), reads=[("halof", cc)],
                     writes=[(rkey, "h")])
            S.op("act", lambda e: e.copy(raw[:, 2:514], pb[:]), reads=[pkey], writes=[rkey])
            S.op("act", lambda e: e.activation(out=acc[:], in_=pb[:], func=AF.Identity, bias=PC(cbn, cc),
                                               scale=PC(cwn, cc * 3 + 2)), reads=[pkey, "pcol"], writes=[akey])
            return (raw, rkey, acc, akey, cc)

        def st2(ta, tb_):
            for tp in range(2):
                for (raw, rkey, acc, akey, cc) in (ta, tb_):
                    S.op("dve", lambda e: e.scalar_tensor_tensor(out=acc[:], in0=raw[:, tp:tp + 512],
                                                                 scalar=PC(cwn, cc * 3 + tp), in1=acc[:], op0=ALU.mult,
                                                                 op1=ALU.add), reads=[rkey, (rkey, "h"), akey, "pcol"],
                         writes=[akey])
            for (raw, rkey, acc, akey, cc) in (ta, tb_):
                S.op("pool", lambda e: e.tensor_copy(halo[:, cc, :], raw[:, 512:514]), reads=[rkey], writes=[("halof", cc)])

        def st3(tg, tv, c):
            ag, agkey = tg[2], tg[3]
            av, avkey = tv[2], tv[3]
            sg, sgkey = sg_ring.nxt()
            S.op("act", lambda e: e.activation(out=sg[:], in_=ag[:], func=AF.Silu), reads=[agkey], writes=[sgkey])
            ob, okey = ob_ring.nxt()
            S.op("pool", lambda e: e.tensor_tensor(ob[:], sg[:], av[:], op=ALU.mult), reads=[sgkey, avkey], writes=[okey])
            S.dma("sp", gT[c * 128:(c + 1) * 128, tok0:tok0 + 512], ob[:], reads=[okey])

        its = {}
        for n in range(22 + 2):
            if n == 11 and tt + 1 < NTT:
                pro[tt + 1] = prologue(xs[tt + 1])
            if n < 22:
                its[n] = (st1(n), st1(22 + n))
            if 0 <= n - 1 < 22:
                st2(its[n - 1][0], its[n - 1][1])
            if 0 <= n - 2 < 22:
                st3(its[n - 2][0], its[n - 2][1], n - 2)


def phase_e1(k, es):
    nc, S = k.nc, k.S
    sb = lambda n, s, d: es.enter_context(nc.sbuf_tensor(n + k.sfx, s, d))
    ps = lambda n, s, d: es.enter_context(nc.psum_tensor(n + k.sfx, s, d))
    PC, PR = k.PC, k.PR
    win = sb("winm", [128, 8, MLC], BF16)
    BW = 386

    def wk(kc, c0, c1):
        return [("winm", kc, b) for b in range(c0 // BW, (c1 - 1) // BW + 1)]

    for b in range(8):
        for kc in range(8):
            S.dma("pool", win[:, kc, b * BW:(b + 1) * BW], k.ml_in_w[kc * 128:(kc + 1) * 128, b * BW:(b + 1) * BW],
                  writes=[("winm", kc, b)])
    xt_ring = Ring(sb, "xtm", 2, [128, 4, D], F32)
    hn_ring = Ring(sb, "hnm", 1, [128, 4, D], BF16)
    hnT_ring = Ring(sb, "hnTm", 2, [128, 8, 512], BF16)
    ptr_ring = Ring(ps, "ptrm", 2, [128, 1024], BF16)
    pb_ring = Ring(ps, "pbm", 5, [128, 512], F32)
    junk = sb("junkm", [128, D], BF16)
    small = (sb("ssqm", [128, 4], F32), sb("lnvm", [128, 4], F32), sb("rstdm", [128, 4], F32))
    ob_ring = Ring(sb, "obm", 4, [128, 512], BF16)
    gg = sb("ggm", [128, 4, 16], F32)
    th = sb("thm", [128, 4, 16], F32)
    ef = sb("efm", [128, 4, 8], F32)
    gt_ring = Ring(sb, "gtm", 2, [128, 4, 16], F32)

    def load_x(tt):
        xt, xkey = xt_ring.nxt()
        S.dma("sp", xt[:], k.h2[tt * 512:(tt + 1) * 512, :].rearrange("(j p) d -> p j d", p=128), writes=[xkey])
        return xt, xkey

    def prologue(xx):
        xt, xkey = xx
        hnT, hkey = hnT_ring.nxt()
        norm_transpose(k, S, xt, xkey, PR("nw10"), hn_ring, hnT, hkey, ptr_ring, junk, small)
        return hnT, hkey

    xs = {0: load_x(0)}
    if NTT > 1:
        xs[1] = load_x(1)
    pro = {0: prologue(xs[0])}
    for tt in range(NTT):
        if tt + 2 < NTT:
            xs[tt + 2] = load_x(tt + 2)
        hnT, hkey = pro[tt]
        tok0 = tt * 512

        def proj_fm(col0):
            pb, pkey = pb_ring.nxt()
            for kc in range(8):
                S.op("pe", lambda e: e.matmul(pb[:], lhsT=win[:, kc, col0:col0 + 128], rhs=hnT[:, kc, :],
                                              start=(kc == 0), stop=(kc == 7)),
                     reads=[hkey] + wk(kc, col0, col0 + 128), writes=[pkey], accumulate=(kc > 0))
            return pb, pkey

        def proj_tm(j, col0, n):
            pb, pkey = pb_ring.nxt()
            for kc in range(8):
                S.op("pe", lambda e: e.matmul(pb[:, 0:n], lhsT=hnT[:, kc, j * 128:(j + 1) * 128],
                                              rhs=win[:, kc, col0:col0 + n], start=(kc == 0), stop=(kc == 7)),
                     reads=[hkey] + wk(kc, col0, col0 + n), writes=[pkey], accumulate=(kc > 0))
            return pb, pkey

        for c in range(4):
            pb, pkey = proj_fm(c * 128)
            ob, okey = ob_ring.nxt()
            S.op("act", lambda e: e.mul(ob[:], pb[:], 0.125), reads=[pkey], writes=[okey])
            S.dma("sp", k.mqT[c * 128:(c + 1) * 128, tok0:tok0 + 512], ob[:], reads=[okey])
        for c in range(4):
            pb, pkey = proj_fm(512 + c * 128)
            ob, okey = ob_ring.nxt()
            S.op("dve", lambda e: e.tensor_copy(ob[:], pb[:]), reads=[pkey], writes=[okey])
            S.dma("sp", k.mkT[c * 128:(c + 1) * 128, tok0:tok0 + 512], ob[:], reads=[okey])
        for c in range(8):
            pb, pkey = proj_fm(2048 + c * 128)
            ob, okey = ob_ring.nxt()
            S.op("act", lambda e: e.activation(out=ob[:], in_=pb[:], func=AF.Sigmoid), reads=[pkey], writes=[okey])
            S.dma("sp", k.soT[c * 128:(c + 1) * 128, tok0:tok0 + 512], ob[:], reads=[okey])
        if tt + 1 < NTT:
            pro[tt + 1] = prologue(xs[tt + 1])
        for j in range(4):
            for part in range(3):
                col0 = 512 if part == 0 else 1024 + (part - 1) * 512
                pb, pkey = proj_tm(j, col0, 512)
                ob, okey = ob_ring.nxt()
                if part == 1:
                    S.op("act", lambda e: e.copy(ob[:], pb[:]), reads=[pkey], writes=[okey])
                else:
                    S.op("dve", lambda e: e.tensor_copy(ob[:], pb[:]), reads=[pkey], writes=[okey])
                if part == 0:
                    S.dma("sp", k.mkt[tok0 + j * 128:tok0 + (j + 1) * 128, :], ob[:], reads=[okey])
                else:
                    S.dma("sp", k.mvt[tok0 + j * 128:tok0 + (j + 1) * 128, (part - 1) * 512:part * 512], ob[:],
                          reads=[okey])
        pb, pkey = pb_ring.nxt()
        for j in range(4):
            for kc in range(8):
                S.op("pe", lambda e: e.matmul(pb[:, j * 16:(j + 1) * 16], lhsT=hnT[:, kc, j * 128:(j + 1) * 128],
                                              rhs=win[:, kc, 3072:3088], start=(kc == 0), stop=(kc == 7)),
                     reads=[hkey] + wk(kc, 3072, 3088), writes=[pkey], accumulate=(j + kc > 0))
        gt, gtkey = gt_ring.nxt()
        r0 = PROW["mib"][0]
        S.op("dve", lambda e: e.tensor_tensor(gg[:], pb[:, 0:64].rearrange("p (j h) -> p j h", h=16),
                                              k.prow[:, r0:r0 + 16].unsqueeze(1).to_broadcast([128, 4, 16]), op=ALU.add),
             reads=[pkey, "prow"], writes=["ggm"])
        S.op("act", lambda e: e.activation(out=th[:], in_=gg[:], func=AF.Tanh, scale=1.0 / 15.0), reads=["ggm"],
             writes=["thm"])
        S.op("act", lambda e: e.mul(gt[:, :, 0:8], th[:, :, 0:8], 15.0), reads=["thm"], writes=[gtkey])
        S.op("act", lambda e: e.activation(out=ef[:], in_=th[:, :, 8:16], func=AF.Exp, scale=-15.0), reads=["thm"],
             writes=["efm"])
        S.op("act", lambda e: e.activation(out=ef[:], in_=ef[:], func=AF.Ln, bias=1.0), reads=["efm"], writes=["efm"])
        S.op("act", lambda e: e.mul(gt[:, :, 8:16], ef[:], -1.0), reads=["efm"], writes=[gtkey], accumulate=True)
        S.dma("sp", k.gts[tok0:tok0 + 512, :].rearrange("(j p) h -> p j h", p=128), gt[:], reads=[gtkey])


def phase_f1(k, es):
    nc, S = k.nc, k.S
    sb = lambda n, s, d: es.enter_context(nc.sbuf_tensor(n + k.sfx, s, d))
    ps = lambda n, s, d: es.enter_context(nc.psum_tensor(n + k.sfx, s, d))
    CF, CB, PC = k.CF, k.CB, k.PC
    q_ring = Ring(sb, "mq", 2, [128, 4, 512], BF16)
    k_ring = Ring(sb, "mk", 2, [128, 4, 512], BF16)
    kt_ring = Ring(sb, "mkt", 2, [128, 4, 512], BF16)
    vt_ring = Ring(sb, "mvt", 2, [128, 4, 1024], BF16)
    so_ring = Ring(sb, "mso", 2, [128, 8, 512], BF16)
    gl_ring = Ring(sb, "mgl", 2, [128, 4, 16], F32)
    ho_ring = Ring(sb, "mho", 2, [128, 8, 512], BF16)
    g4 = sb("mg4", [128, 512], F32)
    sm_ring = Ring(sb, "msm", 3, [128, 32], F32)
    cdp_ring = Ring(sb, "mcdp", 3, [128, 4], F32)
    dt_ring = Ring(sb, "mdt", 3, [128, 512], F32)
    eb_ring = Ring(sb, "meb", 3, [128, 512], F32)
    st_ring = Ring(sb, "mst", 3, [128, 512], BF16)
    qe_ring = Ring(sb, "mqe", 3, [128, 512], BF16)
    ad_ring = Ring(sb, "mad", 3, [128, 512], F32)
    hT_ring = Ring(sb, "mhT", 3, [128, 512], F32)
    sq_ring = Ring(sb, "msq", 3, [128, 512], BF16)
    ln_ring = Ring(sb, "mln", 3, [128, 512], F32)
    rs_ring = Ring(sb, "mrs", 3, [128, 512], F32)
    kd_ring = Ring(sb, "mkd", 3, [128, 8, 64], BF16)
    Cf = sb("mCf", [128, 4, 128], F32)
    Cb_ = sb("mCb", [128, 4, 128], BF16)
    nf = sb("mnf", [128, 4], F32)
    nb = sb("mnb", [128, 4], BF16)
    pG_ring = Ring(ps, "mpG", 1, [128, 64], F32)
    pD_ring = Ring(ps, "mpD", 2, [128, 512], F32)
    pkq_ring = Ring(ps, "mpkq", 1, [128, 512], F32)
    pN_ring = Ring(ps, "mpN", 1, [128, 512], F32)
    pden_ring = Ring(ps, "mpden", 1, [128, 512], F32)
    pss_ring = Ring(ps, "mpss", 1, [128, 512], F32)
    pC_ring = Ring(ps, "mpC", 1, [128, 4, 128], F32)

    for q in range(4):
        S.op("pool", lambda e: e.tensor_copy(g4[:, q * 128:(q + 1) * 128], CF("gt")), reads=["cf"], writes=["mg4"],
             accumulate=(q > 0))

    def load(tt):
        tok0 = tt * 512
        r = {}
        for name, ring, src in (("q", q_ring, k.mqT), ("k", k_ring, k.mkT), ("so", so_ring, k.soT)):
            t, key = ring.nxt()
            S.dma("sp", t[:], src[:, tok0:tok0 + 512].rearrange("(c p) t -> p c t", p=128), writes=[key])
            r[name] = (t, key)
        for name, ring, src in (("kt", kt_ring, k.mkt), ("vt", vt_ring, k.mvt), ("gl", gl_ring, k.gts)):
            t, key = ring.nxt()
            S.dma("sp", t[:], src[tok0:tok0 + 512, :].rearrange("(j p) d -> p j d", p=128), writes=[key])
            r[name] = (t, key)
        r["ho"] = ho_ring.nxt()
        return r

    tiles = {}

    def get_tile(tt):
        if tt not in tiles:
            tiles[tt] = load(tt)
        return tiles[tt]

    def P(c):
        cur = get_tile(c["tt"])
        j = c["j"]
        (gl, glkey), (kt, ktkey) = cur["gl"], cur["kt"]
        logi = gl[:, j, 0:8]
        logf = gl[:, j, 8:16]
        pG, pGkey = pG_ring.nxt()
        c["pG"], c["pGkey"] = pG, pGkey
        for i, cname in enumerate(("le", "gt", "ones")):
            S.op("pe", lambda e: e.matmul(pG[:, i * 8:(i + 1) * 8], lhsT=CF(cname), rhs=logf, start=True, stop=True),
                 reads=[glkey, "cf"], writes=[(pGkey, "g")], accumulate=(i > 0))
        sm, smkey = sm_ring.nxt()
        cdp, cdpkey = cdp_ring.nxt()
        c["sm"], c["smkey"], c["cdp"], c["cdpkey"] = sm, smkey, cdp, cdpkey
        S.op("dve", lambda e: e.tensor_tensor(sm[:, 0:8], logi, pG[:, 0:8], op=ALU.subtract),
             reads=[glkey, (pGkey, "g")], writes=[smkey])
        S.op("dve", lambda e: e.tensor_tensor(sm[:, 24:32], logi, pG[:, 8:16], op=ALU.add),
             reads=[glkey, (pGkey, "g")], writes=[smkey], accumulate=True)
        S.op("act", lambda e: e.activation(out=sm[:, 8:16], in_=sm[:, 24:32], func=AF.Exp), reads=[smkey], writes=[smkey])
        S.op("act", lambda e: e.activation(out=cdp[0:64, :], in_=pG[0:64, 16:24:2], func=AF.Exp),
             reads=[(pGkey, "g")], writes=[cdpkey])
        S.op("act", lambda e: e.activation(out=cdp[64:128, :], in_=pG[64:128, 17:24:2], func=AF.Exp),
             reads=[(pGkey, "g")], writes=[cdpkey], accumulate=True)
        kd, kdkey = kd_ring.nxt()
        c["kd"], c["kdkey"] = kd, kdkey
        S.op("pool", lambda e: e.tensor_tensor(kd[:], kt[:, j, :].rearrange("p (h d) -> p h d", d=64),
                                               sm[:, 8:16].unsqueeze(2).to_broadcast([128, 8, 64]), op=ALU.mult),
             reads=[ktkey, smkey], writes=[kdkey])

    def H(c):
        cur = get_tile(c["tt"])
        j = c["j"]
        js = slice(j * 128, (j + 1) * 128)
        (mq, qkey), (mk, kkey), (so, sokey) = cur["q"], cur["k"], cur["so"]
        (kt, ktkey), (vt, vtkey), (gl, glkey) = cur["kt"], cur["vt"], cur["gl"]
        ho, hokey = cur["ho"]
        sm, smkey, kd, kdkey, pG, pGkey = c["sm"], c["smkey"], c["kd"], c["kdkey"], c["pG"], c["pGkey"]
        if c["tt"] % 4 == 0 and j == 0:
            S.op("pool", lambda e: e.memset(Cf[:], 0.0), writes=["mCf"])
            S.op("pool", lambda e: e.memset(Cb_[:], 0.0), writes=["mCb"])
            S.op("pool", lambda e: e.memset(nf[:], 0.0), writes=["mnf"])
            S.op("pool", lambda e: e.memset(nb[:], 0.0), writes=["mnb"])
        pC, pCkey = pC_ring.nxt()
        c["pC"], c["pCkey"] = pC, pCkey
        CS = lambda hh: slice(hh * 128, (hh + 1) * 128)

        def front(hq):
            pDm, pDmkey = pD_ring.nxt()
            pDu, pDukey = pD_ring.nxt()
            pkq, pkqkey = pkq_ring.nxt()
            pN, pNkey = pN_ring.nxt()
            pden, pdenkey = pden_ring.nxt()
            hs = [hq * 4 + hh for hh in range(4)]
            S.op("pe", lambda e: e.matmul(pDm[:], lhsT=CF("negbig"), rhs=g4[:], start=True, stop=False),
                 reads=["cf", "mg4"], writes=[pDmkey])
            for hh, h in enumerate(hs):
                po, pr = (h % 2) * 64, h // 2
                lb = gl[:, j, 8 + h:9 + h].to_broadcast([128, 128])
                S.op("pe", lambda e: e.matmul(pDm[:, CS(hh)], lhsT=lb, rhs=CF("le"), start=False, stop=True),
                     reads=[glkey, "cf"], writes=[pDmkey], accumulate=True)
                S.op("pe", lambda e: e.matmul(pDu[:, CS(hh)], lhsT=lb, rhs=CF("le"), start=True, stop=True),
                     reads=[glkey, "cf"], writes=[pDukey], accumulate=(hh > 0))
                S.op("pe", lambda e: e.matmul(pkq[:, CS(hh)], lhsT=mk[po:po + 64, pr, js], rhs=mq[po:po + 64, pr, js],
                                              start=True, stop=True),
                     reads=[qkey, kkey], writes=[pkqkey], accumulate=(hh > 0))
            eb, ebkey = eb_ring.nxt()
            S.op("act", lambda e: e.activation(out=eb[:], in_=pDu[:], func=AF.Exp), reads=[pDukey], writes=[ebkey])
            dtl, dtkey = dt_ring.nxt()
            for hh, h in enumerate(hs):
                S.op("act", lambda e: e.activation(out=dtl[:, CS(hh)], in_=pDm[:, CS(hh)], func=AF.Exp,
                                                   bias=sm[:, h:h + 1]),
                     reads=[pDmkey, smkey], writes=[dtkey], accumulate=(hh > 0))
            st, stkey = st_ring.nxt()
            S.op("dve", lambda e: e.tensor_tensor(st[:], dtl[:], pkq[:], op=ALU.mult), reads=[dtkey, pkqkey],
                 writes=[stkey])
            qe, qekey = qe_ring.nxt()
            for hh, h in enumerate(hs):
                po, pr = (h % 2) * 64, h // 2
                S.op("pool", lambda e: e.tensor_tensor(qe[po:po + 64, CS(hh)], mq[po:po + 64, pr, js],
                                                       eb[po:po + 64, CS(hh)], op=ALU.mult),
                     reads=[qkey, ebkey], writes=[qekey], accumulate=(hh > 0))
            for hh, h in enumerate(hs):
                po, pr = (h % 2) * 64, h // 2
                S.op("pe", lambda e: e.matmul(pN[:, CS(hh)], lhsT=vt[:, j, h * 128:(h + 1) * 128], rhs=st[:, CS(hh)],
                                              start=True, stop=False), reads=[vtkey, stkey], writes=[pNkey],
                     accumulate=(hh > 0))
                S.op("pe", lambda e: e.matmul(pN[:, CS(hh)], lhsT=Cb_[po:po + 64, pr, :], rhs=qe[po:po + 64, CS(hh)],
                                              start=False, stop=True), reads=["mCb", qekey], writes=[pNkey],
                     accumulate=True)
                S.op("pe", lambda e: e.matmul(pden[:, CS(hh)], lhsT=CB("ones"), rhs=st[:, CS(hh)], start=True, stop=False),
                     reads=["cb", stkey], writes=[pdenkey], accumulate=(hh > 0))
                S.op("pe", lambda e: e.matmul(pden[:, CS(hh)], lhsT=nb[po:po + 64, pr:pr + 1].to_broadcast([64, 128]),
                                              rhs=qe[po:po + 64, CS(hh)], start=False, stop=True),
                     reads=["mnb", qekey], writes=[pdenkey], accumulate=True)
            for hh, h in enumerate(hs):
                po, pr = (h % 2) * 64, h // 2
                S.op("pe", lambda e: e.matmul(pC[po:po + 64, pr, :], lhsT=kd[:, h, :], rhs=vt[:, j, h * 128:(h + 1) * 128],
                                              start=True, stop=True), reads=[kdkey, vtkey], writes=[pCkey],
                     accumulate=(h > 0))
                S.op("pe", lambda e: e.matmul(pG[po:po + 64, 32 + pr:33 + pr], lhsT=kd[:, h, :], rhs=CB("ones")[:, 0:1],
                                              start=True, stop=True), reads=[kdkey, "cb"], writes=[(pGkey, "n")],
                     accumulate=(h > 0))
            return {"hq": hq, "hs": hs, "pN": pN, "pNkey": pNkey, "pden": pden, "pdenkey": pdenkey}

        def tailA(g):
            pN, pNkey, pden, pdenkey = g["pN"], g["pNkey"], g["pden"], g["pdenkey"]
            ad, adkey = ad_ring.nxt()
            S.op("act", lambda e: e.activation(out=ad[:], in_=pden[:], func=AF.Abs), reads=[pdenkey], writes=[adkey])
            S.op("dve", lambda e: e.tensor_scalar(ad[:], ad[:], 1.0, None, op0=ALU.max), reads=[adkey], writes=[adkey])
            S.op("dve", lambda e: e.reciprocal(ad[:], ad[:]), reads=[adkey], writes=[adkey])
            hT, hTkey = hT_ring.nxt()
            S.op("dve", lambda e: e.tensor_tensor(hT[:], pN[:], ad[:], op=ALU.mult), reads=[pNkey, adkey], writes=[hTkey])
            sq, sqkey = sq_ring.nxt()
            S.op("act", lambda e: e.activation(out=sq[:], in_=hT[:], func=AF.Square), reads=[hTkey], writes=[sqkey])
            g.update(hT=hT, hTkey=hTkey, sq=sq, sqkey=sqkey)

        def tailB(g):
            hq, hs, hT, hTkey, sq, sqkey = g["hq"], g["hs"], g["hT"], g["hTkey"], g["sq"], g["sqkey"]
            pss, psskey = pss_ring.nxt()
            S.op("pe", lambda e: e.matmul(pss[:], lhsT=CB("ones"), rhs=sq[:], start=True, stop=True),
                 reads=["cb", sqkey], writes=[psskey])
            lnb, lnkey = ln_ring.nxt()
            rs, rskey = rs_ring.nxt()
            S.op("act", lambda e: e.activation(out=lnb[:], in_=pss[:], func=AF.Ln, bias=EPS, scale=1.0 / 128),
                 reads=[psskey], writes=[lnkey])
            S.op("act", lambda e: e.activation(out=rs[:], in_=lnb[:], func=AF.Exp, scale=-0.5), reads=[lnkey],
                 writes=[rskey])
            for hh, h in enumerate(hs):
                S.op("dve", lambda e: e.scalar_tensor_tensor(out=hT[:, CS(hh)], in0=hT[:, CS(hh)], scalar=PC("mnw", h),
                                                             in1=rs[:, CS(hh)], op0=ALU.mult, op1=ALU.mult),
                     reads=[hTkey, rskey, "pcol"], writes=[hTkey])
            S.op("pool", lambda e: e.tensor_tensor(ho[:, hq * 4:(hq + 1) * 4, js], hT[:].rearrange("p (h t) -> p h t", t=128),
                                                   so[:, hq * 4:(hq + 1) * 4, js], op=ALU.mult),
                 reads=[hTkey, sokey], writes=[hokey], accumulate=(j + hq > 0))

        g0 = front(0)
        tailA(g0)
        g1 = front(1)
        tailB(g0)
        tailA(g1)
        tailB(g1)

    def E(c):
        cdp, cdpkey, pC, pCkey, pG, pGkey = c["cdp"], c["cdpkey"], c["pC"], c["pCkey"], c["pG"], c["pGkey"]
        cdb = cdp[:].unsqueeze(2).to_broadcast([128, 4, 128])
        S.op("dve", lambda e: e.tensor_tensor(Cf[:], Cf[:], cdb, op=ALU.mult), reads=["mCf", cdpkey], writes=["mCf"])
        S.op("dve", lambda e: e.tensor_tensor(Cf[:], Cf[:], pC[:], op=ALU.add), reads=["mCf", pCkey], writes=["mCf"])
        S.op("act", lambda e: e.copy(Cb_[:], Cf[:]), reads=["mCf"], writes=["mCb"])
        S.op("dve", lambda e: e.tensor_tensor(nf[:], nf[:], cdp[:], op=ALU.mult), reads=["mnf", cdpkey], writes=["mnf"])
        S.op("dve", lambda e: e.tensor_tensor(nf[:], nf[:], pG[:, 32:36], op=ALU.add), reads=["mnf", (pGkey, "n")],
             writes=["mnf"])
        S.op("act", lambda e: e.copy(nb[:], nf[:]), reads=["mnf"], writes=["mnb"])
        if c["j"] == 3:
            tok0 = c["tt"] * 512
            ho, hokey = get_tile(c["tt"])["ho"]
            S.dma("sp", k.hmT[:, tok0:tok0 + 512].rearrange("(c p) t -> p c t", p=128), ho[:], reads=[hokey])

    chunks = [{"tt": tt, "j": j} for tt in range(NTT) for j in range(4)]
    get_tile(0)
    P(chunks[0])
    for ci, c in enumerate(chunks):
        if c["j"] == 0 and c["tt"] + 1 < NTT:
            get_tile(c["tt"] + 1)
        H(c)
        if ci + 1 < len(chunks):
            P(chunks[ci + 1])
        E(c)


_NC_CACHE = {}


def kernel(**inputs):
    inp = {k_: np.asarray(v) for k_, v in inputs.items()}
    if "nc" not in _NC_CACHE:
        _NC_CACHE["nc"] = build()
    nc = _NC_CACHE["nc"]
    pc, pr = pack_params(inp)
    consts = make_consts()
    x = np.ascontiguousarray(inp["x"], dtype=np.float32)
    shared = {
        "pcol": pc, "prow": pr, "consts": consts,
        "hy_in_w": np.ascontiguousarray(inp["hy_in_w"][0], dtype=np.float32),
        "hy_out_w": np.ascontiguousarray(inp["hy_out_w"][0], dtype=np.float32),
        "ml_in_w": np.ascontiguousarray(inp["ml_in_w"][0], dtype=np.float32),
        "ml_out_w": np.ascontiguousarray(inp["ml_out_w"][0], dtype=np.float32),
        "ffn_up_w": np.ascontiguousarray(inp["ffn_up_w"], dtype=np.float32),
        "ffn_down_w": np.ascontiguousarray(inp["ffn_down_w"], dtype=np.float32),
    }
    in_maps = []
    for c in range(NCORES):
        m = dict(shared)
        m["x"] = np.ascontiguousarray(x[2 * c:2 * c + 2].reshape(NTOK, D))
        in_maps.append(m)
    res = run_bass_kernel_spmd(nc, in_maps, core_ids=list(range(NCORES)))
    out = np.concatenate([np.asarray(r["out"]).reshape(2, L, D) for r in res.results], axis=0)
    return out.astype(np.float32)
```
